# Optimizing a Trainium2 kernel written in Bass

```python
import jax
import jax.numpy as jnp
from jax import lax
import numpy as np

D_MODEL = 1024
BATCH = 16
SEQ = 2048
DEPTH = 2

CTX_LEN = 256
GRID_W = 64
HEAD_DIM = 128
N_HEADS = 8
N_KV_HEADS = 2
WINDOW = 128
BLOCK = 128
ROPE_THETA = 10000.0
LRU_W = 1024
LRU_BLOCKS = 8
LRU_BW = LRU_W // LRU_BLOCKS
LRU_C = 8.0
CONV_W = 4
CONV_LEFT = 2
N_EXPERTS = 16
EC_FACTOR = 2
D_EXPERT = 2048
N_MOD = 6
EPS = 1e-6
Q_W = N_HEADS * HEAD_DIM
KV_W = N_KV_HEADS * HEAD_DIM
IN_SPLITS = (Q_W, KV_W, KV_W, LRU_W, LRU_W, D_MODEL, D_MODEL)
IN_W = Q_W + 2 * KV_W + 2 * LRU_W + 2 * D_MODEL

kernel_name = 'hybrid_dit_swa_rglru_ecmoe'


def rmsnorm(x, g):
    xf = x.astype(jnp.float32)
    y = xf * lax.rsqrt(jnp.mean(xf * xf, axis=-1, keepdims=True) + EPS)
    return (y * g.astype(jnp.float32)).astype(x.dtype)


def modulate(h, shift, scale):
    return h * (1 + scale) + shift


def split_in(proj):
    bounds = np.cumsum(IN_SPLITS)[:-1].tolist()
    return jnp.split(proj, bounds, axis=-1)


def to_heads(t, n):
    return t.reshape(t.shape[:-1] + (n, HEAD_DIM))


def rope_1d(v, pos):
    half = v.shape[-1] // 2
    freqs = ROPE_THETA ** (-jnp.arange(half, dtype=jnp.float32) / half)
    ang = pos.astype(jnp.float32)[:, None] * freqs[None, :]
    cos = jnp.cos(ang)[:, None, :].astype(v.dtype)
    sin = jnp.sin(ang)[:, None, :].astype(v.dtype)
    v1, v2 = v[..., :half], v[..., half:]
    return jnp.concatenate([v1 * cos - v2 * sin, v1 * sin + v2 * cos], axis=-1)


def axial_rope(v, rows, cols):
    h = v.shape[-1] // 2
    return jnp.concatenate([rope_1d(v[..., :h], rows), rope_1d(v[..., h:], cols)], axis=-1)


def sink_softmax(scores, sink):
    m = sink
    for s in scores:
        m = jnp.maximum(m, jnp.max(s, axis=-1))
    ps = [jnp.exp(s - m[..., None]) for s in scores]
    denom = jnp.exp(sink - m)
    for p in ps:
        denom = denom + jnp.sum(p, axis=-1)
    return [p / denom[..., None] for p in ps]


def _band(t, nb):
    B, S, KV, d = t.shape
    tp = jnp.pad(t, ((0, 0), (BLOCK, BLOCK), (0, 0), (0, 0))).reshape(B, nb + 2, BLOCK, KV, d)
    return jnp.concatenate([tp[:, :-2], tp[:, 1:-1], tp[:, 2:]], axis=2)


def banded_attention(q, k, v, k_ctx, v_ctx, sink):
    B, S, H, d = q.shape
    G = H // N_KV_HEADS
    nb = S // BLOCK
    scale = HEAD_DIM ** -0.5
    qb = q.reshape(B, nb, BLOCK, N_KV_HEADS, G, d)
    kb, vb = _band(k, nb), _band(v, nb)
    qpos = jnp.arange(S).reshape(nb, BLOCK)
    kpos = (jnp.arange(nb)[:, None] - 1) * BLOCK + jnp.arange(3 * BLOCK)[None, :]
    kp = kpos[:, None, :]
    valid = (kp >= 0) & (kp < S) & (jnp.abs(qpos[:, :, None] - kp) <= WINDOW)
    s_band = jnp.einsum('bnqkgd,bnskd->bnkgqs', qb, kb).astype(jnp.float32) * scale
    s_band = jnp.where(valid[None, :, None, None], s_band, -jnp.inf)
    s_ctx = jnp.einsum('bnqkgd,blkd->bnkgql', qb, k_ctx).astype(jnp.float32) * scale
    p_band, p_ctx = sink_softmax([s_band, s_ctx], sink.reshape(N_KV_HEADS, G, 1).astype(jnp.float32))
    o = (jnp.einsum('bnkgqs,bnskd->bnqkgd', p_band.astype(v.dtype), vb)
         + jnp.einsum('bnkgql,blkd->bnqkgd', p_ctx.astype(v.dtype), v_ctx))
    return o.reshape(B, S, H * d)


def context_attention(q, k, v, sink):
    B, L, H, d = q.shape
    G = H // N_KV_HEADS
    qg = q.reshape(B, L, N_KV_HEADS, G, d)
    s = jnp.einsum('blkgd,bmkd->bkglm', qg, k).astype(jnp.float32) * (HEAD_DIM ** -0.5)
    (p,) = sink_softmax([s], sink.reshape(N_KV_HEADS, G, 1).astype(jnp.float32))
    o = jnp.einsum('bkglm,bmkd->blkgd', p.astype(v.dtype), v)
    return o.reshape(B, L, H * d)


def short_conv(u, w, b):
    T = u.shape[1]
    up = jnp.pad(u, ((0, 0), (CONV_LEFT, CONV_W - 1 - CONV_LEFT), (0, 0)))
    out = b
    for kk in range(CONV_W):
        out = out + w[kk] * up[:, kk:kk + T]
    return out


def _combine(e1, e2):
    a1, b1 = e1
    a2, b2 = e2
    return a1 * a2, a2 * b1 + b2


def linear_scan(a, b, h0, reverse):
    edge = -1 if reverse else 0
    b = b.at[:, edge].add(a[:, edge] * h0)
    _, h = lax.associative_scan(_combine, (a, b), reverse=reverse, axis=1)
    return h


def rg_lru(u, wa, ba, wx, bx, lam, h0, reverse):
    B, T, W = u.shape
    ub = u.reshape(B, T, LRU_BLOCKS, LRU_BW)
    gate_r = jax.nn.sigmoid(jnp.einsum('btni,nij->btnj', ub, wa).reshape(B, T, W) + ba)
    gate_i = jax.nn.sigmoid(jnp.einsum('btni,nij->btnj', ub, wx).reshape(B, T, W) + bx)
    log_a = (-LRU_C * gate_r.astype(jnp.float32)) * jax.nn.softplus(-lam.astype(jnp.float32))
    a = jnp.exp(log_a)
    b = jnp.sqrt(-jnp.expm1(2.0 * log_a)) * (gate_i * u).astype(jnp.float32)
    return linear_scan(a, b, h0, reverse)


def recurrent_mixer(u_ctx, u_lat, conv_w, conv_b, lru_wa, lru_ba, lru_wx, lru_bx, lru_lambda, need_ctx_out):
    uc = short_conv(u_ctx, conv_w, conv_b)
    ul = short_conv(u_lat, conv_w, conv_b)
    h0 = jnp.zeros((u_ctx.shape[0], LRU_W), jnp.float32)
    hc_f = rg_lru(uc, lru_wa[0], lru_ba[0], lru_wx[0], lru_bx[0], lru_lambda[0], h0, False)
    hc_b = rg_lru(uc, lru_wa[1], lru_ba[1], lru_wx[1], lru_bx[1], lru_lambda[1], h0, True)
    hl_f = rg_lru(ul, lru_wa[0], lru_ba[0], lru_wx[0], lru_bx[0], lru_lambda[0], hc_f[:, -1], False)
    hl_b = rg_lru(ul, lru_wa[1], lru_ba[1], lru_wx[1], lru_bx[1], lru_lambda[1], hc_b[:, 0], True)
    y_lat = (hl_f + hl_b).astype(u_lat.dtype)
    y_ctx = (hc_f + hc_b).astype(u_ctx.dtype) if need_ctx_out else None
    return y_lat, y_ctx


def merge_branches(att, rec, z, m_att, m_rec, w_attn_br, w_rec_br, w_out):
    att_d = att @ w_attn_br
    rec_d = (rec * jax.nn.gelu(z)) @ w_rec_br
    return (jax.nn.sigmoid(m_att) * att_d + jax.nn.sigmoid(m_rec) * rec_d) @ w_out


def expert_choice_moe(h, w_router, w_gate, w_up, w_down):
    B, n, D = h.shape
    cap = EC_FACTOR * n // N_EXPERTS
    aff = jax.nn.softmax((h @ w_router).astype(jnp.float32), axis=-1)
    g, idx = lax.top_k(jnp.swapaxes(aff, 1, 2), cap)
    xe = jax.vmap(lambda hb, ib: hb[ib])(h, idx)
    hid = jax.nn.silu(jnp.einsum('becd,edf->becf', xe, w_gate)) * jnp.einsum('becd,edf->becf', xe, w_up)
    ye = jnp.einsum('becf,efd->becd', hid, w_down) * g[..., None].astype(h.dtype)
    return jax.vmap(lambda ib, yb: jnp.zeros((n, D), yb.dtype).at[ib.reshape(-1)].add(yb.reshape(-1, D)))(idx, ye)


def hybrid_layer(x, ctx, mod_lat, mod_ctx, rows, cols, norm_mix_g, w_in, attn_sink, conv_w, conv_b,
                 lru_wa, lru_ba, lru_wx, lru_bx, lru_lambda, w_attn_br, w_rec_br, w_out,
                 norm_ffn_g, w_router, w_gate, w_up, w_down, last):
    sh1, sc1, g1, sh2, sc2, g2 = jnp.split(mod_lat, N_MOD, axis=-1)
    csh1, csc1, cg1, csh2, csc2, cg2 = jnp.split(mod_ctx, N_MOD, axis=-1)
    hl = modulate(rmsnorm(x, norm_mix_g), sh1, sc1)
    hc = modulate(rmsnorm(ctx, norm_mix_g), csh1, csc1)
    ql, kl, vl, ul, zl, mal, mrl = split_in(hl @ w_in)
    qc, kc, vc, uc, zc, mac, mrc = split_in(hc @ w_in)
    kc_h, vc_h = to_heads(kc, N_KV_HEADS), to_heads(vc, N_KV_HEADS)
    att_lat = banded_attention(axial_rope(to_heads(ql, N_HEADS), rows, cols),
                               axial_rope(to_heads(kl, N_KV_HEADS), rows, cols),
                               to_heads(vl, N_KV_HEADS), kc_h, vc_h, attn_sink)
    rec_lat, rec_ctx = recurrent_mixer(uc, ul, conv_w, conv_b, lru_wa, lru_ba, lru_wx, lru_bx, lru_lambda, not last)
    x = x + g1 * merge_branches(att_lat, rec_lat, zl, mal, mrl, w_attn_br, w_rec_br, w_out)
    x = x + g2 * expert_choice_moe(modulate(rmsnorm(x, norm_ffn_g), sh2, sc2), w_router, w_gate, w_up, w_down)
    if not last:
        att_ctx = context_attention(to_heads(qc, N_HEADS), kc_h, vc_h, attn_sink)
        ctx = ctx + cg1 * merge_branches(att_ctx, rec_ctx, zc, mac, mrc, w_attn_br, w_rec_br, w_out)
        ctx = ctx + cg2 * expert_choice_moe(modulate(rmsnorm(ctx, norm_ffn_g), csh2, csc2), w_router, w_gate, w_up, w_down)
    return x, ctx


def setup_inputs(seed: int = 0) -> dict:
    key = jax.random.key(seed)
    ks = jax.random.split(key, 26)
    f32 = jnp.float32
    D = D_MODEL

    def nrm(k, shape, scale):
        return jax.random.normal(k, shape, f32) * scale

    a0 = jax.random.uniform(ks[12], (DEPTH, 2, LRU_W), f32, 0.9, 0.999) ** (1.0 / LRU_C)
    return {
        'x': nrm(ks[0], (BATCH, SEQ, D), 1.0),
        'c': nrm(ks[1], (BATCH, D), 1.0),
        'ctx': nrm(ks[2], (BATCH, CTX_LEN, D), 1.0),
        'c_ctx': nrm(ks[3], (D,), 1.0),
        'ada_w': nrm(ks[4], (DEPTH, D, N_MOD * D), 0.5 * D ** -0.5),
        'ada_b': nrm(ks[5], (DEPTH, N_MOD * D), 0.02),
        'norm_mix_g': 1.0 + nrm(ks[6], (DEPTH, D), 0.02),
        'w_in': nrm(ks[7], (DEPTH, D, IN_W), D ** -0.5),
        'attn_sink': nrm(ks[8], (DEPTH, N_HEADS), 0.5),
        'conv_w': nrm(ks[9], (DEPTH, CONV_W, LRU_W), CONV_W ** -0.5),
        'conv_b': nrm(ks[10], (DEPTH, LRU_W), 0.02),
        'lru_wa': nrm(ks[11], (DEPTH, 2, LRU_BLOCKS, LRU_BW, LRU_BW), LRU_BW ** -0.5),
        'lru_ba': nrm(ks[13], (DEPTH, 2, LRU_W), 0.02),
        'lru_wx': nrm(ks[14], (DEPTH, 2, LRU_BLOCKS, LRU_BW, LRU_BW), LRU_BW ** -0.5),
        'lru_bx': nrm(ks[15], (DEPTH, 2, LRU_W), 0.02),
        'lru_lambda': jnp.log(a0) - jnp.log1p(-a0),
        'w_attn_br': nrm(ks[16], (DEPTH, Q_W, D), Q_W ** -0.5),
        'w_rec_br': nrm(ks[17], (DEPTH, LRU_W, D), LRU_W ** -0.5),
        'w_out': nrm(ks[18], (DEPTH, D, D), D ** -0.5),
        'norm_ffn_g': 1.0 + nrm(ks[19], (DEPTH, D), 0.02),
        'w_router': nrm(ks[20], (DEPTH, D, N_EXPERTS), D ** -0.5),
        'w_gate': nrm(ks[21], (DEPTH, N_EXPERTS, D, D_EXPERT), D ** -0.5),
        'w_up': nrm(ks[22], (DEPTH, N_EXPERTS, D, D_EXPERT), D ** -0.5),
        'w_down': nrm(ks[23], (DEPTH, N_EXPERTS, D_EXPERT, D), D_EXPERT ** -0.5),
        'final_norm_g': 1.0 + nrm(ks[24], (D,), 0.02),
    }


def reference(x, c, ctx, c_ctx, ada_w, ada_b, norm_mix_g, w_in, attn_sink, conv_w, conv_b,
              lru_wa, lru_ba, lru_wx, lru_bx, lru_lambda, w_attn_br, w_rec_br, w_out,
              norm_ffn_g, w_router, w_gate, w_up, w_down, final_norm_g):
    S = x.shape[1]
    ROWS = S // GRID_W
    rows = jnp.repeat(jnp.arange(ROWS, dtype=jnp.int32), GRID_W)
    cols = jnp.tile(jnp.arange(GRID_W, dtype=jnp.int32), ROWS)
    c_act = jax.nn.silu(c)
    cctx_act = jax.nn.silu(c_ctx)
    for l in range(DEPTH):
        mod_lat = (c_act @ ada_w[l] + ada_b[l])[:, None, :]
        mod_ctx = cctx_act @ ada_w[l] + ada_b[l]
        x, ctx = hybrid_layer(x, ctx, mod_lat, mod_ctx, rows, cols, norm_mix_g[l], w_in[l], attn_sink[l],
                              conv_w[l], conv_b[l], lru_wa[l], lru_ba[l], lru_wx[l], lru_bx[l], lru_lambda[l],
                              w_attn_br[l], w_rec_br[l], w_out[l], norm_ffn_g[l], w_router[l],
                              w_gate[l], w_up[l], w_down[l], l == DEPTH - 1)
    return rmsnorm(x, final_norm_g)
```

```python
import numpy as np
import concourse.bass as bass
import concourse.mybir as mybir

F32 = mybir.dt.float32
BF16 = mybir.dt.bfloat16
I32 = mybir.dt.int32
AF = mybir.ActivationFunctionType
ALU = mybir.AluOpType
AX = mybir.AxisListType
from concourse.bass_utils import run_bass_kernel_spmd
ENGS = ("pe", "act", "dve", "pool", "sp")


class Op:
    __slots__ = ("eng", "emit", "deps", "dma_deps", "signal", "count", "is_dma", "dsem", "dval", "prev_dval", "idx")
    _ctr = 0

    def __init__(self, eng, emit, is_dma=False):
        self.eng = eng
        self.emit = emit
        self.deps = {}
        self.dma_deps = []
        self.signal = False
        self.count = 0
        self.is_dma = is_dma
        self.dsem = None
        self.dval = 0
        self.prev_dval = 0
        Op._ctr += 1
        self.idx = Op._ctr


class Tile:
    def __init__(self, ap, name=""):
        self.ap = ap
        self.name = name
        self.w_c = {}
        self.w_d = []
        self.r_c = {}
        self.r_d = []
        self.g_c = {}
        self.g_d = []

    def __getitem__(self, k):
        return self.ap[k]


def _merge(dst_c, dst_d, src_c, src_d):
    for e, o in src_c.items():
        if e not in dst_c or dst_c[e].idx < o.idx:
            dst_c[e] = o
    for o in src_d:
        if o not in dst_d:
            dst_d.append(o)


class FW:
    def __init__(self, nc, n_dma_sems=4):
        self.nc = nc
        self.ops = {e: [] for e in ENGS}
        self.sem = {}
        self.dma_pool = {}
        self.dma_rr = {}
        self.n_dma_sems = n_dma_sems
        self.out_dmas = []
        self.last = {e: None for e in ENGS}
        self._cms = []

    def setup_sems(self, es):
        nc = self.nc
        for e in ENGS:
            self.sem[e] = es.enter_context(nc.semaphore("s_" + e))
        for q in ("sp", "act", "pool"):
            self.dma_pool[q] = [[es.enter_context(nc.semaphore("d_%s%d" % (q, i))), 0] for i in range(self.n_dma_sems)]
            self.dma_rr[q] = 0

    def _add_read(self, op, t):
        _merge(op.deps, op.dma_deps, t.w_c, t.w_d)
        if op.is_dma:
            t.r_d.append(op)
        else:
            t.r_c[op.eng] = op

    def _add_write(self, op, t, partial):
        if (not partial) or t.r_c or t.r_d:
            t.g_c = {}
            t.g_d = []
            _merge(t.g_c, t.g_d, t.r_c, t.r_d)
            _merge(t.g_c, t.g_d, t.w_c, t.w_d)
            t.r_c, t.r_d, t.w_c, t.w_d = {}, [], {}, []
        _merge(op.deps, op.dma_deps, t.g_c, t.g_d)
        if op.is_dma:
            t.w_d.append(op)
        else:
            t.w_c[op.eng] = op

    def _record(self, o, reads, writes, pwrites):
        wset = set(id(t) for t in writes) | set(id(t) for t in pwrites)
        for t in reads:
            _merge(o.deps, o.dma_deps, t.w_c, t.w_d)
        for t in writes:
            self._add_write(o, t, False)
        for t in pwrites:
            self._add_write(o, t, True)
        for t in reads:
            if id(t) not in wset:
                if o.is_dma:
                    t.r_d.append(o)
                else:
                    t.r_c[o.eng] = o

    def op(self, eng, emit, reads=(), writes=(), pwrites=()):
        o = Op(eng, emit)
        self._record(o, reads, writes, pwrites)
        self.ops[eng].append(o)
        self.last[eng] = o
        return o

    def dma(self, q, out, in_, reads=(), writes=(), pwrites=(), **kw):
        o = Op(q, (lambda eng: eng.dma_start(out=out, in_=in_, **kw)), is_dma=True)
        pool = self.dma_pool[q]
        i = self.dma_rr[q]
        self.dma_rr[q] = (i + 1) % len(pool)
        o.dsem = pool[i][0]
        o.prev_dval = pool[i][1]
        pool[i][1] += 16
        o.dval = pool[i][1]
        self._record(o, reads, writes, pwrites)
        self.ops[q].append(o)
        self.out_dmas.append(o)
        return o

    def barrier(self):
        lasts = {e: o for e, o in self.last.items() if o is not None}
        for e in ENGS:
            o = Op(e, None)
            for e2, o2 in lasts.items():
                if e2 != e:
                    o.deps[e2] = o2
            o.dma_deps = list(self.out_dmas)
            self.ops[e].append(o)
        self.out_dmas = []

    def finalize(self):
        for e in ENGS:
            for o in self.ops[e]:
                for e2, p in o.deps.items():
                    if p.is_dma:
                        continue
                    if e2 == e and e == "pe":
                        continue
                    p.signal = True
        for e in ENGS:
            c = 0
            for o in self.ops[e]:
                if o.is_dma or o.emit is None:
                    continue
                if o.signal:
                    c += 1
                    o.count = c
        nc = self.nc
        engmap = {"pe": "tensor", "act": "scalar", "dve": "vector", "pool": "gpsimd", "sp": "sync"}
        stats = {}
        with nc.Block() as block:
            for e in ENGS:
                def body(engine, e=e):
                    waited = {}
                    nw = 0

                    def wait(sem, val):
                        nonlocal nw
                        k = id(sem)
                        if waited.get(k, 0) >= val:
                            return
                        waited[k] = val
                        engine.wait_ge(sem, val)
                        nw += 1

                    for o in self.ops[e]:
                        for e2, p in o.deps.items():
                            if p.is_dma:
                                wait(p.dsem, p.dval)
                                continue
                            if e2 == e and e == "pe":
                                continue
                            if p is o:
                                continue
                            wait(self.sem[e2], p.count)
                        for p in o.dma_deps:
                            wait(p.dsem, p.dval)
                        if o.emit is None:
                            continue
                        if o.is_dma:
                            if o.prev_dval > 0:
                                wait(o.dsem, o.prev_dval)
                            ins = o.emit(engine)
                            ins.then_inc(o.dsem, 16)
                        else:
                            ins = o.emit(engine)
                            if o.signal:
                                ins.then_inc(self.sem[e], 1)
                    stats[e] = (len(self.ops[e]), nw)
                getattr(block, engmap[e])(body)
        return stats


class Sbuf:
    def __init__(self, big, nwords):
        self.big = big
        self.n = nwords
        self.off = 0
        self.marks = []

    def mark(self):
        self.marks.append(self.off)

    def release(self):
        self.off = self.marks.pop()

    def alloc(self, nelem, dtype=F32, parts=128, name=""):
        if dtype == BF16:
            nw = (nelem + 1) // 2
        else:
            nw = nelem
        nw = (nw + 7) // 8 * 8
        assert self.off + nw <= self.n, "SBUF overflow %s: %d + %d > %d" % (name, self.off, nw, self.n)
        ap = self.big[0:parts, self.off:self.off + nw]
        self.off += nw
        if dtype == BF16:
            ap = ap.bitcast(BF16)[:, 0:nelem]
        elif dtype != F32:
            ap = ap.bitcast(dtype)[:, 0:nelem]
        else:
            ap = ap[:, 0:nelem]
        return Tile(ap, name)

from contextlib import ExitStack
import ml_dtypes

D = 1024
NS = 2
LC = 256
S = 2048
T = LC + S
DEPTH = 2
NE = 16
DEXP = 2048
IN_W = 5632
CAPL = 256
CAPC = 32
EPS = 1e-6
KC = 8

SB_WORDS = 46080


def host_consts():
    c = {}
    half = 32
    freqs = (10000.0 ** (-np.arange(half, dtype=np.float32) / half)).astype(np.float32)
    t = np.arange(S)
    rows = (t // 64).astype(np.float32)
    cols = (t % 64).astype(np.float32)
    C = np.zeros((128, S), np.float32)
    Sn = np.zeros((128, S), np.float32)
    for d in range(128):
        pos = rows if d < 64 else cols
        j = (d % 64) % 32
        ang = (pos * freqs[j]).astype(np.float32)
        C[d] = np.cos(ang)
        Sn[d] = np.sin(ang)
    c["ropeC"] = C
    c["ropeS"] = Sn
    Pm = np.zeros((128, 128), np.float32)
    for d in range(128):
        i = d % 64
        if i < 32:
            Pm[d + 32, d] = -1.0
        else:
            Pm[d - 32, d] = 1.0
    c["pm"] = Pm
    c["ident"] = np.eye(128, dtype=np.float32)
    kk = np.arange(128)[:, None]
    qq = np.arange(128)[None, :]
    mprev = (qq <= kk).astype(np.float32)
    mnext = (kk <= qq).astype(np.float32)
    c["mprev"] = np.tile(mprev, (1, 4))
    c["mnext"] = np.tile(mnext, (1, 4))
    c["iota_f"] = np.tile(np.arange(256, dtype=np.float32)[None, :], (128, 1))
    c["iota_p"] = np.stack([np.arange(128, dtype=np.float32), np.arange(128, dtype=np.float32) + 128], axis=1)
    sel = np.zeros((32, 32, 128), np.float32)
    for r in range(32):
        sel[r, r, :] = 1.0
    c["rowsel"] = sel.transpose(1, 0, 2).reshape(32, 32 * 128)
    bd = np.zeros((32, 32), np.float32)
    bd[:16, :16] = 1.0
    bd[16:, 16:] = 1.0
    c["bdones"] = bd
    return c


CONST_SHAPES = {"ropeC": (128, S), "ropeS": (128, S), "pm": (128, 128), "ident": (128, 128),
                "mprev": (128, 512), "mnext": (128, 512), "iota_f": (128, 256), "iota_p": (128, 2),
                "rowsel": (32, 32 * 128), "bdones": (32, 32)}


class KB:
    def __init__(self, debug=None, stop_after=None):
        self.debug = debug or []
        self.stop_after = stop_after
        self.nc = bass.Bass("TRN2", target_bir_lowering=False)
        self.dr = {}

    def din(self, name, shape, dt=F32):
        self.dr[name] = self.nc.dram_tensor(name, list(shape), dt, kind="ExternalInput").ap()
        return self.dr[name]

    def dscr(self, name, shape, dt=F32, out=False):
        kind = "ExternalOutput" if (out or name in self.debug) else "Internal"
        self.dr[name] = self.nc.dram_tensor(name, list(shape), dt, kind=kind).ap()
        return self.dr[name]

    def mm(self, pst, out, lhsT, rhs, start, stop, reads):
        self.fw.op("pe", lambda e: e.matmul(out, lhsT=lhsT, rhs=rhs, start=start, stop=stop), reads=reads, pwrites=[pst])

    def tr(self, pst, out, in_, ident, reads):
        self.fw.op("pe", lambda e: e.transpose(out, in_, ident), reads=reads, pwrites=[pst])

    def act(self, out, in_, func, reads, writes=(), pwrites=(), **kw):
        self.fw.op("act", lambda e: e.activation(out=out, in_=in_, func=func, **kw), reads=reads, writes=writes, pwrites=pwrites)

    def tt(self, eng, out, in0, in1, op, reads, writes=(), pwrites=()):
        self.fw.op(eng, lambda e: e.tensor_tensor(out=out, in0=in0, in1=in1, op=op), reads=reads, writes=writes, pwrites=pwrites)

    def ts(self, eng, out, in0, s1, op0, reads, s2=None, op1=None, writes=(), pwrites=()):
        if op1 is None:
            self.fw.op(eng, lambda e: e.tensor_scalar(out=out, in0=in0, scalar1=s1, scalar2=None, op0=op0), reads=reads, writes=writes, pwrites=pwrites)
        else:
            self.fw.op(eng, lambda e: e.tensor_scalar(out=out, in0=in0, scalar1=s1, scalar2=s2, op0=op0, op1=op1), reads=reads, writes=writes, pwrites=pwrites)

    def stt(self, out, in0, scalar, in1, op0, op1, reads, writes=(), pwrites=()):
        self.fw.op("dve", lambda e: e.scalar_tensor_tensor(out=out, in0=in0, scalar=scalar, in1=in1, op0=op0, op1=op1), reads=reads, writes=writes, pwrites=pwrites)

    def cp(self, eng, out, in_, reads, writes=(), pwrites=()):
        if eng == "act":
            self.fw.op("act", lambda e: e.copy(out=out, in_=in_), reads=reads, writes=writes, pwrites=pwrites)
        else:
            self.fw.op(eng, lambda e: e.tensor_copy(out=out, in_=in_), reads=reads, writes=writes, pwrites=pwrites)

    def dma(self, q, out, in_, reads=(), writes=(), pwrites=(), maxdesc=512):
        shp = tuple(out.shape)
        if len(shp) == 3 and shp[0] * shp[1] > maxdesc and tuple(in_.shape) == shp:
            step = max(1, maxdesc // shp[0])
            first = True
            for i0 in range(0, shp[1], step):
                i1 = min(shp[1], i0 + step)
                if first:
                    self.fw.dma(q, out[:, i0:i1, :], in_[:, i0:i1, :], reads=reads, writes=writes, pwrites=pwrites)
                    first = False
                else:
                    self.fw.dma(q, out[:, i0:i1, :], in_[:, i0:i1, :], reads=reads, pwrites=tuple(writes) + tuple(pwrites))
            return
        self.fw.dma(q, out, in_, reads=reads, writes=writes, pwrites=pwrites)

    def phase_end(self):
        self.fw.barrier()

    def build(self):
        nc = self.nc
        din, dscr = self.din, self.dscr
        din("xT", (NS, D, T))
        din("cT", (128, KC, 3))
        din("ada_w", (DEPTH, D, 6 * D))
        din("ada_bT", (DEPTH, 128, 48))
        din("gmixT", (DEPTH, 128, KC))
        din("gffnT", (DEPTH, 128, KC))
        din("gfinT", (128, KC))
        din("w_in", (DEPTH, D, IN_W))
        din("sink", (DEPTH, 1, 8))
        din("convT", (DEPTH, 128, 5, KC))
        din("lru_w", (DEPTH, 2, 2, 8, 128, 128))
        din("lru_vT", (DEPTH, 128, 3, 2, KC))
        din("w_attn_br", (DEPTH, D, D))
        din("w_rec_br", (DEPTH, D, D))
        din("w_out", (DEPTH, D, D))
        din("w_router", (DEPTH, D, NE))
        if self.stop_after not in ("p0", "p1", "p2", "p3", "p4", "p5", "wip"):
            din("w_gate", (DEPTH, NE, D, DEXP))
            din("w_up", (DEPTH, NE, D, DEXP))
            din("w_down", (DEPTH, NE, DEXP, D))
        for k, shp in CONST_SHAPES.items():
            din("c_" + k, shp)
        dscr("outT", (NS, D, S), out=True)
        dscr("X1", (NS, D, T)); dscr("X2", (NS, D, T)); dscr("X3", (NS, D, T))
        dscr("qT", (NS, D, T), BF16); dscr("kT", (NS, 256, T), BF16); dscr("vtm", (NS, T, 256), BF16)
        dscr("uT", (NS, D, T)); dscr("gzT", (NS, D, T), BF16); dscr("smaT", (NS, D, T), BF16); dscr("smrT", (NS, D, T), BF16)
        dscr("attT", (NS, D, T), BF16); dscr("rgT", (NS, D, T), BF16)
        dscr("h2tm", (NS, T, D), BF16); dscr("lgT", (NS, NE, T))
        dscr("ye", (NS, NE, CAPL, D), BF16); dscr("yec", (NS, NE, CAPC, D), BF16); dscr("xe", (NE, D, 512 + 2 * CAPC), BF16)
        dscr("modc", (DEPTH, 128, 6, KC, 3))
        dscr("rt", (2, 32, 3, S))

        with ExitStack() as es:
            big = es.enter_context(nc.sbuf_tensor("big", [128, SB_WORDS], F32))
            pst = [es.enter_context(nc.psum_tensor("ps%d" % i, [128, 512], F32)) for i in range(8)]
            self.fw = FW(nc)
            self.fw.setup_sems(es)
            self.sb = Sbuf(big, SB_WORDS)
            self.ps = [Tile(p, "ps%d" % i) for i, p in enumerate(pst)]
            self._body()
            self.fw.barrier()
            self.stats = self.fw.finalize()
        return nc

    def _body(self):
        sb = self.sb
        dr = self.dr
        fw = self.fw
        self.ident = sb.alloc(128, F32, name="ident")
        self.dma("sp", self.ident[:, :], dr["c_ident"][:, :], writes=[self.ident])
        self.identb = sb.alloc(128, BF16, name="identb")
        self.dma("pool", self.identb[:, :], dr["c_ident"][:, :], writes=[self.identb])
        self.pmb = sb.alloc(128, BF16, name="pmb")
        self.dma("pool", self.pmb[:, :], dr["c_pm"][:, :], writes=[self.pmb])
        self.onesb = sb.alloc(128, BF16, name="onesb")
        fw.op("dve", lambda e: e.memset(self.onesb[:, :], 1.0), writes=[self.onesb])
        self.epsc = sb.alloc(1, F32, name="epsc")
        fw.op("dve", lambda e: e.memset(self.epsc[:, :], EPS), writes=[self.epsc])
        self.silu_c = sb.alloc(KC * 3, F32, name="silu_c")
        ctmp = sb.alloc(KC * 3, F32, name="ctmp")
        self.dma("sp", ctmp[:, :], dr["cT"].rearrange("p c e -> p (c e)"), writes=[ctmp])
        self.act(self.silu_c[:, :], ctmp[:, :], AF.Silu, reads=[ctmp], writes=[self.silu_c])
        self.modc = [sb.alloc(6 * KC * 3, F32, name="modc%d" % l) for l in range(DEPTH)]
        self.A1 = [sb.alloc(KC * 3, F32) for l in range(DEPTH)]
        self.A2 = [sb.alloc(KC * 3, F32) for l in range(DEPTH)]
        self.posT_lat = sb.alloc(16 * 32, F32, name="posT_lat")
        self.posT_ctx = sb.alloc(2 * 32, F32, name="posT_ctx")
        self.base_off = sb.off
        Xs = ["xT", "X1", "X2", "X3"]
        for l in range(DEPTH):
            last = l == DEPTH - 1
            self.p0_mod(l)
            if self.stop_after == "p0":
                return
            self.p1_inproj(l, dr["xT"] if l == 0 else dr["X2"])
            if self.stop_after == "p1":
                return
            self.p2_attn(l, last)
            if self.stop_after == "p2":
                return
            self.p3_rec(l)
            if self.stop_after == "p3":
                return
            if self.stop_after == "wip_DISABLED":
                self.pF_copy()
                return
            self.p4_merge(l, dr["xT"] if l == 0 else dr["X2"], dr["X1"], last)
            if self.stop_after == "p4":
                return
            self.p5_route(l, last)
            if self.stop_after == "p5":
                return
            self.p6_experts(l, last)
            if self.stop_after == "p6":
                return
            self.p7_scatter(l, dr["X1"], dr["X2"] if not last else None, last)
            if self.stop_after == "p7":
                return

    def p0_mod(self, l):
        sb, dr, fw, ps = self.sb, self.dr, self.fw, self.ps
        sb.mark()
        NB = 12
        wbuf = [sb.alloc(KC * 512, F32, name="adaw%d" % i) for i in range(2)]
        adab = sb.alloc(48, F32)
        gm = sb.alloc(KC, F32)
        gf = sb.alloc(KC, F32)
        self.dma("sp", adab[:, :], dr["ada_bT"][l], writes=[adab])
        self.dma("sp", gm[:, :], dr["gmixT"][l], writes=[gm])
        self.dma("sp", gf[:, :], dr["gffnT"][l], writes=[gf])
        pt = ps[0]
        modc = self.modc[l]
        for nb in range(NB):
            wb = wbuf[nb % 2]
            self.dma("sp" if nb % 2 == 0 else "act", wb[:, :].rearrange("p (c n) -> p c n", c=KC),
                   dr["ada_w"][l, :, nb * 512:(nb + 1) * 512].rearrange("(c p) n -> p c n", p=128), writes=[wb])
            for jj in range(4):
                j = nb * 4 + jj
                for k in range(KC):
                    self.mm(pt, pt[:, j * 3:(j + 1) * 3], wb[:, k * 512 + jj * 128:k * 512 + (jj + 1) * 128],
                            self.silu_c[:, k * 3:(k + 1) * 3], k == 0, k == KC - 1, reads=[wb, self.silu_c])
        for e3 in range(3):
            self.tt("dve", modc[:, :].rearrange("p (j e) -> p j e", e=3)[:, :, e3],
                    pt[:, 0:144].rearrange("p (j e) -> p j e", e=3)[:, :, e3], adab[:, :], ALU.add,
                    reads=[pt, adab], pwrites=[modc])
        m4 = modc[:, :].rearrange("p (w c e) -> p w c e", w=6, c=KC)
        a1 = self.A1[l][:, :].rearrange("p (c e) -> p c e", e=3)
        a2 = self.A2[l][:, :].rearrange("p (c e) -> p c e", e=3)
        for e3 in range(3):
            self.stt(a1[:, :, e3], m4[:, 1, :, e3], 1.0, gm[:, :], ALU.add, ALU.mult, reads=[modc, gm], pwrites=[self.A1[l]])
            self.stt(a2[:, :, e3], m4[:, 4, :, e3], 1.0, gf[:, :], ALU.add, ALU.mult, reads=[modc, gf], pwrites=[self.A2[l]])
        if "modc" in self.debug:
            self.dma("sp", dr["modc"][l].rearrange("p w c e -> p (w c e)"), modc[:, :], reads=[modc])
        self.phase_end()
        sb.release()

    def mcol(self, l, which, c, e):
        i = (which * KC + c) * 3 + e
        return self.modc[l][:, i:i + 1]

    def rstd_from(self, xt_tiles, w, sqb, pt, rstd, lnv):
        for c in range(KC):
            self.act(sqb[c][:, :w], xt_tiles[c][:, :w], AF.Square, reads=[xt_tiles[c]], writes=[sqb[c]])
        for c in range(KC):
            self.mm(pt, pt[:, :w], self.onesb[:, :], sqb[c][:, :w], c == 0, c == KC - 1, reads=[self.onesb, sqb[c]])
        self.act(lnv[:, :w], pt[:, :w], AF.Ln, reads=[pt], writes=[lnv], scale=1.0 / D, bias=self.epsc[:, 0:1])
        self.act(rstd[:, :w], lnv[:, :w], AF.Exp, reads=[lnv], writes=[rstd], scale=-0.5)

    def p1_inproj(self, l, Xin):
        sb, dr, fw, ps = self.sb, self.dr, self.fw, self.ps
        sb.mark()
        W = 256
        win = sb.alloc(KC * IN_W, BF16, name="win")
        winv = win[:, :].rearrange("p (c n) -> p c n", c=KC)
        for nb in range(11):
            self.dma("pool", winv[:, :, nb * 512:(nb + 1) * 512],
                   dr["w_in"][l, :, nb * 512:(nb + 1) * 512].rearrange("(c p) n -> p c n", p=128), pwrites=[win])
        ropeC = sb.alloc(S, F32); ropeS = sb.alloc(S, F32); pmb = self.pmb
        self.dma("sp", ropeC[:, :], dr["c_ropeC"][:, :], writes=[ropeC])
        self.dma("sp", ropeS[:, :], dr["c_ropeS"][:, :], writes=[ropeS])
        xt = [[sb.alloc(W, F32) for c in range(KC)] for i in range(2)]
        sqb = [sb.alloc(W, BF16) for c in range(KC)]
        hT = [sb.alloc(W, BF16) for c in range(KC)]
        tmpf = [sb.alloc(W, F32) for i in range(2)]
        rstd = sb.alloc(W, F32); lnv = sb.alloc(W, F32)
        qraw = [sb.alloc(W, BF16) for i in range(2)]
        t1 = [sb.alloc(W, F32) for i in range(2)]
        t2 = [sb.alloc(W, F32) for i in range(2)]
        tmpe = [sb.alloc(W, F32) for i in range(2)]
        tmpe2 = [sb.alloc(W, F32) for i in range(2)]
        st_qk = [sb.alloc(W, BF16) for c in range(10)]
        st_u = [sb.alloc(W, F32) for c in range(KC)]
        st_g = [[sb.alloc(W, BF16) for c in range(KC)] for i in range(3)]
        st_v = [sb.alloc(256, BF16) for i in range(2)]
        ntile = T // W
        it = 0
        import os
        lim = int(os.environ.get("K_P1_TILES", "999"))
        for s in range(NS):
            for ti in range(ntile):
                if it >= lim:
                    continue
                if it < int(os.environ.get("K_P1_SKIP", "0")):
                    it += 1
                    continue
                t0 = ti * W
                is_ctx = ti == 0
                e3 = 2 if is_ctx else s
                xb = xt[it % 2]
                for c in range(KC):
                    self.dma("sp", xb[c][:, :], Xin[s, c * 128:(c + 1) * 128, t0:t0 + W], writes=[xb[c]])
                self.rstd_from(xb, W, sqb, ps[0], rstd, lnv)
                for c in range(KC):
                    tf = tmpf[c % 2]
                    self.stt(tf[:, :], xb[c][:, :], self.A1[l][:, c * 3 + e3:c * 3 + e3 + 1], rstd[:, :], ALU.mult, ALU.mult,
                             reads=[xb[c], self.A1[l], rstd], writes=[tf])
                    self.act(hT[c][:, :], tf[:, :], AF.Identity, reads=[tf, self.modc[l]], writes=[hT[c]],
                             bias=self.mcol(l, 0, c, e3), scale=1.0)
                for oc in range(44):
                    if 10 <= oc < 12:
                        continue
                    pt = ps[1 + (oc % 4)]
                    for k in range(KC):
                        self.mm(pt, pt[:, :W], winv[:, k, oc * 128:(oc + 1) * 128], hT[k][:, :], k == 0, k == KC - 1, reads=[win, hT[k]])
                    if oc < 10:
                        dst = st_qk[oc]
                        if is_ctx or os.environ.get("K_NOROPE"):
                            self.cp("act", dst[:, :], pt[:, :W], reads=[pt], writes=[dst])
                        else:
                            qr_ = qraw[oc % 2]; a1 = t1[oc % 2]; a2 = t2[oc % 2]
                            p2 = ps[5 + (oc % 2)]
                            lt0 = t0 - LC
                            self.cp("act", qr_[:, :], pt[:, :W], reads=[pt], writes=[qr_])
                            self.mm(p2, p2[:, :W], (self.identb if os.environ.get("K_ID") else pmb)[:, :], qr_[:, :], True, True, reads=[self.identb if os.environ.get("K_ID") else pmb, qr_])
                            rv = int(os.environ.get("K_ROPEV", "9"))
                            if rv == 1:
                                self.cp("act", dst[:, :], p2[:, :W], reads=[p2], writes=[dst])
                            elif rv == 2:
                                self.tt("dve", a1[:, :], pt[:, :W], ropeC[:, lt0:lt0 + W], ALU.mult, reads=[pt, ropeC], writes=[a1])
                                self.cp("act", dst[:, :], p2[:, :W], reads=[p2], writes=[dst])
                            elif rv == 3:
                                self.tt("dve", a1[:, :], pt[:, :W], ropeC[:, lt0:lt0 + W], ALU.mult, reads=[pt, ropeC], writes=[a1])
                                self.tt("dve", a2[:, :], p2[:, :W], ropeS[:, lt0:lt0 + W], ALU.mult, reads=[p2, ropeS], writes=[a2])
                                self.cp("act", dst[:, :], p2[:, :W], reads=[p2], writes=[dst])
                            else:
                                e1_ = tmpe[oc % 2]; e2_ = tmpe2[oc % 2]
                                self.cp("act", e1_[:, :], pt[:, :W], reads=[pt], writes=[e1_])
                                self.cp("act", e2_[:, :], p2[:, :W], reads=[p2], writes=[e2_])
                                self.tt("dve", a1[:, :], e1_[:, :], ropeC[:, lt0:lt0 + W], ALU.mult, reads=[e1_, ropeC], writes=[a1])
                                self.tt("dve", a2[:, :], e2_[:, :], ropeS[:, lt0:lt0 + W], ALU.mult, reads=[e2_, ropeS], writes=[a2])
                                self.tt("dve", dst[:, :], a1[:, :], a2[:, :], ALU.add, reads=[a1, a2], writes=[dst])
                    elif oc < 20:
                        dst = st_u[oc - 12]
                        self.cp("act", dst[:, :], pt[:, :W], reads=[pt], writes=[dst])
                    else:
                        g = (oc - 20) // 8
                        dst = st_g[g][(oc - 20) % 8]
                        self.act(dst[:, :], pt[:, :W], AF.Gelu if g == 0 else AF.Sigmoid, reads=[pt], writes=[dst])
                for j in range(W // 128):
                    if os.environ.get("K_NOV"):
                        continue
                    pt = ps[7]
                    for k in range(KC):
                        self.mm(pt, pt[:, :256], hT[k][:, j * 128:(j + 1) * 128], winv[:, k, 1280:1536], k == 0, k == KC - 1, reads=[win, hT[k]])
                    sv = st_v[j % 2]
                    self.cp("dve", sv[:, :], pt[:, :256], reads=[pt], writes=[sv])
                    self.dma("act", dr["vtm"][s, t0 + j * 128:t0 + (j + 1) * 128, :], sv[:, :], reads=[sv])
                for c in range(KC):
                    self.dma("act", dr["qT"][s, c * 128:(c + 1) * 128, t0:t0 + W], st_qk[c][:, :], reads=[st_qk[c]])
                for c in range(2):
                    self.dma("act", dr["kT"][s, c * 128:(c + 1) * 128, t0:t0 + W], st_qk[8 + c][:, :], reads=[st_qk[8 + c]])
                for c in range(KC):
                    msk = int(os.environ.get("K_ST", "15"))
                    if msk & 1:
                        self.dma("sp", dr["uT"][s, c * 128:(c + 1) * 128, t0:t0 + W], st_u[c][:, :], reads=[st_u[c]])
                    if msk & 2:
                        self.dma("sp", dr["gzT"][s, c * 128:(c + 1) * 128, t0:t0 + W], st_g[0][c][:, :], reads=[st_g[0][c]])
                    if msk & 4:
                        self.dma("act", dr["smaT"][s, c * 128:(c + 1) * 128, t0:t0 + W], st_g[1][c][:, :], reads=[st_g[1][c]])
                    if msk & 8:
                        self.dma("act", dr["smrT"][s, c * 128:(c + 1) * 128, t0:t0 + W], st_g[2][c][:, :], reads=[st_g[2][c]])
                it += 1
        self.phase_end()
        sb.release()

    def p2_attn(self, l, last):
        sb, dr, fw, ps = self.sb, self.dr, self.fw, self.ps
        sb.mark()
        mprev = sb.alloc(512, BF16); mnext = sb.alloc(512, BF16)
        self.dma("pool", mprev[:, :], dr["c_mprev"][:, :], writes=[mprev])
        self.dma("pool", mnext[:, :], dr["c_mnext"][:, :], writes=[mnext])
        sk = sb.alloc(8, F32); ske = sb.alloc(8, F32); onef = sb.alloc(128, F32)
        esf = sb.alloc(1024, F32); eshi = sb.alloc(1024, BF16); eslo = sb.alloc(1024, BF16)
        self.dma("sp", sk[0:1, :], dr["sink"][l], writes=[sk])
        self.act(ske[0:1, :], sk[0:1, :], AF.Exp, reads=[sk], writes=[ske])
        fw.op("dve", lambda e: e.memset(onef[:, :], 1.0), writes=[onef])
        for h in range(8):
            self.ts("dve", esf[0:1, h * 128:(h + 1) * 128], onef[0:1, :], ske[0:1, h:h + 1], ALU.mult, reads=[onef, ske], pwrites=[esf])
        self.cp("dve", eshi[0:1, :], esf[0:1, :], reads=[esf], writes=[eshi])
        self.tt("dve", eslo[0:1, :], esf[0:1, :], eshi[0:1, :], ALU.subtract, reads=[esf, eshi], writes=[eslo])
        qall = sb.alloc(8 * T, BF16, name="qall")
        qv = qall[:, :].rearrange("p (h t) -> p h t", h=8)
        kt = [sb.alloc(T, BF16) for i in range(2)]
        vt = sb.alloc(18 * 256, BF16)
        Er = [sb.alloc(512, BF16) for i in range(5)]
        lnD = sb.alloc(512, F32); rD = sb.alloc(512, F32)
        ost = [sb.alloc(4 * 512, BF16) for i in range(2)]
        scale = 1.0 / np.sqrt(128.0)
        for s in range(NS):
            for h in range(8):
                self.dma("sp", qv[:, h, :], dr["qT"][s, h * 128:(h + 1) * 128, :], pwrites=[qall])
            for c in range(2):
                self.dma("sp", kt[c][:, :], dr["kT"][s, c * 128:(c + 1) * 128, :], writes=[kt[c]])
            self.dma("sp", vt[:, :].rearrange("p (b f) -> p b f", f=256), dr["vtm"][s].rearrange("(b p) f -> p b f", p=128), writes=[vt])
            qblocks = [("lat", i) for i in range(16)]
            if not last:
                qblocks = [("ctx", 0), ("ctx", 1)] + qblocks
            for kind, i in qblocks:
                if kind == "lat":
                    tq = LC + i * 128
                    keys = [(0, 0, None), (128, 1, None)]
                    if i > 0:
                        keys.append((LC + (i - 1) * 128, 2 + i - 1, mprev))
                    keys.append((LC + i * 128, 2 + i, None))
                    if i < 15:
                        keys.append((LC + (i + 1) * 128, 2 + i + 1, mnext))
                    sw, si = 512, i % 4
                else:
                    tq = i * 128
                    keys = [(0, 0, None), (128, 1, None)]
                    sw, si = 256, i
                for kv in range(2):
                    for idx, (kc0, vb, mk) in enumerate(keys):
                        pS = ps[idx % 3]
                        for g in range(4):
                            self.mm(pS, pS[:, g * 128:(g + 1) * 128], kt[kv][:, kc0:kc0 + 128], qv[:, 4 * kv + g, tq:tq + 128], True, True, reads=[kt[kv], qall])
                        E = Er[idx]
                        self.act(E[:, :], pS[:, :], AF.Exp, reads=[pS], writes=[E], scale=float(scale))
                        if mk is not None:
                            self.tt("dve", E[:, :], E[:, :], mk[:, :], ALU.mult, reads=[E, mk], writes=[E])
                    pO, pD = ps[3 + (kv % 2) * 2], ps[4 + (kv % 2) * 2]
                    n = len(keys)
                    for idx, (kc0, vb, mk) in enumerate(keys):
                        self.mm(pO, pO[:, :], vt[:, vb * 256 + kv * 128:vb * 256 + (kv + 1) * 128], Er[idx][:, :], idx == 0, idx == n - 1, reads=[vt, Er[idx]])
                    for idx, (kc0, vb, mk) in enumerate(keys):
                        self.mm(pD, pD[:, :], self.onesb[:, :], Er[idx][:, :], idx == 0, False, reads=[self.onesb, Er[idx]])
                    self.mm(pD, pD[:, :], self.onesb[0:1, :], eshi[0:1, kv * 512:(kv + 1) * 512], False, False, reads=[self.onesb, eshi])
                    self.mm(pD, pD[:, :], self.onesb[0:1, :], eslo[0:1, kv * 512:(kv + 1) * 512], False, True, reads=[self.onesb, eslo])
                    self.act(lnD[:, :], pD[:, :], AF.Ln, reads=[pD], writes=[lnD])
                    self.act(rD[:, :], lnD[:, :], AF.Exp, reads=[lnD], writes=[rD], scale=-1.0)
                    ov = ost[kv][:, :].rearrange("p (g t) -> p g t", g=4)
                    self.tt("dve", ov[:, :, si * 128:(si + 1) * 128], pO[:, :].rearrange("p (g t) -> p g t", g=4),
                            rD[:, :].rearrange("p (g t) -> p g t", g=4), ALU.mult, reads=[pO, rD], pwrites=[ost[kv]])
                if (kind == "lat" and i % 4 == 3) or (kind == "ctx" and i == 1):
                    tq0 = tq + 128 - sw
                    for kv in range(2):
                        ov = ost[kv][:, :].rearrange("p (g t) -> p g t", g=4)
                        self.dma("pool", dr["attT"][s, kv * 512:(kv + 1) * 512, tq0:tq0 + sw].rearrange("(g d) t -> d g t", d=128),
                               ov[:, :, 0:sw], reads=[ost[kv]])
        self.phase_end()
        sb.release()

    def p3_rec(self, l):
        sb, dr, fw, ps = self.sb, self.dr, self.fw, self.ps
        sb.mark()
        lw = sb.alloc(2 * 2 * 8 * 128, BF16, name="lw")
        lwv = lw[:, :].rearrange("p (w d b j) -> p w d b j", w=2, d=2, b=8)
        for w_ in range(2):
            for d_ in range(2):
                self.dma("pool", lwv[:, w_, d_, :, :], dr["lru_w"][l, w_, d_].rearrange("b i j -> i b j"), pwrites=[lw])
        lv = sb.alloc(48, F32); cv = sb.alloc(40, F32)
        self.dma("sp", lv[:, :], dr["lru_vT"][l].rearrange("p a d c -> p (a d c)"), writes=[lv])
        self.dma("sp", cv[:, :], dr["convT"][l].rearrange("p a c -> p (a c)"), writes=[cv])
        onec = sb.alloc(1, F32)
        fw.op("dve", lambda e: e.memset(onec[:, :], 1.0), writes=[onec])
        e1 = sb.alloc(16, F32); l1 = sb.alloc(16, F32); cl = sb.alloc(16, F32)
        self.act(e1[:, :], lv[:, 32:48], AF.Exp, reads=[lv], writes=[e1], scale=-1.0)
        self.act(l1[:, :], e1[:, :], AF.Ln, reads=[e1, onec], writes=[l1], bias=onec[:, 0:1], scale=1.0)
        self.ts("dve", cl[:, :], l1[:, :], -8.0, ALU.mult, reads=[l1], writes=[cl])
        bu = [dict(u=sb.alloc(T, F32), uc=sb.alloc(T, F32), ucb=sb.alloc(T, BF16), gz=sb.alloc(T, BF16)) for i in range(2)]
        bd = [dict(r=sb.alloc(T, F32), gi=sb.alloc(T, F32), a=sb.alloc(T, F32)) for d_ in range(2)]
        hf = sb.alloc(T, F32); hb = sb.alloc(T, F32); og = sb.alloc(T, BF16)
        tiles = [(0, 256)] + [(256 + 512 * i, 512) for i in range(4)]
        items = [(s, c) for s in range(NS) for c in range(KC)]

        def conv_act(i):
            s, c = items[i]
            B = bu[i % 2]
            u, uc, gz = B["u"], B["uc"], B["gz"]
            self.dma("sp", u[:, :], dr["uT"][s, c * 128:(c + 1) * 128, :], writes=[u])
            self.dma("sp", gz[:, :], dr["gzT"][s, c * 128:(c + 1) * 128, :], writes=[gz])
            self.act(uc[:, :], u[:, :], AF.Identity, reads=[u, cv], writes=[uc], scale=cv[:, 2 * KC + c:2 * KC + c + 1], bias=cv[:, 4 * KC + c:4 * KC + c + 1])

        def conv_dve(i):
            s, c = items[i]
            B = bu[i % 2]
            u, uc, ucb = B["u"], B["uc"], B["ucb"]
            for k, d in ((0, -2), (1, -1), (3, 1)):
                for (sa, sb_) in ((0, LC), (LC, T)):
                    lo = max(sa, sa - d); hi = min(sb_, sb_ - d)
                    self.stt(uc[:, lo:hi], u[:, lo + d:hi + d], cv[:, k * KC + c:k * KC + c + 1], uc[:, lo:hi], ALU.mult, ALU.add,
                             reads=[u, cv, uc], pwrites=[uc])
            self.cp("dve", ucb[:, :], uc[:, :], reads=[uc], writes=[ucb])

        def gates(i, d_):
            s, c = items[i]
            ucb = bu[i % 2]["ucb"]
            r, gi, a = bd[d_]["r"], bd[d_]["gi"], bd[d_]["a"]
            for ti, (t0, w) in enumerate(tiles):
                pA = ps[(2 * ti) % 8]; pX = ps[(2 * ti + 1) % 8]
                self.mm(pA, pA[:, :w], lwv[:, 0, d_, c, :], ucb[:, t0:t0 + w], True, True, reads=[lw, ucb])
                self.mm(pX, pX[:, :w], lwv[:, 1, d_, c, :], ucb[:, t0:t0 + w], True, True, reads=[lw, ucb])
                self.act(r[:, t0:t0 + w], pA[:, :w], AF.Sigmoid, reads=[pA, lv], pwrites=[r], bias=lv[:, (0 * 2 + d_) * KC + c:(0 * 2 + d_) * KC + c + 1], scale=1.0)
                self.act(gi[:, t0:t0 + w], pX[:, :w], AF.Sigmoid, reads=[pX, lv], pwrites=[gi], bias=lv[:, (1 * 2 + d_) * KC + c:(1 * 2 + d_) * KC + c + 1], scale=1.0)
            self.act(a[:, :], r[:, :], AF.Exp, reads=[r, cl], writes=[a], scale=cl[:, d_ * KC + c:d_ * KC + c + 1])
            self.act(r[:, :], a[:, :], AF.Square, reads=[a], writes=[r])
            self.act(r[:, :], r[:, :], AF.Sqrt, reads=[r, onec], writes=[r], scale=-1.0, bias=onec[:, 0:1])

        def scan_dve(i, d_):
            uc = bu[i % 2]["uc"]
            q_, gi, a = bd[d_]["r"], bd[d_]["gi"], bd[d_]["a"]
            self.tt("dve", gi[:, :], gi[:, :], uc[:, :], ALU.mult, reads=[gi, uc], writes=[gi])
            self.tt("dve", gi[:, :], gi[:, :], q_[:, :], ALU.mult, reads=[gi, q_], writes=[gi])
            if d_ == 0:
                fw.op("dve", lambda e, a=a, gi=gi: e.tensor_tensor_scan(out=hf[:, :], data0=a[:, :], data1=gi[:, :], initial=0.0, op0=ALU.mult, op1=ALU.add),
                      reads=[a, gi], writes=[hf])
            else:
                fw.op("dve", lambda e, a=a, gi=gi: e.tensor_tensor_scan(out=hb[:, LC - 1::-1], data0=a[:, LC - 1::-1], data1=gi[:, LC - 1::-1], initial=0.0, op0=ALU.mult, op1=ALU.add),
                      reads=[a, gi], writes=[hb])
                fw.op("dve", lambda e, a=a, gi=gi: e.tensor_tensor_scan(out=hb[:, T - 1:LC - 1:-1], data0=a[:, T - 1:LC - 1:-1], data1=gi[:, T - 1:LC - 1:-1], initial=hb[:, 0:1], op0=ALU.mult, op1=ALU.add),
                      reads=[a, gi, hb], pwrites=[hb])

        def tail(i):
            s, c = items[i]
            gz = bu[i % 2]["gz"]
            self.tt("dve", hf[:, :], hf[:, :], hb[:, :], ALU.add, reads=[hf, hb], writes=[hf])
            self.tt("dve", og[:, :], hf[:, :], gz[:, :], ALU.mult, reads=[hf, gz], writes=[og])
            self.dma("pool", dr["rgT"][s, c * 128:(c + 1) * 128, :], og[:, :], reads=[og])

        n_it = len(items)
        conv_act(0)
        conv_dve(0)
        for i in range(n_it):
            if i + 1 < n_it:
                conv_act(i + 1)
            gates(i, 0)
            if i + 1 < n_it:
                conv_dve(i + 1)
            gates(i, 1)
            scan_dve(i, 0)
            scan_dve(i, 1)
            tail(i)
        self.phase_end()
        sb.release()

    def pF_copy(self):
        sb, dr, fw = self.sb, self.dr, self.fw
        sb.mark()
        buf = [sb.alloc(S, F32) for i in range(2)]
        i = 0
        for s in range(NS):
            for c in range(KC):
                b = buf[i % 2]
                self.dma("sp", b[:, :], dr["xT"][s, c * 128:(c + 1) * 128, LC:T], writes=[b])
                self.dma("sp", dr["outT"][s, c * 128:(c + 1) * 128, :], b[:, :], reads=[b])
                i += 1
        self.phase_end()
        sb.release()

    def p4_merge(self, l, Xin, Xout, last):
        sb, dr, fw, ps = self.sb, self.dr, self.fw, self.ps
        sb.mark()
        W = 512
        wts = []
        for name in ("w_attn_br", "w_rec_br", "w_out"):
            wt = sb.alloc(KC * D, BF16, name=name)
            wv = wt[:, :].rearrange("p (c n) -> p c n", c=KC)
            for hh in range(2):
                self.dma("pool", wv[:, :, hh * 512:(hh + 1) * 512], dr[name][l, :, hh * 512:(hh + 1) * 512].rearrange("(c p) n -> p c n", p=128), pwrites=[wt])
            wts.append((wt, wv))
        (wa, wav), (wr, wrv), (wo, wov) = wts
        wrt = sb.alloc(KC * NE, F32)
        wrtv = wrt[:, :].rearrange("p (c e) -> p c e", c=KC)
        self.dma("sp", wrtv, dr["w_router"][l].rearrange("(c p) e -> p c e", p=128), writes=[wrt])
        att = sb.alloc(KC * W, BF16); rg = sb.alloc(KC * W, BF16); sma = sb.alloc(KC * W, BF16); smr = sb.alloc(KC * W, BF16)
        xin = sb.alloc(KC * W, F32)
        mg = [sb.alloc(W, BF16) for c in range(KC)]
        xn = [sb.alloc(W, F32) for c in range(KC)]
        sqb = [sb.alloc(W, BF16) for c in range(KC)]
        h2f = [sb.alloc(W, F32) for c in range(KC)]
        h2b = [sb.alloc(W, BF16) for c in range(KC)]
        tm1 = [sb.alloc(W, F32) for i in range(2)]; tm2 = [sb.alloc(W, F32) for i in range(2)]
        rstd = sb.alloc(W, F32); lnv = sb.alloc(W, F32)
        lgs = sb.alloc(W, F32)
        stg = [sb.alloc(D, BF16) for i in range(2)]
        v3 = lambda t: t[:, :].rearrange("p (c w) -> p c w", c=KC)
        tiles4 = [(0, 256)] + [(LC + 512 * i, 512) for i in range(4)]
        for s in range(NS):
            for ti, (t0, w) in enumerate(tiles4):
                if last and ti == 0:
                    continue
                e3 = 2 if ti == 0 else s
                for (tl, nm) in ((att, "attT"), (rg, "rgT"), (sma, "smaT"), (smr, "smrT")):
                    self.dma("sp", v3(tl)[:, :, :w], dr[nm][s, :, t0:t0 + w].rearrange("(c p) w -> p c w", p=128), writes=[tl])
                self.dma("sp", v3(xin)[:, :, :w], Xin[s, :, t0:t0 + w].rearrange("(c p) w -> p c w", p=128), writes=[xin])
                for m in range(KC):
                    pA = ps[(2 * m) % 4]; pR = ps[(2 * m + 1) % 4]
                    for k in range(KC):
                        self.mm(pA, pA[:, :w], wav[:, k, m * 128:(m + 1) * 128], v3(att)[:, k, :w], k == 0, k == KC - 1, reads=[wa, att])
                    for k in range(KC):
                        self.mm(pR, pR[:, :w], wrv[:, k, m * 128:(m + 1) * 128], v3(rg)[:, k, :w], k == 0, k == KC - 1, reads=[wr, rg])
                    a1 = tm1[m % 2]; a2 = tm2[m % 2]
                    self.tt("dve", a1[:, :w], pA[:, :w], v3(sma)[:, m, :w], ALU.mult, reads=[pA, sma], writes=[a1])
                    self.tt("dve", a2[:, :w], pR[:, :w], v3(smr)[:, m, :w], ALU.mult, reads=[pR, smr], writes=[a2])
                    self.tt("dve", mg[m][:, :w], a1[:, :w], a2[:, :w], ALU.add, reads=[a1, a2], writes=[mg[m]])
                for m in range(KC):
                    pD = ps[4 + (m % 2)]
                    for k in range(KC):
                        self.mm(pD, pD[:, :w], wov[:, k, m * 128:(m + 1) * 128], mg[k][:, :w], k == 0, k == KC - 1, reads=[wo, mg[k]])
                    self.stt(xn[m][:, :w], pD[:, :w], self.mcol(l, 2, m, e3), v3(xin)[:, m, :w], ALU.mult, ALU.add, reads=[pD, self.modc[l], xin], writes=[xn[m]])
                    self.dma("sp", Xout[s, m * 128:(m + 1) * 128, t0:t0 + w], xn[m][:, :w], reads=[xn[m]])
                self.rstd_from(xn, w, sqb, ps[6], rstd, lnv)
                for m in range(KC):
                    tf = tm1[m % 2]
                    self.stt(tf[:, :w], xn[m][:, :w], self.A2[l][:, m * 3 + e3:m * 3 + e3 + 1], rstd[:, :w], ALU.mult, ALU.mult, reads=[xn[m], self.A2[l], rstd], writes=[tf])
                    self.act(h2f[m][:, :w], tf[:, :w], AF.Identity, reads=[tf, self.modc[l]], writes=[h2f[m]], bias=self.mcol(l, 3, m, e3), scale=1.0)
                    self.cp("act", h2b[m][:, :w], h2f[m][:, :w], reads=[h2f[m]], writes=[h2b[m]])
                pL = ps[7]
                for k in range(KC):
                    self.mm(pL, pL[0:NE, :w], wrtv[:, k, :], h2f[k][:, :w], k == 0, k == KC - 1, reads=[wrt, h2f[k]])
                self.cp("act", lgs[0:NE, :w], pL[0:NE, :w], reads=[pL], writes=[lgs])
                self.dma("sp", dr["lgT"][s, :, t0:t0 + w], lgs[0:NE, :w], reads=[lgs])
                for j in range(w // 128):
                    pT = ps[2 + j % 2]
                    pTb = pT[:, :].bitcast(BF16)
                    for m in range(KC):
                        self.tr(pT, pTb[:, m * 128:(m + 1) * 128], h2b[m][:, j * 128:(j + 1) * 128], self.identb[:, :], reads=[h2b[m], self.identb])
                    sg = stg[j % 2]
                    self.cp("act", sg[:, :], pTb[:, 0:D], reads=[pT], writes=[sg])
                    self.dma("pool", dr["h2tm"][s, t0 + j * 128:t0 + (j + 1) * 128, :], sg[:, :], reads=[sg])
        self.phase_end()
        sb.release()

    def p5_route(self, l, last):
        sb, dr, fw, ps = self.sb, self.dr, self.fw, self.ps
        sb.mark()
        bd = sb.alloc(32, F32)
        self.dma("sp", bd[0:32, :], dr["c_bdones"][:, :], writes=[bd])
        lg = sb.alloc(S, F32); E = sb.alloc(S, F32); rs = sb.alloc(S, F32); aff = sb.alloc(S, F32); work = sb.alloc(S, F32)
        m8 = sb.alloc(8, F32); mask = sb.alloc(S, F32); pin = sb.alloc(S, F32); posm = sb.alloc(S, F32); onesf = sb.alloc(S, F32)
        fw.op("dve", lambda e: e.memset(onesf[:, :], 1.0), writes=[onesf])
        groups = [(0, LC, S, CAPL, self.posT_lat)]
        if not last:
            groups.append((1, 0, LC, CAPC, self.posT_ctx))
        P = 32
        for gi, a0, n, cap, posT in groups:
            self.dma("sp", lg[0:P, :n], dr["lgT"][:, :, a0:a0 + n].rearrange("s e t -> (s e) t"), writes=[lg])
            self.act(E[0:P, :n], lg[0:P, :n], AF.Exp, reads=[lg], writes=[E])
            for c0 in range(0, n, 512):
                w = min(512, n - c0)
                pS = ps[(c0 // 512) % 2]
                self.mm(pS, pS[0:P, :w], bd[0:P, 0:P], E[0:P, c0:c0 + w], True, True, reads=[bd, E])
                fw.op("dve", lambda e, c0=c0, w=w, pS=pS: e.reciprocal(out=rs[0:P, c0:c0 + w], in_=pS[0:P, :w]), reads=[pS], pwrites=[rs])
            self.tt("dve", aff[0:P, :n], E[0:P, :n], rs[0:P, :n], ALU.mult, reads=[E, rs], writes=[aff])
            self.cp("dve", work[0:P, :n], aff[0:P, :n], reads=[aff], writes=[work])
            rounds = cap // 8
            for r_ in range(rounds):
                fw.op("dve", lambda e, n=n: e.max(out=m8[0:P, :], in_=work[0:P, :n]), reads=[work], writes=[m8])
                if r_ < rounds - 1:
                    fw.op("dve", lambda e, n=n: e.match_replace(out=work[0:P, :n], in_to_replace=m8[0:P, :], in_values=work[0:P, :n], imm_value=-1.0),
                          reads=[m8, work], writes=[work])
            self.ts("dve", mask[0:P, :n], aff[0:P, :n], m8[0:P, 7:8], ALU.is_ge, reads=[aff, m8], writes=[mask])
            fw.op("dve", lambda e, n=n: e.tensor_tensor_scan(out=pin[0:P, :n], data0=onesf[0:P, :n], data1=mask[0:P, :n], initial=0.0, op0=ALU.mult, op1=ALU.add),
                  reads=[onesf, mask], writes=[pin])
            self.tt("dve", pin[0:P, :n], pin[0:P, :n], mask[0:P, :n], ALU.mult, reads=[pin, mask], writes=[pin])
            self.ts("dve", posm[0:P, :n], pin[0:P, :n], -1.0, ALU.add, reads=[pin], writes=[posm])
            self.dma("sp", dr["rt"][gi, :, 0, 0:n], aff[0:P, :n], reads=[aff])
            self.dma("sp", dr["rt"][gi, :, 1, 0:n], posm[0:P, :n], reads=[posm])
            self.dma("sp", dr["rt"][gi, :, 2, 0:n], mask[0:P, :n], reads=[mask])
            pT = ps[2]
            ntc = n // 128
            for tc in range(ntc):
                self.tr(pT, pT[:, tc * 32:(tc + 1) * 32], posm[0:P, tc * 128:(tc + 1) * 128], self.ident[0:P, 0:P], reads=[posm, self.ident])
            self.cp("act", posT[:, 0:ntc * 32], pT[:, 0:ntc * 32], reads=[pT], writes=[posT])
        self.phase_end()
        sb.release()

    def p6_experts(self, l, last):
        self.p6a_gather(l, last)
        self.p6b_ffn(l, last)

    def p6a_gather(self, l, last):
        sb, dr, fw, ps = self.sb, self.dr, self.fw, self.ps
        sb.mark()
        NCX = 0 if last else 2 * CAPC
        NX = 512 + NCX
        iota_f = sb.alloc(256, F32)
        self.dma("sp", iota_f[:, :], dr["c_iota_f"][:, :], writes=[iota_f])
        h2l = []
        h2c = []
        for s in range(NS):
            t_ = sb.alloc(16 * D, BF16, name="h2l")
            tv = t_[:, :].rearrange("p (c d) -> p c d", c=16)
            for q4 in range(4):
                self.dma("sp", tv[:, q4 * 4:(q4 + 1) * 4, :], dr["h2tm"][s, LC + q4 * 512:LC + (q4 + 1) * 512, :].rearrange("(c p) d -> p c d", p=128), pwrites=[t_])
            h2l.append((t_, tv))
            if not last:
                c_ = sb.alloc(2 * D, BF16, name="h2c")
                cv_ = c_[:, :].rearrange("p (c d) -> p c d", c=2)
                self.dma("sp", cv_, dr["h2tm"][s, 0:LC, :].rearrange("(c p) d -> p c d", p=128), writes=[c_])
                h2c.append((c_, cv_))
        xeT = [[sb.alloc(NX, BF16) for k in range(KC)] for i in range(2)]
        sel = [[sb.alloc(256, BF16) for i in range(16)] for j in range(2)]
        selc = [sb.alloc(32, BF16) for i in range(2)]
        it = 0
        for e in range(NE):
            xb = xeT[e % 2]
            for s in range(NS):
                col = s * NE + e
                sl = sel[it % 2]
                it += 1
                for tc in range(16):
                    self.ts("dve", sl[tc][:, :], iota_f[:, 0:256], self.posT_lat[:, tc * 32 + col:tc * 32 + col + 1], ALU.is_equal,
                            reads=[iota_f, self.posT_lat], writes=[sl[tc]])
                for m in range(KC):
                    pX = ps[m % 4]
                    for tc in range(16):
                        self.mm(pX, pX[:, :256], h2l[s][1][:, tc, m * 128:(m + 1) * 128], sl[tc][:, :], tc == 0, tc == 15, reads=[h2l[s][0], sl[tc]])
                    self.cp("act", xb[m][:, s * 256:(s + 1) * 256], pX[:, :256], reads=[pX], pwrites=[xb[m]])
                if NCX:
                    for tc in range(2):
                        self.ts("dve", selc[tc][:, :], iota_f[:, 0:32], self.posT_ctx[:, tc * 32 + col:tc * 32 + col + 1], ALU.is_equal,
                                reads=[iota_f, self.posT_ctx], writes=[selc[tc]])
                    for m in range(KC):
                        pX = ps[4 + m % 4]
                        for tc in range(2):
                            self.mm(pX, pX[:, :32], h2c[s][1][:, tc, m * 128:(m + 1) * 128], selc[tc][:, :], tc == 0, tc == 1, reads=[h2c[s][0], selc[tc]])
                        self.cp("act", xb[m][:, 512 + s * 32:512 + (s + 1) * 32], pX[:, :32], reads=[pX], pwrites=[xb[m]])
            for m in range(KC):
                self.dma("sp", dr["xe"][e, m * 128:(m + 1) * 128, 0:NX], xb[m][:, :], reads=[xb[m]])
        self.phase_end()
        sb.release()

    def p6b_ffn(self, l, last):
        sb, dr, fw, ps = self.sb, self.dr, self.fw, self.ps
        sb.mark()
        NCX = 0 if last else 2 * CAPC
        NX = 512 + NCX
        NR = 4
        wring = [sb.alloc(8192, BF16, name="wring%d" % i) for i in range(NR)]
        xeb = [sb.alloc(KC * NX, BF16) for i in range(2)]
        hid = [sb.alloc(NX, BF16) for f in range(16)]
        sgt = [sb.alloc(512, F32) for i in range(2)]
        sgc = [sb.alloc(64, F32) for i in range(2)]
        yst = [sb.alloc(512, BF16) for i in range(2)]
        units = []
        for e in range(NE):
            for fh in range(2):
                units.append(("w_gate", e, fh)); units.append(("w_up", e, fh))
            for dh in range(2):
                units.append(("w_down", e, dh))
        loaded = {}

        def issue(ui):
            if ui >= len(units) or ui in loaded:
                return
            nm, e, hh = units[ui]
            wt = wring[ui % NR]
            if nm == "w_down":
                self.dma("pool", wt[:, :].rearrange("p (c n) -> p c n", c=16), dr[nm][l, e, :, hh * 512:(hh + 1) * 512].rearrange("(c p) n -> p c n", p=128), writes=[wt])
            else:
                self.dma("pool", wt[:, :].rearrange("p (c n) -> p c n", c=KC), dr[nm][l, e, :, hh * 1024:(hh + 1) * 1024].rearrange("(c p) n -> p c n", p=128), writes=[wt])
            loaded[ui] = wt

        for ui in range(NR - 1):
            issue(ui)
        ui = 0
        yi = 0
        for e in range(NE):
            xt_ = xeb[e % 2]
            xv = xt_[:, :].rearrange("p (k n) -> p k n", k=KC)
            self.dma("sp", xv, dr["xe"][e, :, 0:NX].rearrange("(k p) n -> p k n", p=128), writes=[xt_])
            for fh in range(2):
                for uj in range(ui, ui + NR):
                    issue(uj)
                wg = loaded[ui]; wu = loaded[ui + 1]; ui += 2
                wgv = wg[:, :].rearrange("p (c n) -> p c n", c=KC)
                wuv = wu[:, :].rearrange("p (c n) -> p c n", c=KC)
                for f in range(8):
                    fi = fh * 8 + f
                    pG, pU, pGc, pUc = ps[fi % 2], ps[2 + fi % 2], ps[4], ps[5]
                    for k in range(KC):
                        self.mm(pG, pG[:, :512], wgv[:, k, f * 128:(f + 1) * 128], xv[:, k, 0:512], k == 0, k == KC - 1, reads=[wg, xt_])
                    for k in range(KC):
                        self.mm(pU, pU[:, :512], wuv[:, k, f * 128:(f + 1) * 128], xv[:, k, 0:512], k == 0, k == KC - 1, reads=[wu, xt_])
                    if NCX:
                        for k in range(KC):
                            self.mm(pGc, pGc[:, :NCX], wgv[:, k, f * 128:(f + 1) * 128], xv[:, k, 512:NX], k == 0, k == KC - 1, reads=[wg, xt_])
                        for k in range(KC):
                            self.mm(pUc, pUc[:, :NCX], wuv[:, k, f * 128:(f + 1) * 128], xv[:, k, 512:NX], k == 0, k == KC - 1, reads=[wu, xt_])
                    sg = sgt[fi % 2]
                    self.act(sg[:, :], pG[:, :512], AF.Silu, reads=[pG], writes=[sg])
                    self.tt("dve", hid[fi][:, 0:512], sg[:, :], pU[:, :512], ALU.mult, reads=[sg, pU], pwrites=[hid[fi]])
                    if NCX:
                        sc_ = sgc[fi % 2]
                        self.act(sc_[:, :], pGc[:, :NCX], AF.Silu, reads=[pGc], writes=[sc_])
                        self.tt("dve", hid[fi][:, 512:NX], sc_[:, :], pUc[:, :NCX], ALU.mult, reads=[sc_, pUc], pwrites=[hid[fi]])
            for dh in range(2):
                for uj in range(ui, ui + NR):
                    issue(uj)
                wd = loaded[ui]; ui += 1
                wdv = wd[:, :].rearrange("p (c n) -> p c n", c=16)
                rgs = [(s * 256 + c * 128, 128, ("ye", s, c)) for s in range(NS) for c in range(2)]
                if NCX:
                    rgs.append((512, NCX, ("yec",)))
                for (c0, M, dst) in rgs:
                    pY = ps[6 + yi % 2]
                    ys = yst[yi % 2]
                    yi += 1
                    for f in range(16):
                        self.mm(pY, pY[0:M, :512], hid[f][:, c0:c0 + M], wdv[:, f, :], f == 0, f == 15, reads=[hid[f], wd])
                    self.cp("act", ys[0:M, :], pY[0:M, :512], reads=[pY], writes=[ys])
                    if dst[0] == "ye":
                        self.dma("sp", dr["ye"][dst[1], e, dst[2] * 128:(dst[2] + 1) * 128, dh * 512:(dh + 1) * 512], ys[0:128, :], reads=[ys])
                    else:
                        for s in range(NS):
                            self.dma("sp", dr["yec"][s, e, :, dh * 512:(dh + 1) * 512], ys[s * CAPC:(s + 1) * CAPC, :], reads=[ys])
        self.phase_end()
        sb.release()

    def p7_scatter(self, l, Xm, Xout, last):
        sb, dr, fw, ps = self.sb, self.dr, self.fw, self.ps
        sb.mark()
        W = 512
        rowsel = sb.alloc(32 * 128, BF16)
        self.dma("pool", rowsel[0:32, :], dr["c_rowsel"][:, :], writes=[rowsel])
        iota_p = sb.alloc(2, F32)
        self.dma("sp", iota_p[:, :], dr["c_iota_p"][:, :], writes=[iota_p])
        gfin = sb.alloc(KC, F32)
        self.dma("sp", gfin[:, :], dr["gfinT"][:, :], writes=[gfin])
        affr = sb.alloc(S, BF16); posr = sb.alloc(S, BF16)
        yeall = sb.alloc(NE * 2 * D, BF16, name="yeall")
        selT = [sb.alloc(W, BF16) for i in range(32)]
        affb = [sb.alloc(W, F32) for i in range(2)]
        xin = sb.alloc(KC * W, F32)
        xn = [sb.alloc(W, F32) for c in range(KC)]
        sqb = [sb.alloc(W, BF16) for c in range(KC)]
        ot = [sb.alloc(W, F32) for i in range(2)]
        rstd = sb.alloc(W, F32); lnv = sb.alloc(W, F32)
        groups = [(0, LC, S, 128, 2)]
        if not last:
            groups.append((1, 0, LC, CAPC, 1))
        v3 = lambda t: t[:, :].rearrange("p (c w) -> p c w", c=KC)
        for gi, a0, n, NP, ncc in groups:
            self.dma("pool", affr[0:32, :n], dr["rt"][gi, :, 0, 0:n], writes=[affr])
            self.dma("pool", posr[0:32, :n], dr["rt"][gi, :, 1, 0:n], writes=[posr])
            for s in range(NS):
                e3 = s if gi == 0 else 2
                if gi == 0:
                    yv = yeall[:, :].rearrange("p (e c d) -> p e c d", e=NE, c=2)
                    for q8 in range(8):
                        self.dma("sp", yv[:, q8 * 2:(q8 + 1) * 2, :, :], dr["ye"][s, q8 * 2:(q8 + 1) * 2, :, :].rearrange("e (c p) d -> p e c d", p=128), pwrites=[yeall])
                else:
                    yv = yeall[:, 0:NE * D].rearrange("p (e c d) -> p e c d", e=NE, c=1)
                    self.dma("sp", yv[0:CAPC, :, 0, :], dr["yec"][s].rearrange("e j d -> j e d"), writes=[yeall])
                for c0 in range(0, n, W):
                    w = min(W, n - c0)
                    for e in range(NE):
                        r_ = s * NE + e
                        pP = ps[e % 2]; pA = ps[2 + e % 2]
                        self.mm(pP, pP[:, :w], rowsel[0:32, r_ * 128:(r_ + 1) * 128], posr[0:32, c0:c0 + w], True, True, reads=[rowsel, posr])
                        self.mm(pA, pA[:, :w], rowsel[0:32, r_ * 128:(r_ + 1) * 128], affr[0:32, c0:c0 + w], True, True, reads=[rowsel, affr])
                        ab = affb[e % 2]
                        self.cp("act", ab[:, :w], pA[:, :w], reads=[pA], writes=[ab])
                        for cc in range(ncc):
                            st_ = selT[e * 2 + cc]
                            self.stt(st_[0:NP, :w], pP[0:NP, :w], iota_p[0:NP, cc:cc + 1], ab[0:NP, :w], ALU.is_equal, ALU.mult, reads=[pP, iota_p, ab], writes=[st_])
                    self.dma("sp", v3(xin)[:, :, :w], Xm[s, :, a0 + c0:a0 + c0 + w].rearrange("(c p) w -> p c w", p=128), writes=[xin])
                    for m in range(KC):
                        pY = ps[4 + m % 2]
                        nmm = NE * ncc
                        idx = 0
                        for e in range(NE):
                            for cc in range(ncc):
                                self.mm(pY, pY[:, :w], yv[0:NP, e, cc, m * 128:(m + 1) * 128], selT[e * 2 + cc][0:NP, :w], idx == 0, idx == nmm - 1, reads=[yeall, selT[e * 2 + cc]])
                                idx += 1
                        self.stt(xn[m][:, :w], pY[:, :w], self.mcol(l, 5, m, e3), v3(xin)[:, m, :w], ALU.mult, ALU.add, reads=[pY, self.modc[l], xin], writes=[xn[m]])
                        if not last:
                            self.dma("sp", Xout[s, m * 128:(m + 1) * 128, a0 + c0:a0 + c0 + w], xn[m][:, :w], reads=[xn[m]])
                    if last:
                        self.rstd_from(xn, w, sqb, ps[6], rstd, lnv)
                        for m in range(KC):
                            o_ = ot[m % 2]
                            self.stt(o_[:, :w], xn[m][:, :w], gfin[:, m:m + 1], rstd[:, :w], ALU.mult, ALU.mult, reads=[xn[m], gfin, rstd], writes=[o_])
                            self.dma("sp", dr["outT"][s, m * 128:(m + 1) * 128, c0:c0 + w], o_[:, :w], reads=[o_])
        self.phase_end()
        sb.release()

def prep_shared(inp):
    f = lambda a: np.ascontiguousarray(a, dtype=np.float32)
    sh = {}
    sh["ada_w"] = f(inp["ada_w"])
    sh["ada_bT"] = f(inp["ada_b"].reshape(DEPTH, 48, 128).transpose(0, 2, 1))
    sh["gmixT"] = f(inp["norm_mix_g"].reshape(DEPTH, KC, 128).transpose(0, 2, 1))
    sh["gffnT"] = f(inp["norm_ffn_g"].reshape(DEPTH, KC, 128).transpose(0, 2, 1))
    sh["gfinT"] = f(inp["final_norm_g"].reshape(KC, 128).T)
    sh["w_in"] = f(inp["w_in"])
    sh["sink"] = f(inp["attn_sink"].reshape(DEPTH, 1, 8))
    cw = np.concatenate([inp["conv_w"], inp["conv_b"][:, None, :]], axis=1)
    sh["convT"] = f(cw.reshape(DEPTH, 5, KC, 128).transpose(0, 3, 1, 2))
    sh["lru_w"] = f(np.stack([inp["lru_wa"], inp["lru_wx"]], axis=1))
    lv = np.stack([inp["lru_ba"], inp["lru_bx"], inp["lru_lambda"]], axis=1)
    sh["lru_vT"] = f(lv.reshape(DEPTH, 3, 2, KC, 128).transpose(0, 4, 1, 2, 3))
    for k in ("w_attn_br", "w_rec_br", "w_out", "w_router", "w_gate", "w_up", "w_down"):
        sh[k] = f(inp[k])
    for k, v in host_consts().items():
        sh["c_" + k] = f(v)
    return sh


def prep_core(inp, core):
    b0 = core * NS
    xs = inp["x"][b0:b0 + NS]
    cs = inp["ctx"][b0:b0 + NS]
    xT = np.concatenate([cs, xs], axis=1).transpose(0, 2, 1)
    cc = np.stack([inp["c"][b0], inp["c"][b0 + 1], inp["c_ctx"]], axis=1)
    return {"xT": np.ascontiguousarray(xT, dtype=np.float32),
            "cT": np.ascontiguousarray(cc.reshape(KC, 128, 3).transpose(1, 0, 2), dtype=np.float32)}


_NC_CACHE = {}


def kernel(**inputs):
    inp = {k: np.asarray(v) for k, v in inputs.items()}
    n = 8
    if "nc" not in _NC_CACHE:
        kb = KB()
        _NC_CACHE["nc"] = kb.build()
        _NC_CACHE["names"] = set(kb.dr.keys())
    nc = _NC_CACHE["nc"]
    sh = prep_shared(inp)
    in_maps = []
    for core in range(n):
        m = dict(sh)
        m.update(prep_core(inp, core))
        in_maps.append({k: v for k, v in m.items() if k in _NC_CACHE["names"]})
    res = run_bass_kernel_spmd(nc, in_maps, core_ids=list(range(n)))
    outs = [np.asarray(r["outT"]).transpose(0, 2, 1) for r in res.results]
    return np.ascontiguousarray(np.concatenate(outs, axis=0), dtype=np.float32)
```

```python
import numpy as np
import concourse.bass as bass
import concourse.mybir as mybir

F32 = mybir.dt.float32
BF16 = mybir.dt.bfloat16
I32 = mybir.dt.int32
AF = mybir.ActivationFunctionType
ALU = mybir.AluOpType
AX = mybir.AxisListType
from concourse.bass_utils import run_bass_kernel_spmd
ENGS = ("pe", "act", "dve", "pool", "sp")


class Op:
    __slots__ = ("eng", "emit", "deps", "dma_deps", "signal", "count", "is_dma", "dsem", "dval", "prev_dval", "idx")
    _ctr = 0

    def __init__(self, eng, emit, is_dma=False):
        self.eng = eng
        self.emit = emit
        self.deps = {}
        self.dma_deps = []
        self.signal = False
        self.count = 0
        self.is_dma = is_dma
        self.dsem = None
        self.dval = 0
        self.prev_dval = 0
        Op._ctr += 1
        self.idx = Op._ctr


class Tile:
    def __init__(self, ap, name=""):
        self.ap = ap
        self.name = name
        self.w_c = {}
        self.w_d = []
        self.r_c = {}
        self.r_d = []
        self.g_c = {}
        self.g_d = []

    def __getitem__(self, k):
        return self.ap[k]


def _merge(dst_c, dst_d, src_c, src_d):
    for e, o in src_c.items():
        if e not in dst_c or dst_c[e].idx < o.idx:
            dst_c[e] = o
    for o in src_d:
        if o not in dst_d:
            dst_d.append(o)


class FW:
    def __init__(self, nc, n_dma_sems=4):
        self.nc = nc
        self.ops = {e: [] for e in ENGS}
        self.sem = {}
        self.dma_pool = {}
        self.dma_rr = {}
        self.n_dma_sems = n_dma_sems
        self.out_dmas = []
        self.last = {e: None for e in ENGS}
        self._cms = []

    def setup_sems(self, es):
        nc = self.nc
        for e in ENGS:
            self.sem[e] = es.enter_context(nc.semaphore("s_" + e))
        for q in ("sp", "act", "pool"):
            self.dma_pool[q] = [[es.enter_context(nc.semaphore("d_%s%d" % (q, i))), 0] for i in range(self.n_dma_sems)]
            self.dma_rr[q] = 0

    def _add_read(self, op, t):
        _merge(op.deps, op.dma_deps, t.w_c, t.w_d)
        if op.is_dma:
            t.r_d.append(op)
        else:
            t.r_c[op.eng] = op

    def _add_write(self, op, t, partial):
        if (not partial) or t.r_c or t.r_d:
            t.g_c = {}
            t.g_d = []
            _merge(t.g_c, t.g_d, t.r_c, t.r_d)
            _merge(t.g_c, t.g_d, t.w_c, t.w_d)
            t.r_c, t.r_d, t.w_c, t.w_d = {}, [], {}, []
        _merge(op.deps, op.dma_deps, t.g_c, t.g_d)
        if op.is_dma:
            t.w_d.append(op)
        else:
            t.w_c[op.eng] = op

    def _record(self, o, reads, writes, pwrites):
        wset = set(id(t) for t in writes) | set(id(t) for t in pwrites)
        for t in reads:
            _merge(o.deps, o.dma_deps, t.w_c, t.w_d)
        for t in writes:
            self._add_write(o, t, False)
        for t in pwrites:
            self._add_write(o, t, True)
        for t in reads:
            if id(t) not in wset:
                if o.is_dma:
                    t.r_d.append(o)
                else:
                    t.r_c[o.eng] = o

    def op(self, eng, emit, reads=(), writes=(), pwrites=()):
        o = Op(eng, emit)
        self._record(o, reads, writes, pwrites)
        self.ops[eng].append(o)
        self.last[eng] = o
        return o

    def dma(self, q, out, in_, reads=(), writes=(), pwrites=(), **kw):
        o = Op(q, (lambda eng: eng.dma_start(out=out, in_=in_, **kw)), is_dma=True)
        pool = self.dma_pool[q]
        i = self.dma_rr[q]
        self.dma_rr[q] = (i + 1) % len(pool)
        o.dsem = pool[i][0]
        o.prev_dval = pool[i][1]
        pool[i][1] += 16
        o.dval = pool[i][1]
        self._record(o, reads, writes, pwrites)
        self.ops[q].append(o)
        self.out_dmas.append(o)
        return o

    def barrier(self):
        lasts = {e: o for e, o in self.last.items() if o is not None}
        for e in ENGS:
            o = Op(e, None)
            for e2, o2 in lasts.items():
                if e2 != e:
                    o.deps[e2] = o2
            o.dma_deps = list(self.out_dmas)
            self.ops[e].append(o)
        self.out_dmas = []

    def finalize(self):
        for e in ENGS:
            for o in self.ops[e]:
                for e2, p in o.deps.items():
                    if p.is_dma:
                        continue
                    if e2 == e and e == "pe":
                        continue
                    p.signal = True
        for e in ENGS:
            c = 0
            for o in self.ops[e]:
                if o.is_dma or o.emit is None:
                    continue
                if o.signal:
                    c += 1
                    o.count = c
        nc = self.nc
        engmap = {"pe": "tensor", "act": "scalar", "dve": "vector", "pool": "gpsimd", "sp": "sync"}
        stats = {}
        with nc.Block() as block:
            for e in ENGS:
                def body(engine, e=e):
                    waited = {}
                    nw = 0

                    def wait(sem, val):
                        nonlocal nw
                        k = id(sem)
                        if waited.get(k, 0) >= val:
                            return
                        waited[k] = val
                        engine.wait_ge(sem, val)
                        nw += 1

                    for o in self.ops[e]:
                        for e2, p in o.deps.items():
                            if p.is_dma:
                                wait(p.dsem, p.dval)
                                continue
                            if e2 == e and e == "pe":
                                continue
                            if p is o:
                                continue
                            wait(self.sem[e2], p.count)
                        for p in o.dma_deps:
                            wait(p.dsem, p.dval)
                        if o.emit is None:
                            continue
                        if o.is_dma:
                            if o.prev_dval > 0:
                                wait(o.dsem, o.prev_dval)
                            ins = o.emit(engine)
                            ins.then_inc(o.dsem, 16)
                        else:
                            ins = o.emit(engine)
                            if o.signal:
                                ins.then_inc(self.sem[e], 1)
                    stats[e] = (len(self.ops[e]), nw)
                getattr(block, engmap[e])(body)
        return stats


class Sbuf:
    def __init__(self, big, nwords):
        self.big = big
        self.n = nwords
        self.off = 0
        self.marks = []

    def mark(self):
        self.marks.append(self.off)

    def release(self):
        self.off = self.marks.pop()

    def alloc(self, nelem, dtype=F32, parts=128, name=""):
        if dtype == BF16:
            nw = (nelem + 1) // 2
        else:
            nw = nelem
        nw = (nw + 7) // 8 * 8
        assert self.off + nw <= self.n, "SBUF overflow %s: %d + %d > %d" % (name, self.off, nw, self.n)
        ap = self.big[0:parts, self.off:self.off + nw]
        self.off += nw
        if dtype == BF16:
            ap = ap.bitcast(BF16)[:, 0:nelem]
        elif dtype != F32:
            ap = ap.bitcast(dtype)[:, 0:nelem]
        else:
            ap = ap[:, 0:nelem]
        return Tile(ap, name)

from contextlib import ExitStack
import ml_dtypes

D = 1024
NS = 2
LC = 256
S = 2048
T = LC + S
DEPTH = 2
NE = 16
DEXP = 2048
IN_W = 5632
CAPL = 256
CAPC = 32
EPS = 1e-6
KC = 8

SB_WORDS = 46080


def host_consts():
    c = {}
    half = 32
    freqs = (10000.0 ** (-np.arange(half, dtype=np.float32) / half)).astype(np.float32)
    t = np.arange(S)
    rows = (t // 64).astype(np.float32)
    cols = (t % 64).astype(np.float32)
    C = np.zeros((128, S), np.float32)
    Sn = np.zeros((128, S), np.float32)
    for d in range(128):
        pos = rows if d < 64 else cols
        j = (d % 64) % 32
        ang = (pos * freqs[j]).astype(np.float32)
        C[d] = np.cos(ang)
        Sn[d] = np.sin(ang)
    c["ropeC"] = C
    c["ropeS"] = Sn
    Pm = np.zeros((128, 128), np.float32)
    for d in range(128):
        i = d % 64
        if i < 32:
            Pm[d + 32, d] = -1.0
        else:
            Pm[d - 32, d] = 1.0
    c["pm"] = Pm
    c["ident"] = np.eye(128, dtype=np.float32)
    kk = np.arange(128)[:, None]
    qq = np.arange(128)[None, :]
    mprev = (qq <= kk).astype(np.float32)
    mnext = (kk <= qq).astype(np.float32)
    c["mprev"] = np.tile(mprev, (1, 4))
    c["mnext"] = np.tile(mnext, (1, 4))
    c["iota_f"] = np.tile(np.arange(256, dtype=np.float32)[None, :], (128, 1))
    c["iota_p"] = np.stack([np.arange(128, dtype=np.float32), np.arange(128, dtype=np.float32) + 128], axis=1)
    sel = np.zeros((32, 32, 128), np.float32)
    for r in range(32):
        sel[r, r, :] = 1.0
    c["rowsel"] = sel.transpose(1, 0, 2).reshape(32, 32 * 128)
    bd = np.zeros((32, 32), np.float32)
    bd[:16, :16] = 1.0
    bd[16:, 16:] = 1.0
    c["bdones"] = bd
    return c


CONST_SHAPES = {"ropeC": (128, S), "ropeS": (128, S), "pm": (128, 128), "ident": (128, 128),
                "mprev": (128, 512), "mnext": (128, 512), "iota_f": (128, 256), "iota_p": (128, 2),
                "rowsel": (32, 32 * 128), "bdones": (32, 32)}


class KB:
    def __init__(self, debug=None, stop_after=None):
        self.debug = debug or []
        self.stop_after = stop_after
        self.nc = bass.Bass("TRN2", target_bir_lowering=False)
        self.dr = {}

    def din(self, name, shape, dt=F32):
        self.dr[name] = self.nc.dram_tensor(name, list(shape), dt, kind="ExternalInput").ap()
        return self.dr[name]

    def dscr(self, name, shape, dt=F32, out=False):
        kind = "ExternalOutput" if (out or name in self.debug) else "Internal"
        self.dr[name] = self.nc.dram_tensor(name, list(shape), dt, kind=kind).ap()
        return self.dr[name]

    def mm(self, pst, out, lhsT, rhs, start, stop, reads):
        self.fw.op("pe", lambda e: e.matmul(out, lhsT=lhsT, rhs=rhs, start=start, stop=stop), reads=reads, pwrites=[pst])

    def tr(self, pst, out, in_, ident, reads):
        self.fw.op("pe", lambda e: e.transpose(out, in_, ident), reads=reads, pwrites=[pst])

    def act(self, out, in_, func, reads, writes=(), pwrites=(), **kw):
        self.fw.op("act", lambda e: e.activation(out=out, in_=in_, func=func, **kw), reads=reads, writes=writes, pwrites=pwrites)

    def tt(self, eng, out, in0, in1, op, reads, writes=(), pwrites=()):
        self.fw.op(eng, lambda e: e.tensor_tensor(out=out, in0=in0, in1=in1, op=op), reads=reads, writes=writes, pwrites=pwrites)

    def ts(self, eng, out, in0, s1, op0, reads, s2=None, op1=None, writes=(), pwrites=()):
        if op1 is None:
            self.fw.op(eng, lambda e: e.tensor_scalar(out=out, in0=in0, scalar1=s1, scalar2=None, op0=op0), reads=reads, writes=writes, pwrites=pwrites)
        else:
            self.fw.op(eng, lambda e: e.tensor_scalar(out=out, in0=in0, scalar1=s1, scalar2=s2, op0=op0, op1=op1), reads=reads, writes=writes, pwrites=pwrites)

    def stt(self, out, in0, scalar, in1, op0, op1, reads, writes=(), pwrites=()):
        self.fw.op("dve", lambda e: e.scalar_tensor_tensor(out=out, in0=in0, scalar=scalar, in1=in1, op0=op0, op1=op1), reads=reads, writes=writes, pwrites=pwrites)

    def cp(self, eng, out, in_, reads, writes=(), pwrites=()):
        if eng == "act":
            self.fw.op("act", lambda e: e.copy(out=out, in_=in_), reads=reads, writes=writes, pwrites=pwrites)
        else:
            self.fw.op(eng, lambda e: e.tensor_copy(out=out, in_=in_), reads=reads, writes=writes, pwrites=pwrites)

    def dma(self, q, out, in_, reads=(), writes=(), pwrites=(), maxdesc=512):
        shp = tuple(out.shape)
        if len(shp) == 3 and shp[0] * shp[1] > maxdesc and tuple(in_.shape) == shp:
            step = max(1, maxdesc // shp[0])
            first = True
            for i0 in range(0, shp[1], step):
                i1 = min(shp[1], i0 + step)
                if first:
                    self.fw.dma(q, out[:, i0:i1, :], in_[:, i0:i1, :], reads=reads, writes=writes, pwrites=pwrites)
                    first = False
                else:
                    self.fw.dma(q, out[:, i0:i1, :], in_[:, i0:i1, :], reads=reads, pwrites=tuple(writes) + tuple(pwrites))
            return
        self.fw.dma(q, out, in_, reads=reads, writes=writes, pwrites=pwrites)

    def phase_end(self):
        self.fw.barrier()

    def build(self):
        nc = self.nc
        din, dscr = self.din, self.dscr
        din("xT", (NS, D, T))
        din("cT", (128, KC, 3))
        din("ada_w", (DEPTH, D, 6 * D))
        din("ada_bT", (DEPTH, 128, 48))
        din("gmixT", (DEPTH, 128, KC))
        din("gffnT", (DEPTH, 128, KC))
        din("gfinT", (128, KC))
        din("w_in", (DEPTH, D, IN_W))
        din("sink", (DEPTH, 1, 8))
        din("convT", (DEPTH, 128, 5, KC))
        din("lru_w", (DEPTH, 2, 2, 8, 128, 128))
        din("lru_vT", (DEPTH, 128, 3, 2, KC))
        din("w_attn_br", (DEPTH, D, D))
        din("w_rec_br", (DEPTH, D, D))
        din("w_out", (DEPTH, D, D))
        din("w_router", (DEPTH, D, NE))
        if self.stop_after not in ("p0", "p1", "p2", "p3", "p4", "p5", "wip"):
            din("w_gate", (DEPTH, NE, D, DEXP))
            din("w_up", (DEPTH, NE, D, DEXP))
            din("w_down", (DEPTH, NE, DEXP, D))
        for k, shp in CONST_SHAPES.items():
            din("c_" + k, shp)
        dscr("outT", (NS, D, S), out=True)
        dscr("X1", (NS, D, T)); dscr("X2", (NS, D, T)); dscr("X3", (NS, D, T))
        dscr("qT", (NS, D, T), BF16); dscr("kT", (NS, 256, T), BF16); dscr("vtm", (NS, T, 256), BF16)
        dscr("uT", (NS, D, T)); dscr("gzT", (NS, D, T), BF16); dscr("smaT", (NS, D, T), BF16); dscr("smrT", (NS, D, T), BF16)
        dscr("attT", (NS, D, T), BF16); dscr("rgT", (NS, D, T), BF16)
        dscr("h2tm", (NS, T, D), BF16); dscr("lgT", (NS, NE, T))
        dscr("ye", (NS, NE, CAPL, D), BF16); dscr("yec", (NS, NE, CAPC, D), BF16); dscr("xe", (NE, D, 512 + 2 * CAPC), BF16)
        dscr("modc", (DEPTH, 128, 6, KC, 3))
        dscr("rt", (2, 32, 3, S))

        with ExitStack() as es:
            big = es.enter_context(nc.sbuf_tensor("big", [128, SB_WORDS], F32))
            pst = [es.enter_context(nc.psum_tensor("ps%d" % i, [128, 512], F32)) for i in range(8)]
            self.fw = FW(nc)
            self.fw.setup_sems(es)
            self.sb = Sbuf(big, SB_WORDS)
            self.ps = [Tile(p, "ps%d" % i) for i, p in enumerate(pst)]
            self._body()
            self.fw.barrier()
            self.stats = self.fw.finalize()
        return nc

    def _body(self):
        sb = self.sb
        dr = self.dr
        fw = self.fw
        self.ident = sb.alloc(128, F32, name="ident")
        self.dma("sp", self.ident[:, :], dr["c_ident"][:, :], writes=[self.ident])
        self.identb = sb.alloc(128, BF16, name="identb")
        self.dma("pool", self.identb[:, :], dr["c_ident"][:, :], writes=[self.identb])
        self.pmb = sb.alloc(128, BF16, name="pmb")
        self.dma("pool", self.pmb[:, :], dr["c_pm"][:, :], writes=[self.pmb])
        self.onesb = sb.alloc(128, BF16, name="onesb")
        fw.op("dve", lambda e: e.memset(self.onesb[:, :], 1.0), writes=[self.onesb])
        self.epsc = sb.alloc(1, F32, name="epsc")
        fw.op("dve", lambda e: e.memset(self.epsc[:, :], EPS), writes=[self.epsc])
        self.silu_c = sb.alloc(KC * 3, F32, name="silu_c")
        ctmp = sb.alloc(KC * 3, F32, name="ctmp")
        self.dma("sp", ctmp[:, :], dr["cT"].rearrange("p c e -> p (c e)"), writes=[ctmp])
        self.act(self.silu_c[:, :], ctmp[:, :], AF.Silu, reads=[ctmp], writes=[self.silu_c])
        self.modc = [sb.alloc(6 * KC * 3, F32, name="modc%d" % l) for l in range(DEPTH)]
        self.A1 = [sb.alloc(KC * 3, F32) for l in range(DEPTH)]
        self.A2 = [sb.alloc(KC * 3, F32) for l in range(DEPTH)]
        self.posT_lat = sb.alloc(16 * 32, F32, name="posT_lat")
        self.posT_ctx = sb.alloc(2 * 32, F32, name="posT_ctx")
        self.base_off = sb.off
        Xs = ["xT", "X1", "X2", "X3"]
        for l in range(DEPTH):
            last = l == DEPTH - 1
            self.p0_mod(l)
            if self.stop_after == "p0":
                return
            self.p1_inproj(l, dr["xT"] if l == 0 else dr["X2"])
            if self.stop_after == "p1":
                return
            self.p2_attn(l, last)
            if self.stop_after == "p2":
                return
            self.p3_rec(l)
            if self.stop_after == "p3":
                return
            if self.stop_after == "wip_DISABLED":
                self.pF_copy()
                return
            self.p4_merge(l, dr["xT"] if l == 0 else dr["X2"], dr["X1"], last)
            if self.stop_after == "p4":
                return
            self.p5_route(l, last)
            if self.stop_after == "p5":
                return
            self.p6_experts(l, last)
            if self.stop_after == "p6":
                return
            self.p7_scatter(l, dr["X1"], dr["X2"] if not last else None, last)
            if self.stop_after == "p7":
                return

    def p0_mod(self, l):
        sb, dr, fw, ps = self.sb, self.dr, self.fw, self.ps
        sb.mark()
        NB = 12
        wbuf = [sb.alloc(KC * 512, F32, name="adaw%d" % i) for i in range(2)]
        adab = sb.alloc(48, F32)
        gm = sb.alloc(KC, F32)
        gf = sb.alloc(KC, F32)
        self.dma("sp", adab[:, :], dr["ada_bT"][l], writes=[adab])
        self.dma("sp", gm[:, :], dr["gmixT"][l], writes=[gm])
        self.dma("sp", gf[:, :], dr["gffnT"][l], writes=[gf])
        pt = ps[0]
        modc = self.modc[l]
        for nb in range(NB):
            wb = wbuf[nb % 2]
            self.dma("sp" if nb % 2 == 0 else "pool", wb[:, :].rearrange("p (c n) -> p c n", c=KC),
                   dr["ada_w"][l, :, nb * 512:(nb + 1) * 512].rearrange("(c p) n -> p c n", p=128), writes=[wb])
            for jj in range(4):
                j = nb * 4 + jj
                for k in range(KC):
                    self.mm(pt, pt[:, j * 3:(j + 1) * 3], wb[:, k * 512 + jj * 128:k * 512 + (jj + 1) * 128],
                            self.silu_c[:, k * 3:(k + 1) * 3], k == 0, k == KC - 1, reads=[wb, self.silu_c])
        for e3 in range(3):
            self.tt("dve", modc[:, :].rearrange("p (j e) -> p j e", e=3)[:, :, e3],
                    pt[:, 0:144].rearrange("p (j e) -> p j e", e=3)[:, :, e3], adab[:, :], ALU.add,
                    reads=[pt, adab], pwrites=[modc])
        m4 = modc[:, :].rearrange("p (w c e) -> p w c e", w=6, c=KC)
        a1 = self.A1[l][:, :].rearrange("p (c e) -> p c e", e=3)
        a2 = self.A2[l][:, :].rearrange("p (c e) -> p c e", e=3)
        for e3 in range(3):
            self.stt(a1[:, :, e3], m4[:, 1, :, e3], 1.0, gm[:, :], ALU.add, ALU.mult, reads=[modc, gm], pwrites=[self.A1[l]])
            self.stt(a2[:, :, e3], m4[:, 4, :, e3], 1.0, gf[:, :], ALU.add, ALU.mult, reads=[modc, gf], pwrites=[self.A2[l]])
        if "modc" in self.debug:
            self.dma("sp", dr["modc"][l].rearrange("p w c e -> p (w c e)"), modc[:, :], reads=[modc])
        self.phase_end()
        sb.release()

    def mcol(self, l, which, c, e):
        i = (which * KC + c) * 3 + e
        return self.modc[l][:, i:i + 1]

    def rstd_from(self, xt_tiles, w, sqb, pt, rstd, lnv):
        for c in range(KC):
            self.act(sqb[c][:, :w], xt_tiles[c][:, :w], AF.Square, reads=[xt_tiles[c]], writes=[sqb[c]])
        for c in range(KC):
            self.mm(pt, pt[:, :w], self.onesb[:, :], sqb[c][:, :w], c == 0, c == KC - 1, reads=[self.onesb, sqb[c]])
        self.act(lnv[:, :w], pt[:, :w], AF.Ln, reads=[pt], writes=[lnv], scale=1.0 / D, bias=self.epsc[:, 0:1])
        self.act(rstd[:, :w], lnv[:, :w], AF.Exp, reads=[lnv], writes=[rstd], scale=-0.5)

    def p1_inproj(self, l, Xin):
        sb, dr, fw, ps = self.sb, self.dr, self.fw, self.ps
        sb.mark()
        W = 256
        win = sb.alloc(KC * IN_W, BF16, name="win")
        winv = win[:, :].rearrange("p (c n) -> p c n", c=KC)
        for nb in range(11):
            self.dma("pool", winv[:, :, nb * 512:(nb + 1) * 512],
                   dr["w_in"][l, :, nb * 512:(nb + 1) * 512].rearrange("(c p) n -> p c n", p=128), pwrites=[win])
        ropeC = sb.alloc(S, F32); ropeS = sb.alloc(S, F32); pmb = self.pmb
        self.dma("sp", ropeC[:, :], dr["c_ropeC"][:, :], writes=[ropeC])
        self.dma("sp", ropeS[:, :], dr["c_ropeS"][:, :], writes=[ropeS])
        xt = [[sb.alloc(W, F32) for c in range(KC)] for i in range(2)]
        sqb = [sb.alloc(W, BF16) for c in range(KC)]
        hT = [sb.alloc(W, BF16) for c in range(KC)]
        tmpf = [sb.alloc(W, F32) for i in range(2)]
        rstd = sb.alloc(W, F32); lnv = sb.alloc(W, F32)
        qraw = [sb.alloc(W, BF16) for i in range(2)]
        t1 = [sb.alloc(W, F32) for i in range(2)]
        t2 = [sb.alloc(W, F32) for i in range(2)]
        tmpe = [sb.alloc(W, F32) for i in range(2)]
        tmpe2 = [sb.alloc(W, F32) for i in range(2)]
        st_q = sb.alloc(KC * W, BF16); st_k = sb.alloc(2 * W, BF16)
        st_u = sb.alloc(KC * W, F32)
        st_g = [sb.alloc(KC * W, BF16) for i in range(3)]
        st_v = [sb.alloc(256, BF16) for i in range(2)]
        ntile = T // W
        tl = [(s, ti) for s in range(NS) for ti in range(ntile)]
        v3 = lambda t, n: t[:, :].rearrange("p (c w) -> p c w", c=n)

        def load_x(idx):
            s, ti = tl[idx]
            xb = xt[idx % 2]
            for c in range(KC):
                self.dma("sp", xb[c][:, :], Xin[s, c * 128:(c + 1) * 128, ti * W:(ti + 1) * W], writes=[xb[c]])

        load_x(0)
        for it in range(len(tl)):
            s, ti = tl[it]
            t0 = ti * W
            is_ctx = ti == 0
            e3 = 2 if is_ctx else s
            xb = xt[it % 2]
            if it + 1 < len(tl):
                load_x(it + 1)
            self.rstd_from(xb, W, sqb, ps[0], rstd, lnv)
            for c in range(KC):
                tf = tmpf[c % 2]
                self.stt(tf[:, :], xb[c][:, :], self.A1[l][:, c * 3 + e3:c * 3 + e3 + 1], rstd[:, :], ALU.mult, ALU.mult,
                         reads=[xb[c], self.A1[l], rstd], writes=[tf])
                self.act(hT[c][:, :], tf[:, :], AF.Identity, reads=[tf, self.modc[l]], writes=[hT[c]],
                         bias=self.mcol(l, 0, c, e3), scale=1.0)
            for oc in range(44):
                if 10 <= oc < 12:
                    continue
                pt = ps[1 + (oc % 4)]
                for k in range(KC):
                    self.mm(pt, pt[:, :W], winv[:, k, oc * 128:(oc + 1) * 128], hT[k][:, :], k == 0, k == KC - 1, reads=[win, hT[k]])
                if oc < 10:
                    stt_, dst = (st_q, st_q[:, oc * W:(oc + 1) * W]) if oc < 8 else (st_k, st_k[:, (oc - 8) * W:(oc - 7) * W])
                    if is_ctx:
                        self.cp("act", dst, pt[:, :W], reads=[pt], pwrites=[stt_])
                    else:
                        qr_ = qraw[oc % 2]; a1 = t1[oc % 2]; a2 = t2[oc % 2]
                        p2 = ps[5 + (oc % 2)]
                        lt0 = t0 - LC
                        self.cp("act", qr_[:, :], pt[:, :W], reads=[pt], writes=[qr_])
                        self.mm(p2, p2[:, :W], pmb[:, :], qr_[:, :], True, True, reads=[pmb, qr_])
                        e1_ = tmpe[oc % 2]; e2_ = tmpe2[oc % 2]
                        self.cp("act", e1_[:, :], pt[:, :W], reads=[pt], writes=[e1_])
                        self.cp("act", e2_[:, :], p2[:, :W], reads=[p2], writes=[e2_])
                        self.tt("dve", a1[:, :], e1_[:, :], ropeC[:, lt0:lt0 + W], ALU.mult, reads=[e1_, ropeC], writes=[a1])
                        self.tt("dve", a2[:, :], e2_[:, :], ropeS[:, lt0:lt0 + W], ALU.mult, reads=[e2_, ropeS], writes=[a2])
                        self.tt("dve", dst, a1[:, :], a2[:, :], ALU.add, reads=[a1, a2], pwrites=[stt_])
                elif oc < 20:
                    self.cp("act", st_u[:, (oc - 12) * W:(oc - 11) * W], pt[:, :W], reads=[pt], pwrites=[st_u])
                else:
                    g = (oc - 20) // 8
                    c_ = (oc - 20) % 8
                    self.act(st_g[g][:, c_ * W:(c_ + 1) * W], pt[:, :W], AF.Gelu if g == 0 else AF.Sigmoid, reads=[pt], pwrites=[st_g[g]])
            for j in range(W // 128):
                pt = ps[7]
                for k in range(KC):
                    self.mm(pt, pt[:, :256], hT[k][:, j * 128:(j + 1) * 128], winv[:, k, 1280:1536], k == 0, k == KC - 1, reads=[win, hT[k]])
                sv = st_v[j % 2]
                self.cp("dve", sv[:, :], pt[:, :256], reads=[pt], writes=[sv])
                self.dma("pool", dr["vtm"][s, t0 + j * 128:t0 + (j + 1) * 128, :], sv[:, :], reads=[sv])
            fm = lambda nm: dr[nm][s, :, t0:t0 + W].rearrange("(c p) w -> p c w", p=128)
            self.dma("pool", fm("qT"), v3(st_q, KC), reads=[st_q])
            self.dma("pool", dr["kT"][s, :, t0:t0 + W].rearrange("(c p) w -> p c w", p=128), v3(st_k, 2), reads=[st_k])
            self.dma("sp", fm("uT"), v3(st_u, KC), reads=[st_u])
            self.dma("sp", fm("gzT"), v3(st_g[0], KC), reads=[st_g[0]])
            self.dma("pool", fm("smaT"), v3(st_g[1], KC), reads=[st_g[1]])
            self.dma("sp", fm("smrT"), v3(st_g[2], KC), reads=[st_g[2]])
        self.phase_end()
        sb.release()

    def p2_attn(self, l, last):
        sb, dr, fw, ps = self.sb, self.dr, self.fw, self.ps
        sb.mark()
        mprev = sb.alloc(512, BF16); mnext = sb.alloc(512, BF16)
        self.dma("pool", mprev[:, :], dr["c_mprev"][:, :], writes=[mprev])
        self.dma("pool", mnext[:, :], dr["c_mnext"][:, :], writes=[mnext])
        sk = sb.alloc(8, F32); ske = sb.alloc(8, F32); onef = sb.alloc(128, F32)
        esf = sb.alloc(1024, F32); eshi = sb.alloc(1024, BF16); eslo = sb.alloc(1024, BF16)
        self.dma("sp", sk[0:1, :], dr["sink"][l], writes=[sk])
        self.act(ske[0:1, :], sk[0:1, :], AF.Exp, reads=[sk], writes=[ske])
        fw.op("dve", lambda e: e.memset(onef[:, :], 1.0), writes=[onef])
        for h in range(8):
            self.ts("dve", esf[0:1, h * 128:(h + 1) * 128], onef[0:1, :], ske[0:1, h:h + 1], ALU.mult, reads=[onef, ske], pwrites=[esf])
        self.cp("dve", eshi[0:1, :], esf[0:1, :], reads=[esf], writes=[eshi])
        self.tt("dve", eslo[0:1, :], esf[0:1, :], eshi[0:1, :], ALU.subtract, reads=[esf, eshi], writes=[eslo])
        qall = sb.alloc(8 * T, BF16, name="qall")
        qv = qall[:, :].rearrange("p (h t) -> p h t", h=8)
        kt = [sb.alloc(T, BF16) for i in range(2)]
        vt = sb.alloc(18 * 256, BF16)
        Er = [sb.alloc(512, BF16) for i in range(10)]
        lnD2 = [sb.alloc(512, F32) for i in range(2)]; rD2 = [sb.alloc(512, F32) for i in range(2)]
        ost = [sb.alloc(4 * 512, BF16) for i in range(2)]
        scale = 1.0 / np.sqrt(128.0)
        for s in range(NS):
            for h in range(8):
                self.dma("sp", qv[:, h, :], dr["qT"][s, h * 128:(h + 1) * 128, :], pwrites=[qall])
            for c in range(2):
                self.dma("sp", kt[c][:, :], dr["kT"][s, c * 128:(c + 1) * 128, :], writes=[kt[c]])
            self.dma("sp", vt[:, :].rearrange("p (b f) -> p b f", f=256), dr["vtm"][s].rearrange("(b p) f -> p b f", p=128), writes=[vt])
            qblocks = [("lat", i) for i in range(16)]
            if not last:
                qblocks = [("ctx", 0), ("ctx", 1)] + qblocks
            for kind, i in qblocks:
                if kind == "lat":
                    tq = LC + i * 128
                    keys = [(0, 0, None), (128, 1, None)]
                    if i > 0:
                        keys.append((LC + (i - 1) * 128, 2 + i - 1, mprev))
                    keys.append((LC + i * 128, 2 + i, None))
                    if i < 15:
                        keys.append((LC + (i + 1) * 128, 2 + i + 1, mnext))
                    sw, si = 512, i % 4
                else:
                    tq = i * 128
                    keys = [(0, 0, None), (128, 1, None)]
                    sw, si = 256, i
                for kv in range(2):
                    Ek = Er[kv * 5:(kv + 1) * 5]; lnD = lnD2[kv]; rD = rD2[kv]
                    for idx, (kc0, vb, mk) in enumerate(keys):
                        pS = ps[idx % 3]
                        for g in range(4):
                            self.mm(pS, pS[:, g * 128:(g + 1) * 128], kt[kv][:, kc0:kc0 + 128], qv[:, 4 * kv + g, tq:tq + 128], True, True, reads=[kt[kv], qall])
                        E = Ek[idx]
                        self.act(E[:, :], pS[:, :], AF.Exp, reads=[pS], writes=[E], scale=float(scale))
                        if mk is not None:
                            self.tt("dve", E[:, :], E[:, :], mk[:, :], ALU.mult, reads=[E, mk], writes=[E])
                    pO, pD = ps[3 + (kv % 2) * 2], ps[4 + (kv % 2) * 2]
                    n = len(keys)
                    for idx, (kc0, vb, mk) in enumerate(keys):
                        self.mm(pO, pO[:, :], vt[:, vb * 256 + kv * 128:vb * 256 + (kv + 1) * 128], Ek[idx][:, :], idx == 0, idx == n - 1, reads=[vt, Ek[idx]])
                    for idx, (kc0, vb, mk) in enumerate(keys):
                        self.mm(pD, pD[:, :], self.onesb[:, :], Ek[idx][:, :], idx == 0, False, reads=[self.onesb, Ek[idx]])
                    self.mm(pD, pD[:, :], self.onesb[0:1, :], eshi[0:1, kv * 512:(kv + 1) * 512], False, False, reads=[self.onesb, eshi])
                    self.mm(pD, pD[:, :], self.onesb[0:1, :], eslo[0:1, kv * 512:(kv + 1) * 512], False, True, reads=[self.onesb, eslo])
                    self.act(lnD[:, :], pD[:, :], AF.Ln, reads=[pD], writes=[lnD])
                    self.act(rD[:, :], lnD[:, :], AF.Exp, reads=[lnD], writes=[rD], scale=-1.0)
                    ov = ost[kv][:, :].rearrange("p (g t) -> p g t", g=4)
                    self.tt("dve", ov[:, :, si * 128:(si + 1) * 128], pO[:, :].rearrange("p (g t) -> p g t", g=4),
                            rD[:, :].rearrange("p (g t) -> p g t", g=4), ALU.mult, reads=[pO, rD], pwrites=[ost[kv]])
                if (kind == "lat" and i % 4 == 3) or (kind == "ctx" and i == 1):
                    tq0 = tq + 128 - sw
                    for kv in range(2):
                        ov = ost[kv][:, :].rearrange("p (g t) -> p g t", g=4)
                        self.dma("pool", dr["attT"][s, kv * 512:(kv + 1) * 512, tq0:tq0 + sw].rearrange("(g d) t -> d g t", d=128),
                               ov[:, :, 0:sw], reads=[ost[kv]])
        self.phase_end()
        sb.release()

    def p3_rec(self, l):
        sb, dr, fw, ps = self.sb, self.dr, self.fw, self.ps
        sb.mark()
        lw = sb.alloc(2 * 2 * 8 * 128, BF16, name="lw")
        lwv = lw[:, :].rearrange("p (w d b j) -> p w d b j", w=2, d=2, b=8)
        for w_ in range(2):
            for d_ in range(2):
                self.dma("pool", lwv[:, w_, d_, :, :], dr["lru_w"][l, w_, d_].rearrange("b i j -> i b j"), pwrites=[lw])
        lv = sb.alloc(48, F32); cv = sb.alloc(40, F32)
        self.dma("sp", lv[:, :], dr["lru_vT"][l].rearrange("p a d c -> p (a d c)"), writes=[lv])
        self.dma("sp", cv[:, :], dr["convT"][l].rearrange("p a c -> p (a c)"), writes=[cv])
        onec = sb.alloc(1, F32)
        fw.op("dve", lambda e: e.memset(onec[:, :], 1.0), writes=[onec])
        e1 = sb.alloc(16, F32); l1 = sb.alloc(16, F32); cl = sb.alloc(16, F32)
        self.act(e1[:, :], lv[:, 32:48], AF.Exp, reads=[lv], writes=[e1], scale=-1.0)
        self.act(l1[:, :], e1[:, :], AF.Ln, reads=[e1, onec], writes=[l1], bias=onec[:, 0:1], scale=1.0)
        self.ts("dve", cl[:, :], l1[:, :], -8.0, ALU.mult, reads=[l1], writes=[cl])
        bu = [dict(u=sb.alloc(T, F32), uc=sb.alloc(T, F32), ucb=sb.alloc(T, BF16), gz=sb.alloc(T, BF16)) for i in range(2)]
        bd = [dict(r=sb.alloc(T, F32), gi=sb.alloc(T, F32), a=sb.alloc(T, F32)) for d_ in range(2)]
        hf = sb.alloc(T, F32); hb = sb.alloc(T, F32); og = sb.alloc(T, BF16)
        tiles = [(0, 256)] + [(256 + 512 * i, 512) for i in range(4)]
        items = [(s, c) for s in range(NS) for c in range(KC)]

        def conv_act(i):
            s, c = items[i]
            B = bu[i % 2]
            u, uc, gz = B["u"], B["uc"], B["gz"]
            self.dma("sp", u[:, :], dr["uT"][s, c * 128:(c + 1) * 128, :], writes=[u])
            self.dma("sp", gz[:, :], dr["gzT"][s, c * 128:(c + 1) * 128, :], writes=[gz])
            self.act(uc[:, :], u[:, :], AF.Identity, reads=[u, cv], writes=[uc], scale=cv[:, 2 * KC + c:2 * KC + c + 1], bias=cv[:, 4 * KC + c:4 * KC + c + 1])

        def conv_dve(i):
            s, c = items[i]
            B = bu[i % 2]
            u, uc, ucb = B["u"], B["uc"], B["ucb"]
            for k, d in ((0, -2), (1, -1), (3, 1)):
                for (sa, sb_) in ((0, LC), (LC, T)):
                    lo = max(sa, sa - d); hi = min(sb_, sb_ - d)
                    self.stt(uc[:, lo:hi], u[:, lo + d:hi + d], cv[:, k * KC + c:k * KC + c + 1], uc[:, lo:hi], ALU.mult, ALU.add,
                             reads=[u, cv, uc], pwrites=[uc])
            self.cp("dve", ucb[:, :], uc[:, :], reads=[uc], writes=[ucb])

        def gates(i, d_):
            s, c = items[i]
            ucb = bu[i % 2]["ucb"]
            r, gi, a = bd[d_]["r"], bd[d_]["gi"], bd[d_]["a"]
            for ti, (t0, w) in enumerate(tiles):
                pA = ps[(2 * ti) % 8]; pX = ps[(2 * ti + 1) % 8]
                self.mm(pA, pA[:, :w], lwv[:, 0, d_, c, :], ucb[:, t0:t0 + w], True, True, reads=[lw, ucb])
                self.mm(pX, pX[:, :w], lwv[:, 1, d_, c, :], ucb[:, t0:t0 + w], True, True, reads=[lw, ucb])
                self.act(r[:, t0:t0 + w], pA[:, :w], AF.Sigmoid, reads=[pA, lv], pwrites=[r], bias=lv[:, (0 * 2 + d_) * KC + c:(0 * 2 + d_) * KC + c + 1], scale=1.0)
                self.act(gi[:, t0:t0 + w], pX[:, :w], AF.Sigmoid, reads=[pX, lv], pwrites=[gi], bias=lv[:, (1 * 2 + d_) * KC + c:(1 * 2 + d_) * KC + c + 1], scale=1.0)
            self.act(a[:, :], r[:, :], AF.Exp, reads=[r, cl], writes=[a], scale=cl[:, d_ * KC + c:d_ * KC + c + 1])
            self.act(r[:, :], a[:, :], AF.Square, reads=[a], writes=[r])
            self.act(r[:, :], r[:, :], AF.Sqrt, reads=[r, onec], writes=[r], scale=-1.0, bias=onec[:, 0:1])

        def scan_dve(i, d_):
            uc = bu[i % 2]["uc"]
            q_, gi, a = bd[d_]["r"], bd[d_]["gi"], bd[d_]["a"]
            self.tt("dve", gi[:, :], gi[:, :], uc[:, :], ALU.mult, reads=[gi, uc], writes=[gi])
            self.tt("dve", gi[:, :], gi[:, :], q_[:, :], ALU.mult, reads=[gi, q_], writes=[gi])
            if d_ == 0:
                fw.op("dve", lambda e, a=a, gi=gi: e.tensor_tensor_scan(out=hf[:, :], data0=a[:, :], data1=gi[:, :], initial=0.0, op0=ALU.mult, op1=ALU.add),
                      reads=[a, gi], writes=[hf])
            else:
                fw.op("dve", lambda e, a=a, gi=gi: e.tensor_tensor_scan(out=hb[:, LC - 1::-1], data0=a[:, LC - 1::-1], data1=gi[:, LC - 1::-1], initial=0.0, op0=ALU.mult, op1=ALU.add),
                      reads=[a, gi], writes=[hb])
                fw.op("dve", lambda e, a=a, gi=gi: e.tensor_tensor_scan(out=hb[:, T - 1:LC - 1:-1], data0=a[:, T - 1:LC - 1:-1], data1=gi[:, T - 1:LC - 1:-1], initial=hb[:, 0:1], op0=ALU.mult, op1=ALU.add),
                      reads=[a, gi, hb], pwrites=[hb])

        def tail(i):
            s, c = items[i]
            gz = bu[i % 2]["gz"]
            self.tt("dve", hf[:, :], hf[:, :], hb[:, :], ALU.add, reads=[hf, hb], writes=[hf])
            self.tt("dve", og[:, :], hf[:, :], gz[:, :], ALU.mult, reads=[hf, gz], writes=[og])
            self.dma("pool", dr["rgT"][s, c * 128:(c + 1) * 128, :], og[:, :], reads=[og])

        n_it = len(items)
        conv_act(0)
        conv_dve(0)
        for i in range(n_it):
            if i + 1 < n_it:
                conv_act(i + 1)
            gates(i, 0)
            if i + 1 < n_it:
                conv_dve(i + 1)
            gates(i, 1)
            scan_dve(i, 0)
            scan_dve(i, 1)
            tail(i)
        self.phase_end()
        sb.release()

    def pF_copy(self):
        sb, dr, fw = self.sb, self.dr, self.fw
        sb.mark()
        buf = [sb.alloc(S, F32) for i in range(2)]
        i = 0
        for s in range(NS):
            for c in range(KC):
                b = buf[i % 2]
                self.dma("sp", b[:, :], dr["xT"][s, c * 128:(c + 1) * 128, LC:T], writes=[b])
                self.dma("sp", dr["outT"][s, c * 128:(c + 1) * 128, :], b[:, :], reads=[b])
                i += 1
        self.phase_end()
        sb.release()

    def p4_merge(self, l, Xin, Xout, last):
        sb, dr, fw, ps = self.sb, self.dr, self.fw, self.ps
        sb.mark()
        W = 512
        wts = []
        for name in ("w_attn_br", "w_rec_br", "w_out"):
            wt = sb.alloc(KC * D, BF16, name=name)
            wv = wt[:, :].rearrange("p (c n) -> p c n", c=KC)
            for hh in range(2):
                self.dma("pool", wv[:, :, hh * 512:(hh + 1) * 512], dr[name][l, :, hh * 512:(hh + 1) * 512].rearrange("(c p) n -> p c n", p=128), pwrites=[wt])
            wts.append((wt, wv))
        (wa, wav), (wr, wrv), (wo, wov) = wts
        wrt = sb.alloc(KC * NE, F32)
        wrtv = wrt[:, :].rearrange("p (c e) -> p c e", c=KC)
        self.dma("sp", wrtv, dr["w_router"][l].rearrange("(c p) e -> p c e", p=128), writes=[wrt])
        att = sb.alloc(KC * W, BF16); rg = sb.alloc(KC * W, BF16); sma = sb.alloc(KC * W, BF16); smr = sb.alloc(KC * W, BF16)
        xin = sb.alloc(KC * W, F32)
        mg = [sb.alloc(W, BF16) for c in range(KC)]
        xn = [sb.alloc(W, F32) for c in range(KC)]
        sqb = [sb.alloc(W, BF16) for c in range(KC)]
        h2f = [sb.alloc(W, F32) for c in range(KC)]
        h2b = [sb.alloc(W, BF16) for c in range(KC)]
        tm1 = [sb.alloc(W, F32) for i in range(2)]; tm2 = [sb.alloc(W, F32) for i in range(2)]
        rstd = sb.alloc(W, F32); lnv = sb.alloc(W, F32)
        lgs = sb.alloc(W, F32)
        stg = [sb.alloc(D, BF16) for i in range(2)]
        v3 = lambda t: t[:, :].rearrange("p (c w) -> p c w", c=KC)
        tiles4 = [(0, 256)] + [(LC + 512 * i, 512) for i in range(4)]
        for s in range(NS):
            for ti, (t0, w) in enumerate(tiles4):
                if last and ti == 0:
                    continue
                e3 = 2 if ti == 0 else s
                for (tl, nm) in ((att, "attT"), (rg, "rgT"), (sma, "smaT"), (smr, "smrT")):
                    self.dma("sp", v3(tl)[:, :, :w], dr[nm][s, :, t0:t0 + w].rearrange("(c p) w -> p c w", p=128), writes=[tl])
                self.dma("sp", v3(xin)[:, :, :w], Xin[s, :, t0:t0 + w].rearrange("(c p) w -> p c w", p=128), writes=[xin])
                for m in range(KC):
                    pA = ps[(2 * m) % 4]; pR = ps[(2 * m + 1) % 4]
                    for k in range(KC):
                        self.mm(pA, pA[:, :w], wav[:, k, m * 128:(m + 1) * 128], v3(att)[:, k, :w], k == 0, k == KC - 1, reads=[wa, att])
                    for k in range(KC):
                        self.mm(pR, pR[:, :w], wrv[:, k, m * 128:(m + 1) * 128], v3(rg)[:, k, :w], k == 0, k == KC - 1, reads=[wr, rg])
                    a1 = tm1[m % 2]; a2 = tm2[m % 2]
                    self.tt("dve", a1[:, :w], pA[:, :w], v3(sma)[:, m, :w], ALU.mult, reads=[pA, sma], writes=[a1])
                    self.tt("dve", a2[:, :w], pR[:, :w], v3(smr)[:, m, :w], ALU.mult, reads=[pR, smr], writes=[a2])
                    self.tt("dve", mg[m][:, :w], a1[:, :w], a2[:, :w], ALU.add, reads=[a1, a2], writes=[mg[m]])
                for m in range(KC):
                    pD = ps[4 + (m % 2)]
                    for k in range(KC):
                        self.mm(pD, pD[:, :w], wov[:, k, m * 128:(m + 1) * 128], mg[k][:, :w], k == 0, k == KC - 1, reads=[wo, mg[k]])
                    self.stt(xn[m][:, :w], pD[:, :w], self.mcol(l, 2, m, e3), v3(xin)[:, m, :w], ALU.mult, ALU.add, reads=[pD, self.modc[l], xin], writes=[xn[m]])
                    self.dma("sp", Xout[s, m * 128:(m + 1) * 128, t0:t0 + w], xn[m][:, :w], reads=[xn[m]])
                self.rstd_from(xn, w, sqb, ps[6], rstd, lnv)
                for m in range(KC):
                    tf = tm1[m % 2]
                    self.stt(tf[:, :w], xn[m][:, :w], self.A2[l][:, m * 3 + e3:m * 3 + e3 + 1], rstd[:, :w], ALU.mult, ALU.mult, reads=[xn[m], self.A2[l], rstd], writes=[tf])
                    self.act(h2f[m][:, :w], tf[:, :w], AF.Identity, reads=[tf, self.modc[l]], writes=[h2f[m]], bias=self.mcol(l, 3, m, e3), scale=1.0)
                    self.cp("act", h2b[m][:, :w], h2f[m][:, :w], reads=[h2f[m]], writes=[h2b[m]])
                pL = ps[7]
                for k in range(KC):
                    self.mm(pL, pL[0:NE, :w], wrtv[:, k, :], h2f[k][:, :w], k == 0, k == KC - 1, reads=[wrt, h2f[k]])
                self.cp("act", lgs[0:NE, :w], pL[0:NE, :w], reads=[pL], writes=[lgs])
                self.dma("sp", dr["lgT"][s, :, t0:t0 + w], lgs[0:NE, :w], reads=[lgs])
                for j in range(w // 128):
                    pT = ps[2 + j % 2]
                    pTb = pT[:, :].bitcast(BF16)
                    for m in range(KC):
                        self.tr(pT, pTb[:, m * 128:(m + 1) * 128], h2b[m][:, j * 128:(j + 1) * 128], self.identb[:, :], reads=[h2b[m], self.identb])
                    sg = stg[j % 2]
                    self.cp("act", sg[:, :], pTb[:, 0:D], reads=[pT], writes=[sg])
                    self.dma("pool", dr["h2tm"][s, t0 + j * 128:t0 + (j + 1) * 128, :], sg[:, :], reads=[sg])
        self.phase_end()
        sb.release()

    def p5_route(self, l, last):
        sb, dr, fw, ps = self.sb, self.dr, self.fw, self.ps
        sb.mark()
        bd = sb.alloc(32, F32)
        self.dma("sp", bd[0:32, :], dr["c_bdones"][:, :], writes=[bd])
        lg = sb.alloc(S, F32); E = sb.alloc(S, F32); rs = sb.alloc(S, F32); aff = sb.alloc(S, F32); work = sb.alloc(S, F32)
        m8 = sb.alloc(8, F32); mask = sb.alloc(S, F32); pin = sb.alloc(S, F32); posm = sb.alloc(S, F32); onesf = sb.alloc(S, F32)
        fw.op("dve", lambda e: e.memset(onesf[:, :], 1.0), writes=[onesf])
        groups = [(0, LC, S, CAPL, self.posT_lat)]
        if not last:
            groups.append((1, 0, LC, CAPC, self.posT_ctx))
        P = 32
        for gi, a0, n, cap, posT in groups:
            self.dma("sp", lg[0:P, :n], dr["lgT"][:, :, a0:a0 + n].rearrange("s e t -> (s e) t"), writes=[lg])
            self.act(E[0:P, :n], lg[0:P, :n], AF.Exp, reads=[lg], writes=[E])
            for c0 in range(0, n, 512):
                w = min(512, n - c0)
                pS = ps[(c0 // 512) % 2]
                self.mm(pS, pS[0:P, :w], bd[0:P, 0:P], E[0:P, c0:c0 + w], True, True, reads=[bd, E])
                fw.op("dve", lambda e, c0=c0, w=w, pS=pS: e.reciprocal(out=rs[0:P, c0:c0 + w], in_=pS[0:P, :w]), reads=[pS], pwrites=[rs])
            self.tt("dve", aff[0:P, :n], E[0:P, :n], rs[0:P, :n], ALU.mult, reads=[E, rs], writes=[aff])
            self.cp("dve", work[0:P, :n], aff[0:P, :n], reads=[aff], writes=[work])
            rounds = cap // 8
            for r_ in range(rounds):
                fw.op("dve", lambda e, n=n: e.max(out=m8[0:P, :], in_=work[0:P, :n]), reads=[work], writes=[m8])
                if r_ < rounds - 1:
                    fw.op("dve", lambda e, n=n: e.match_replace(out=work[0:P, :n], in_to_replace=m8[0:P, :], in_values=work[0:P, :n], imm_value=-1.0),
                          reads=[m8, work], writes=[work])
            self.ts("dve", mask[0:P, :n], aff[0:P, :n], m8[0:P, 7:8], ALU.is_ge, reads=[aff, m8], writes=[mask])
            fw.op("dve", lambda e, n=n: e.tensor_tensor_scan(out=pin[0:P, :n], data0=onesf[0:P, :n], data1=mask[0:P, :n], initial=0.0, op0=ALU.mult, op1=ALU.add),
                  reads=[onesf, mask], writes=[pin])
            self.tt("dve", pin[0:P, :n], pin[0:P, :n], mask[0:P, :n], ALU.mult, reads=[pin, mask], writes=[pin])
            self.ts("dve", posm[0:P, :n], pin[0:P, :n], -1.0, ALU.add, reads=[pin], writes=[posm])
            self.dma("sp", dr["rt"][gi, :, 0, 0:n], aff[0:P, :n], reads=[aff])
            self.dma("sp", dr["rt"][gi, :, 1, 0:n], posm[0:P, :n], reads=[posm])
            self.dma("sp", dr["rt"][gi, :, 2, 0:n], mask[0:P, :n], reads=[mask])
            pT = ps[2]
            ntc = n // 128
            for tc in range(ntc):
                self.tr(pT, pT[:, tc * 32:(tc + 1) * 32], posm[0:P, tc * 128:(tc + 1) * 128], self.ident[0:P, 0:P], reads=[posm, self.ident])
            self.cp("act", posT[:, 0:ntc * 32], pT[:, 0:ntc * 32], reads=[pT], writes=[posT])
        self.phase_end()
        sb.release()

    def p6_experts(self, l, last):
        self.p6a_gather(l, last)
        self.p6b_ffn(l, last)

    def p6a_gather(self, l, last):
        sb, dr, fw, ps = self.sb, self.dr, self.fw, self.ps
        sb.mark()
        NCX = 0 if last else 2 * CAPC
        NX = 512 + NCX
        iota_f = sb.alloc(256, F32)
        self.dma("sp", iota_f[:, :], dr["c_iota_f"][:, :], writes=[iota_f])
        h2l = []
        h2c = []
        for s in range(NS):
            t_ = sb.alloc(16 * D, BF16, name="h2l")
            tv = t_[:, :].rearrange("p (c d) -> p c d", c=16)
            for q4 in range(4):
                self.dma("sp", tv[:, q4 * 4:(q4 + 1) * 4, :], dr["h2tm"][s, LC + q4 * 512:LC + (q4 + 1) * 512, :].rearrange("(c p) d -> p c d", p=128), pwrites=[t_])
            h2l.append((t_, tv))
            if not last:
                c_ = sb.alloc(2 * D, BF16, name="h2c")
                cv_ = c_[:, :].rearrange("p (c d) -> p c d", c=2)
                self.dma("sp", cv_, dr["h2tm"][s, 0:LC, :].rearrange("(c p) d -> p c d", p=128), writes=[c_])
                h2c.append((c_, cv_))
        xeT = [[sb.alloc(NX, BF16) for k in range(KC)] for i in range(2)]
        sel = [[sb.alloc(256, BF16) for i in range(16)] for j in range(2)]
        selc = [sb.alloc(32, BF16) for i in range(2)]
        it = 0
        for e in range(NE):
            xb = xeT[e % 2]
            for s in range(NS):
                col = s * NE + e
                sl = sel[it % 2]
                it += 1
                for tc in range(16):
                    self.ts("dve", sl[tc][:, :], iota_f[:, 0:256], self.posT_lat[:, tc * 32 + col:tc * 32 + col + 1], ALU.is_equal,
                            reads=[iota_f, self.posT_lat], writes=[sl[tc]])
                for m in range(KC):
                    pX = ps[m % 4]
                    for tc in range(16):
                        self.mm(pX, pX[:, :256], h2l[s][1][:, tc, m * 128:(m + 1) * 128], sl[tc][:, :], tc == 0, tc == 15, reads=[h2l[s][0], sl[tc]])
                    self.cp("act", xb[m][:, s * 256:(s + 1) * 256], pX[:, :256], reads=[pX], pwrites=[xb[m]])
                if NCX:
                    for tc in range(2):
                        self.ts("dve", selc[tc][:, :], iota_f[:, 0:32], self.posT_ctx[:, tc * 32 + col:tc * 32 + col + 1], ALU.is_equal,
                                reads=[iota_f, self.posT_ctx], writes=[selc[tc]])
                    for m in range(KC):
                        pX = ps[4 + m % 4]
                        for tc in range(2):
                            self.mm(pX, pX[:, :32], h2c[s][1][:, tc, m * 128:(m + 1) * 128], selc[tc][:, :], tc == 0, tc == 1, reads=[h2c[s][0], selc[tc]])
                        self.cp("act", xb[m][:, 512 + s * 32:512 + (s + 1) * 32], pX[:, :32], reads=[pX], pwrites=[xb[m]])
            for m in range(KC):
                self.dma("sp", dr["xe"][e, m * 128:(m + 1) * 128, 0:NX], xb[m][:, :], reads=[xb[m]])
        self.phase_end()
        sb.release()

    def p6b_ffn(self, l, last):
        sb, dr, fw, ps = self.sb, self.dr, self.fw, self.ps
        sb.mark()
        NCX = 0 if last else 2 * CAPC
        NX = 512 + NCX
        NR = 4
        wring = [sb.alloc(8192, BF16, name="wring%d" % i) for i in range(NR)]
        xeb = [sb.alloc(KC * NX, BF16) for i in range(2)]
        hid = [sb.alloc(NX, BF16) for f in range(16)]
        sgt = [sb.alloc(512, F32) for i in range(2)]
        sgc = [sb.alloc(64, F32) for i in range(2)]
        yst = [sb.alloc(512, BF16) for i in range(2)]
        units = []
        for e in range(NE):
            for fh in range(2):
                units.append(("w_gate", e, fh)); units.append(("w_up", e, fh))
            for dh in range(2):
                units.append(("w_down", e, dh))
        loaded = {}

        def issue(ui):
            if ui >= len(units) or ui in loaded:
                return
            nm, e, hh = units[ui]
            wt = wring[ui % NR]
            if nm == "w_down":
                self.dma("pool", wt[:, :].rearrange("p (c n) -> p c n", c=16), dr[nm][l, e, :, hh * 512:(hh + 1) * 512].rearrange("(c p) n -> p c n", p=128), writes=[wt])
            else:
                self.dma("pool", wt[:, :].rearrange("p (c n) -> p c n", c=KC), dr[nm][l, e, :, hh * 1024:(hh + 1) * 1024].rearrange("(c p) n -> p c n", p=128), writes=[wt])
            loaded[ui] = wt

        for ui in range(NR - 1):
            issue(ui)
        ui = 0
        yi = 0
        for e in range(NE):
            xt_ = xeb[e % 2]
            xv = xt_[:, :].rearrange("p (k n) -> p k n", k=KC)
            self.dma("sp", xv, dr["xe"][e, :, 0:NX].rearrange("(k p) n -> p k n", p=128), writes=[xt_])
            for fh in range(2):
                for uj in range(ui, ui + NR):
                    issue(uj)
                wg = loaded[ui]; wu = loaded[ui + 1]; ui += 2
                wgv = wg[:, :].rearrange("p (c n) -> p c n", c=KC)
                wuv = wu[:, :].rearrange("p (c n) -> p c n", c=KC)
                for f in range(8):
                    fi = fh * 8 + f
                    pG, pU, pGc, pUc = ps[fi % 2], ps[2 + fi % 2], ps[4], ps[5]
                    for k in range(KC):
                        self.mm(pG, pG[:, :512], wgv[:, k, f * 128:(f + 1) * 128], xv[:, k, 0:512], k == 0, k == KC - 1, reads=[wg, xt_])
                    for k in range(KC):
                        self.mm(pU, pU[:, :512], wuv[:, k, f * 128:(f + 1) * 128], xv[:, k, 0:512], k == 0, k == KC - 1, reads=[wu, xt_])
                    if NCX:
                        for k in range(KC):
                            self.mm(pGc, pGc[:, :NCX], wgv[:, k, f * 128:(f + 1) * 128], xv[:, k, 512:NX], k == 0, k == KC - 1, reads=[wg, xt_])
                        for k in range(KC):
                            self.mm(pUc, pUc[:, :NCX], wuv[:, k, f * 128:(f + 1) * 128], xv[:, k, 512:NX], k == 0, k == KC - 1, reads=[wu, xt_])
                    sg = sgt[fi % 2]
                    self.act(sg[:, :], pG[:, :512], AF.Silu, reads=[pG], writes=[sg])
                    self.tt("dve", hid[fi][:, 0:512], sg[:, :], pU[:, :512], ALU.mult, reads=[sg, pU], pwrites=[hid[fi]])
                    if NCX:
                        sc_ = sgc[fi % 2]
                        self.act(sc_[:, :], pGc[:, :NCX], AF.Silu, reads=[pGc], writes=[sc_])
                        self.tt("dve", hid[fi][:, 512:NX], sc_[:, :], pUc[:, :NCX], ALU.mult, reads=[sc_, pUc], pwrites=[hid[fi]])
            for dh in range(2):
                for uj in range(ui, ui + NR):
                    issue(uj)
                wd = loaded[ui]; ui += 1
                wdv = wd[:, :].rearrange("p (c n) -> p c n", c=16)
                rgs = [(s * 256 + c * 128, 128, ("ye", s, c)) for s in range(NS) for c in range(2)]
                if NCX:
                    rgs.append((512, NCX, ("yec",)))
                for (c0, M, dst) in rgs:
                    pY = ps[6 + yi % 2]
                    ys = yst[yi % 2]
                    yi += 1
                    for f in range(16):
                        self.mm(pY, pY[0:M, :512], hid[f][:, c0:c0 + M], wdv[:, f, :], f == 0, f == 15, reads=[hid[f], wd])
                    self.cp("act", ys[0:M, :], pY[0:M, :512], reads=[pY], writes=[ys])
                    if dst[0] == "ye":
                        self.dma("sp", dr["ye"][dst[1], e, dst[2] * 128:(dst[2] + 1) * 128, dh * 512:(dh + 1) * 512], ys[0:128, :], reads=[ys])
                    else:
                        for s in range(NS):
                            self.dma("sp", dr["yec"][s, e, :, dh * 512:(dh + 1) * 512], ys[s * CAPC:(s + 1) * CAPC, :], reads=[ys])
        self.phase_end()
        sb.release()

    def p7_scatter(self, l, Xm, Xout, last):
        sb, dr, fw, ps = self.sb, self.dr, self.fw, self.ps
        sb.mark()
        W = 512
        rowsel = sb.alloc(32 * 128, BF16)
        self.dma("pool", rowsel[0:32, :], dr["c_rowsel"][:, :], writes=[rowsel])
        iota_p = sb.alloc(2, F32)
        self.dma("sp", iota_p[:, :], dr["c_iota_p"][:, :], writes=[iota_p])
        gfin = sb.alloc(KC, F32)
        self.dma("sp", gfin[:, :], dr["gfinT"][:, :], writes=[gfin])
        affr = sb.alloc(S, BF16); posr = sb.alloc(S, BF16)
        yeall = sb.alloc(NE * 2 * D, BF16, name="yeall")
        selT = [sb.alloc(W, BF16) for i in range(32)]
        affb = [sb.alloc(W, F32) for i in range(2)]
        xin = sb.alloc(KC * W, F32)
        xn = [sb.alloc(W, F32) for c in range(KC)]
        sqb = [sb.alloc(W, BF16) for c in range(KC)]
        ot = [sb.alloc(W, F32) for i in range(2)]
        rstd = sb.alloc(W, F32); lnv = sb.alloc(W, F32)
        groups = [(0, LC, S, 128, 2)]
        if not last:
            groups.append((1, 0, LC, CAPC, 1))
        v3 = lambda t: t[:, :].rearrange("p (c w) -> p c w", c=KC)
        for gi, a0, n, NP, ncc in groups:
            self.dma("pool", affr[0:32, :n], dr["rt"][gi, :, 0, 0:n], writes=[affr])
            self.dma("pool", posr[0:32, :n], dr["rt"][gi, :, 1, 0:n], writes=[posr])
            for s in range(NS):
                e3 = s if gi == 0 else 2
                if gi == 0:
                    yv = yeall[:, :].rearrange("p (e c d) -> p e c d", e=NE, c=2)
                    for q8 in range(8):
                        self.dma("sp", yv[:, q8 * 2:(q8 + 1) * 2, :, :], dr["ye"][s, q8 * 2:(q8 + 1) * 2, :, :].rearrange("e (c p) d -> p e c d", p=128), pwrites=[yeall])
                else:
                    yv = yeall[:, 0:NE * D].rearrange("p (e c d) -> p e c d", e=NE, c=1)
                    self.dma("sp", yv[0:CAPC, :, 0, :], dr["yec"][s].rearrange("e j d -> j e d"), writes=[yeall])
                for c0 in range(0, n, W):
                    w = min(W, n - c0)
                    for e in range(NE):
                        r_ = s * NE + e
                        pP = ps[e % 2]; pA = ps[2 + e % 2]
                        self.mm(pP, pP[:, :w], rowsel[0:32, r_ * 128:(r_ + 1) * 128], posr[0:32, c0:c0 + w], True, True, reads=[rowsel, posr])
                        self.mm(pA, pA[:, :w], rowsel[0:32, r_ * 128:(r_ + 1) * 128], affr[0:32, c0:c0 + w], True, True, reads=[rowsel, affr])
                        ab = affb[e % 2]
                        self.cp("act", ab[:, :w], pA[:, :w], reads=[pA], writes=[ab])
                        for cc in range(ncc):
                            st_ = selT[e * 2 + cc]
                            self.stt(st_[0:NP, :w], pP[0:NP, :w], iota_p[0:NP, cc:cc + 1], ab[0:NP, :w], ALU.is_equal, ALU.mult, reads=[pP, iota_p, ab], writes=[st_])
                    self.dma("sp", v3(xin)[:, :, :w], Xm[s, :, a0 + c0:a0 + c0 + w].rearrange("(c p) w -> p c w", p=128), writes=[xin])
                    for m in range(KC):
                        pY = ps[4 + m % 2]
                        nmm = NE * ncc
                        idx = 0
                        for e in range(NE):
                            for cc in range(ncc):
                                self.mm(pY, pY[:, :w], yv[0:NP, e, cc, m * 128:(m + 1) * 128], selT[e * 2 + cc][0:NP, :w], idx == 0, idx == nmm - 1, reads=[yeall, selT[e * 2 + cc]])
                                idx += 1
                        self.stt(xn[m][:, :w], pY[:, :w], self.mcol(l, 5, m, e3), v3(xin)[:, m, :w], ALU.mult, ALU.add, reads=[pY, self.modc[l], xin], writes=[xn[m]])
                        if not last:
                            self.dma("sp", Xout[s, m * 128:(m + 1) * 128, a0 + c0:a0 + c0 + w], xn[m][:, :w], reads=[xn[m]])
                    if last:
                        self.rstd_from(xn, w, sqb, ps[6], rstd, lnv)
                        for m in range(KC):
                            o_ = ot[m % 2]
                            self.stt(o_[:, :w], xn[m][:, :w], gfin[:, m:m + 1], rstd[:, :w], ALU.mult, ALU.mult, reads=[xn[m], gfin, rstd], writes=[o_])
                            self.dma("sp", dr["outT"][s, m * 128:(m + 1) * 128, c0:c0 + w], o_[:, :w], reads=[o_])
        self.phase_end()
        sb.release()

def prep_shared(inp):
    f = lambda a: np.ascontiguousarray(a, dtype=np.float32)
    sh = {}
    sh["ada_w"] = f(inp["ada_w"])
    sh["ada_bT"] = f(inp["ada_b"].reshape(DEPTH, 48, 128).transpose(0, 2, 1))
    sh["gmixT"] = f(inp["norm_mix_g"].reshape(DEPTH, KC, 128).transpose(0, 2, 1))
    sh["gffnT"] = f(inp["norm_ffn_g"].reshape(DEPTH, KC, 128).transpose(0, 2, 1))
    sh["gfinT"] = f(inp["final_norm_g"].reshape(KC, 128).T)
    sh["w_in"] = f(inp["w_in"])
    sh["sink"] = f(inp["attn_sink"].reshape(DEPTH, 1, 8))
    cw = np.concatenate([inp["conv_w"], inp["conv_b"][:, None, :]], axis=1)
    sh["convT"] = f(cw.reshape(DEPTH, 5, KC, 128).transpose(0, 3, 1, 2))
    sh["lru_w"] = f(np.stack([inp["lru_wa"], inp["lru_wx"]], axis=1))
    lv = np.stack([inp["lru_ba"], inp["lru_bx"], inp["lru_lambda"]], axis=1)
    sh["lru_vT"] = f(lv.reshape(DEPTH, 3, 2, KC, 128).transpose(0, 4, 1, 2, 3))
    for k in ("w_attn_br", "w_rec_br", "w_out", "w_router", "w_gate", "w_up", "w_down"):
        sh[k] = f(inp[k])
    for k, v in host_consts().items():
        sh["c_" + k] = f(v)
    return sh


def prep_core(inp, core):
    b0 = core * NS
    xs = inp["x"][b0:b0 + NS]
    cs = inp["ctx"][b0:b0 + NS]
    xT = np.concatenate([cs, xs], axis=1).transpose(0, 2, 1)
    cc = np.stack([inp["c"][b0], inp["c"][b0 + 1], inp["c_ctx"]], axis=1)
    return {"xT": np.ascontiguousarray(xT, dtype=np.float32),
            "cT": np.ascontiguousarray(cc.reshape(KC, 128, 3).transpose(1, 0, 2), dtype=np.float32)}


_NC_CACHE = {}


def kernel(**inputs):
    inp = {k: np.asarray(v) for k, v in inputs.items()}
    n = 8
    if "nc" not in _NC_CACHE:
        kb = KB()
        _NC_CACHE["nc"] = kb.build()
        _NC_CACHE["names"] = set(kb.dr.keys())
    nc = _NC_CACHE["nc"]
    sh = prep_shared(inp)
    in_maps = []
    for core in range(n):
        m = dict(sh)
        m.update(prep_core(inp, core))
        in_maps.append({k: v for k, v in m.items() if k in _NC_CACHE["names"]})
    res = run_bass_kernel_spmd(nc, in_maps, core_ids=list(range(n)))
    outs = [np.asarray(r["outT"]).transpose(0, 2, 1) for r in res.results]
    return np.ascontiguousarray(np.concatenate(outs, axis=0), dtype=np.float32)
```

```python
import numpy as np
import concourse.bass as bass
import concourse.mybir as mybir

F32 = mybir.dt.float32
BF16 = mybir.dt.bfloat16
I32 = mybir.dt.int32
AF = mybir.ActivationFunctionType
ALU = mybir.AluOpType
AX = mybir.AxisListType
from concourse.bass_utils import run_bass_kernel_spmd
ENGS = ("pe", "act", "dve", "pool", "sp")


class Op:
    __slots__ = ("eng", "emit", "deps", "dma_deps", "signal", "count", "is_dma", "dsem", "dval", "prev_dval", "idx")
    _ctr = 0

    def __init__(self, eng, emit, is_dma=False):
        self.eng = eng
        self.emit = emit
        self.deps = {}
        self.dma_deps = []
        self.signal = False
        self.count = 0
        self.is_dma = is_dma
        self.dsem = None
        self.dval = 0
        self.prev_dval = 0
        Op._ctr += 1
        self.idx = Op._ctr


class Tile:
    def __init__(self, ap, name=""):
        self.ap = ap
        self.name = name
        self.w_c = {}
        self.w_d = []
        self.r_c = {}
        self.r_d = []
        self.g_c = {}
        self.g_d = []

    def __getitem__(self, k):
        return self.ap[k]


def _merge(dst_c, dst_d, src_c, src_d):
    for e, o in src_c.items():
        if e not in dst_c or dst_c[e].idx < o.idx:
            dst_c[e] = o
    for o in src_d:
        if o not in dst_d:
            dst_d.append(o)


class FW:
    def __init__(self, nc, n_dma_sems=4):
        self.nc = nc
        self.ops = {e: [] for e in ENGS}
        self.sem = {}
        self.dma_pool = {}
        self.dma_rr = {}
        self.n_dma_sems = n_dma_sems
        self.out_dmas = []
        self.last = {e: None for e in ENGS}
        self._cms = []

    def setup_sems(self, es):
        nc = self.nc
        for e in ENGS:
            self.sem[e] = es.enter_context(nc.semaphore("s_" + e))
        for q in ("sp", "act", "pool"):
            self.dma_pool[q] = [[es.enter_context(nc.semaphore("d_%s%d" % (q, i))), 0] for i in range(self.n_dma_sems)]
            self.dma_rr[q] = 0

    def _add_read(self, op, t):
        _merge(op.deps, op.dma_deps, t.w_c, t.w_d)
        if op.is_dma:
            t.r_d.append(op)
        else:
            t.r_c[op.eng] = op

    def _add_write(self, op, t, partial):
        if (not partial) or t.r_c or t.r_d:
            t.g_c = {}
            t.g_d = []
            _merge(t.g_c, t.g_d, t.r_c, t.r_d)
            _merge(t.g_c, t.g_d, t.w_c, t.w_d)
            t.r_c, t.r_d, t.w_c, t.w_d = {}, [], {}, []
        _merge(op.deps, op.dma_deps, t.g_c, t.g_d)
        if op.is_dma:
            t.w_d.append(op)
        else:
            t.w_c[op.eng] = op

    def _record(self, o, reads, writes, pwrites):
        wset = set(id(t) for t in writes) | set(id(t) for t in pwrites)
        for t in reads:
            _merge(o.deps, o.dma_deps, t.w_c, t.w_d)
        for t in writes:
            self._add_write(o, t, False)
        for t in pwrites:
            self._add_write(o, t, True)
        for t in reads:
            if id(t) not in wset:
                if o.is_dma:
                    t.r_d.append(o)
                else:
                    t.r_c[o.eng] = o

    def op(self, eng, emit, reads=(), writes=(), pwrites=()):
        o = Op(eng, emit)
        self._record(o, reads, writes, pwrites)
        self.ops[eng].append(o)
        self.last[eng] = o
        return o

    def dma(self, q, out, in_, reads=(), writes=(), pwrites=(), **kw):
        o = Op(q, (lambda eng: eng.dma_start(out=out, in_=in_, **kw)), is_dma=True)
        pool = self.dma_pool[q]
        i = self.dma_rr[q]
        self.dma_rr[q] = (i + 1) % len(pool)
        o.dsem = pool[i][0]
        o.prev_dval = pool[i][1]
        pool[i][1] += 16
        o.dval = pool[i][1]
        self._record(o, reads, writes, pwrites)
        self.ops[q].append(o)
        self.out_dmas.append(o)
        return o

    def barrier(self):
        lasts = {e: o for e, o in self.last.items() if o is not None}
        for e in ENGS:
            o = Op(e, None)
            for e2, o2 in lasts.items():
                if e2 != e:
                    o.deps[e2] = o2
            o.dma_deps = list(self.out_dmas)
            self.ops[e].append(o)
        self.out_dmas = []

    def finalize(self):
        for e in ENGS:
            for o in self.ops[e]:
                for e2, p in o.deps.items():
                    if p.is_dma:
                        continue
                    if e2 == e and e == "pe":
                        continue
                    p.signal = True
        for e in ENGS:
            c = 0
            for o in self.ops[e]:
                if o.is_dma or o.emit is None:
                    continue
                if o.signal:
                    c += 1
                    o.count = c
        nc = self.nc
        engmap = {"pe": "tensor", "act": "scalar", "dve": "vector", "pool": "gpsimd", "sp": "sync"}
        stats = {}
        with nc.Block() as block:
            for e in ENGS:
                def body(engine, e=e):
                    waited = {}
                    nw = 0

                    def wait(sem, val):
                        nonlocal nw
                        k = id(sem)
                        if waited.get(k, 0) >= val:
                            return
                        waited[k] = val
                        engine.wait_ge(sem, val)
                        nw += 1

                    for o in self.ops[e]:
                        for e2, p in o.deps.items():
                            if p.is_dma:
                                wait(p.dsem, p.dval)
                                continue
                            if e2 == e and e == "pe":
                                continue
                            if p is o:
                                continue
                            wait(self.sem[e2], p.count)
                        for p in o.dma_deps:
                            wait(p.dsem, p.dval)
                        if o.emit is None:
                            continue
                        if o.is_dma:
                            if o.prev_dval > 0:
                                wait(o.dsem, o.prev_dval)
                            ins = o.emit(engine)
                            ins.then_inc(o.dsem, 16)
                        else:
                            ins = o.emit(engine)
                            if o.signal:
                                ins.then_inc(self.sem[e], 1)
                    stats[e] = (len(self.ops[e]), nw)
                getattr(block, engmap[e])(body)
        return stats


class Sbuf:
    def __init__(self, big, nwords):
        self.big = big
        self.n = nwords
        self.off = 0
        self.marks = []

    def mark(self):
        self.marks.append(self.off)

    def release(self):
        self.off = self.marks.pop()

    def alloc(self, nelem, dtype=F32, parts=128, name=""):
        if dtype == BF16:
            nw = (nelem + 1) // 2
        else:
            nw = nelem
        nw = (nw + 7) // 8 * 8
        assert self.off + nw <= self.n, "SBUF overflow %s: %d + %d > %d" % (name, self.off, nw, self.n)
        ap = self.big[0:parts, self.off:self.off + nw]
        self.off += nw
        if dtype == BF16:
            ap = ap.bitcast(BF16)[:, 0:nelem]
        elif dtype != F32:
            ap = ap.bitcast(dtype)[:, 0:nelem]
        else:
            ap = ap[:, 0:nelem]
        return Tile(ap, name)

from contextlib import ExitStack
import ml_dtypes

D = 1024
NS = 2
LC = 256
S = 2048
T = LC + S
DEPTH = 2
NE = 16
DEXP = 2048
IN_W = 5632
CAPL = 256
CAPC = 32
EPS = 1e-6
KC = 8

SB_WORDS = 46080


def host_consts():
    c = {}
    half = 32
    freqs = (10000.0 ** (-np.arange(half, dtype=np.float32) / half)).astype(np.float32)
    t = np.arange(S)
    rows = (t // 64).astype(np.float32)
    cols = (t % 64).astype(np.float32)
    C = np.zeros((128, S), np.float32)
    Sn = np.zeros((128, S), np.float32)
    for d in range(128):
        pos = rows if d < 64 else cols
        j = (d % 64) % 32
        ang = (pos * freqs[j]).astype(np.float32)
        C[d] = np.cos(ang)
        Sn[d] = np.sin(ang)
    c["ropeC"] = C
    c["ropeS"] = Sn
    Pm = np.zeros((128, 128), np.float32)
    for d in range(128):
        i = d % 64
        if i < 32:
            Pm[d + 32, d] = -1.0
        else:
            Pm[d - 32, d] = 1.0
    c["pm"] = Pm
    c["ident"] = np.eye(128, dtype=np.float32)
    kk = np.arange(128)[:, None]
    qq = np.arange(128)[None, :]
    mprev = (qq <= kk).astype(np.float32)
    mnext = (kk <= qq).astype(np.float32)
    c["mprev"] = np.tile(mprev, (1, 4))
    c["mnext"] = np.tile(mnext, (1, 4))
    c["iota_f"] = np.tile(np.arange(256, dtype=np.float32)[None, :], (128, 1))
    c["iota_p"] = np.stack([np.arange(128, dtype=np.float32), np.arange(128, dtype=np.float32) + 128], axis=1)
    sel = np.zeros((32, 32, 128), np.float32)
    for r in range(32):
        sel[r, r, :] = 1.0
    c["rowsel"] = sel.transpose(1, 0, 2).reshape(32, 32 * 128)
    bd = np.zeros((32, 32), np.float32)
    bd[:16, :16] = 1.0
    bd[16:, 16:] = 1.0
    c["bdones"] = bd
    return c


CONST_SHAPES = {"ropeC": (128, S), "ropeS": (128, S), "pm": (128, 128), "ident": (128, 128),
                "mprev": (128, 512), "mnext": (128, 512), "iota_f": (128, 256), "iota_p": (128, 2),
                "rowsel": (32, 32 * 128), "bdones": (32, 32)}


class KB:
    def __init__(self, debug=None, stop_after=None):
        self.debug = debug or []
        self.stop_after = stop_after
        self.nc = bass.Bass("TRN2", target_bir_lowering=False)
        self.dr = {}

    def din(self, name, shape, dt=F32):
        self.dr[name] = self.nc.dram_tensor(name, list(shape), dt, kind="ExternalInput").ap()
        return self.dr[name]

    def dscr(self, name, shape, dt=F32, out=False):
        kind = "ExternalOutput" if (out or name in self.debug) else "Internal"
        self.dr[name] = self.nc.dram_tensor(name, list(shape), dt, kind=kind).ap()
        return self.dr[name]

    def mm(self, pst, out, lhsT, rhs, start, stop, reads):
        self.fw.op("pe", lambda e: e.matmul(out, lhsT=lhsT, rhs=rhs, start=start, stop=stop), reads=reads, pwrites=[pst])

    def tr(self, pst, out, in_, ident, reads):
        self.fw.op("pe", lambda e: e.transpose(out, in_, ident), reads=reads, pwrites=[pst])

    def act(self, out, in_, func, reads, writes=(), pwrites=(), **kw):
        self.fw.op("act", lambda e: e.activation(out=out, in_=in_, func=func, **kw), reads=reads, writes=writes, pwrites=pwrites)

    def tt(self, eng, out, in0, in1, op, reads, writes=(), pwrites=()):
        self.fw.op(eng, lambda e: e.tensor_tensor(out=out, in0=in0, in1=in1, op=op), reads=reads, writes=writes, pwrites=pwrites)

    def ts(self, eng, out, in0, s1, op0, reads, s2=None, op1=None, writes=(), pwrites=()):
        if op1 is None:
            self.fw.op(eng, lambda e: e.tensor_scalar(out=out, in0=in0, scalar1=s1, scalar2=None, op0=op0), reads=reads, writes=writes, pwrites=pwrites)
        else:
            self.fw.op(eng, lambda e: e.tensor_scalar(out=out, in0=in0, scalar1=s1, scalar2=s2, op0=op0, op1=op1), reads=reads, writes=writes, pwrites=pwrites)

    def stt(self, out, in0, scalar, in1, op0, op1, reads, writes=(), pwrites=()):
        self.fw.op("dve", lambda e: e.scalar_tensor_tensor(out=out, in0=in0, scalar=scalar, in1=in1, op0=op0, op1=op1), reads=reads, writes=writes, pwrites=pwrites)

    def cp(self, eng, out, in_, reads, writes=(), pwrites=()):
        if eng == "act":
            self.fw.op("act", lambda e: e.copy(out=out, in_=in_), reads=reads, writes=writes, pwrites=pwrites)
        else:
            self.fw.op(eng, lambda e: e.tensor_copy(out=out, in_=in_), reads=reads, writes=writes, pwrites=pwrites)

    def dma(self, q, out, in_, reads=(), writes=(), pwrites=(), maxdesc=512):
        shp = tuple(out.shape)
        if len(shp) == 3 and shp[0] * shp[1] > maxdesc and tuple(in_.shape) == shp:
            step = max(1, maxdesc // shp[0])
            first = True
            for i0 in range(0, shp[1], step):
                i1 = min(shp[1], i0 + step)
                if first:
                    self.fw.dma(q, out[:, i0:i1, :], in_[:, i0:i1, :], reads=reads, writes=writes, pwrites=pwrites)
                    first = False
                else:
                    self.fw.dma(q, out[:, i0:i1, :], in_[:, i0:i1, :], reads=reads, pwrites=tuple(writes) + tuple(pwrites))
            return
        self.fw.dma(q, out, in_, reads=reads, writes=writes, pwrites=pwrites)

    def phase_end(self):
        self.fw.barrier()

    def build(self):
        nc = self.nc
        din, dscr = self.din, self.dscr
        din("xT", (NS, D, T))
        din("cT", (128, KC, 3))
        din("ada_w", (DEPTH, D, 6 * D))
        din("ada_bT", (DEPTH, 128, 48))
        din("gmixT", (DEPTH, 128, KC))
        din("gffnT", (DEPTH, 128, KC))
        din("gfinT", (128, KC))
        din("w_in", (DEPTH, D, IN_W))
        din("sink", (DEPTH, 1, 8))
        din("convT", (DEPTH, 128, 5, KC))
        din("lru_w", (DEPTH, 2, 2, 8, 128, 128))
        din("lru_vT", (DEPTH, 128, 3, 2, KC))
        din("w_attn_br", (DEPTH, D, D))
        din("w_rec_br", (DEPTH, D, D))
        din("w_out", (DEPTH, D, D))
        din("w_router", (DEPTH, D, NE))
        if self.stop_after not in ("p0", "p1", "p2", "p3", "p4", "p5", "wip"):
            din("w_gate", (DEPTH, NE, D, DEXP))
            din("w_up", (DEPTH, NE, D, DEXP))
            din("w_down", (DEPTH, NE, DEXP, D))
        for k, shp in CONST_SHAPES.items():
            din("c_" + k, shp)
        dscr("outT", (NS, D, S), out=True)
        dscr("X1", (NS, D, T)); dscr("X2", (NS, D, T)); dscr("X3", (NS, D, T))
        dscr("qT", (NS, D, T), BF16); dscr("kT", (NS, 256, T), BF16); dscr("vtm", (NS, T, 256), BF16)
        dscr("uT", (NS, D, T)); dscr("gzT", (NS, D, T), BF16); dscr("smaT", (NS, D, T), BF16); dscr("smrT", (NS, D, T), BF16)
        dscr("attT", (NS, D, T), BF16); dscr("rgT", (NS, D, T), BF16)
        dscr("h2tm", (NS, T, D), BF16); dscr("lgT", (NS, NE, T))
        dscr("ye", (NS, NE, CAPL, D), BF16); dscr("yec", (NS, NE, CAPC, D), BF16); dscr("xe", (NE, D, 512 + 2 * CAPC), BF16)
        dscr("modc", (DEPTH, 128, 6, KC, 3))
        dscr("rt", (2, 32, 3, S))

        with ExitStack() as es:
            big = es.enter_context(nc.sbuf_tensor("big", [128, SB_WORDS], F32))
            pst = [es.enter_context(nc.psum_tensor("ps%d" % i, [128, 512], F32)) for i in range(8)]
            self.fw = FW(nc)
            self.fw.setup_sems(es)
            self.sb = Sbuf(big, SB_WORDS)
            self.ps = [Tile(p, "ps%d" % i) for i, p in enumerate(pst)]
            self._body()
            self.fw.barrier()
            self.stats = self.fw.finalize()
        return nc

    def _body(self):
        sb = self.sb
        dr = self.dr
        fw = self.fw
        self.ident = sb.alloc(128, F32, name="ident")
        self.dma("sp", self.ident[:, :], dr["c_ident"][:, :], writes=[self.ident])
        self.identb = sb.alloc(128, BF16, name="identb")
        self.dma("pool", self.identb[:, :], dr["c_ident"][:, :], writes=[self.identb])
        self.pmb = sb.alloc(128, BF16, name="pmb")
        self.dma("pool", self.pmb[:, :], dr["c_pm"][:, :], writes=[self.pmb])
        self.onesb = sb.alloc(128, BF16, name="onesb")
        fw.op("dve", lambda e: e.memset(self.onesb[:, :], 1.0), writes=[self.onesb])
        self.epsc = sb.alloc(1, F32, name="epsc")
        fw.op("dve", lambda e: e.memset(self.epsc[:, :], EPS), writes=[self.epsc])
        self.silu_c = sb.alloc(KC * 3, F32, name="silu_c")
        ctmp = sb.alloc(KC * 3, F32, name="ctmp")
        self.dma("sp", ctmp[:, :], dr["cT"].rearrange("p c e -> p (c e)"), writes=[ctmp])
        self.act(self.silu_c[:, :], ctmp[:, :], AF.Silu, reads=[ctmp], writes=[self.silu_c])
        self.modc = [sb.alloc(6 * KC * 3, F32, name="modc%d" % l) for l in range(DEPTH)]
        self.A1 = [sb.alloc(KC * 3, F32) for l in range(DEPTH)]
        self.A2 = [sb.alloc(KC * 3, F32) for l in range(DEPTH)]
        self.posT_lat = sb.alloc(16 * 32, F32, name="posT_lat")
        self.posT_ctx = sb.alloc(2 * 32, F32, name="posT_ctx")
        self.base_off = sb.off
        Xs = ["xT", "X1", "X2", "X3"]
        for l in range(DEPTH):
            last = l == DEPTH - 1
            self.p0_mod(l)
            if self.stop_after == "p0":
                return
            self.p1_inproj(l, dr["xT"] if l == 0 else dr["X2"])
            if self.stop_after == "p1":
                return
            self.p2_attn(l, last)
            if self.stop_after == "p2":
                return
            self.p3_rec(l)
            if self.stop_after == "p3":
                return
            if self.stop_after == "wip_DISABLED":
                self.pF_copy()
                return
            self.p4_merge(l, dr["xT"] if l == 0 else dr["X2"], dr["X1"], last)
            if self.stop_after == "p4":
                return
            self.p5_route(l, last)
            if self.stop_after == "p5":
                return
            self.p6_experts(l, last)
            if self.stop_after == "p6":
                return
            self.p7_scatter(l, dr["X1"], dr["X2"] if not last else None, last)
            if self.stop_after == "p7":
                return

    def p0_mod(self, l):
        sb, dr, fw, ps = self.sb, self.dr, self.fw, self.ps
        sb.mark()
        NB = 12
        wbuf = [sb.alloc(KC * 512, F32, name="adaw%d" % i) for i in range(2)]
        adab = sb.alloc(48, F32)
        gm = sb.alloc(KC, F32)
        gf = sb.alloc(KC, F32)
        self.dma("sp", adab[:, :], dr["ada_bT"][l], writes=[adab])
        self.dma("sp", gm[:, :], dr["gmixT"][l], writes=[gm])
        self.dma("sp", gf[:, :], dr["gffnT"][l], writes=[gf])
        pt = ps[0]
        modc = self.modc[l]
        for nb in range(NB):
            wb = wbuf[nb % 2]
            self.dma("sp" if nb % 2 == 0 else "pool", wb[:, :].rearrange("p (c n) -> p c n", c=KC),
                   dr["ada_w"][l, :, nb * 512:(nb + 1) * 512].rearrange("(c p) n -> p c n", p=128), writes=[wb])
            for jj in range(4):
                j = nb * 4 + jj
                for k in range(KC):
                    self.mm(pt, pt[:, j * 3:(j + 1) * 3], wb[:, k * 512 + jj * 128:k * 512 + (jj + 1) * 128],
                            self.silu_c[:, k * 3:(k + 1) * 3], k == 0, k == KC - 1, reads=[wb, self.silu_c])
        for e3 in range(3):
            self.tt("dve", modc[:, :].rearrange("p (j e) -> p j e", e=3)[:, :, e3],
                    pt[:, 0:144].rearrange("p (j e) -> p j e", e=3)[:, :, e3], adab[:, :], ALU.add,
                    reads=[pt, adab], pwrites=[modc])
        m4 = modc[:, :].rearrange("p (w c e) -> p w c e", w=6, c=KC)
        a1 = self.A1[l][:, :].rearrange("p (c e) -> p c e", e=3)
        a2 = self.A2[l][:, :].rearrange("p (c e) -> p c e", e=3)
        for e3 in range(3):
            self.stt(a1[:, :, e3], m4[:, 1, :, e3], 1.0, gm[:, :], ALU.add, ALU.mult, reads=[modc, gm], pwrites=[self.A1[l]])
            self.stt(a2[:, :, e3], m4[:, 4, :, e3], 1.0, gf[:, :], ALU.add, ALU.mult, reads=[modc, gf], pwrites=[self.A2[l]])
        if "modc" in self.debug:
            self.dma("sp", dr["modc"][l].rearrange("p w c e -> p (w c e)"), modc[:, :], reads=[modc])
        self.phase_end()
        sb.release()

    def mcol(self, l, which, c, e):
        i = (which * KC + c) * 3 + e
        return self.modc[l][:, i:i + 1]

    def rstd_from(self, xt_tiles, w, sqb, pt, rstd, lnv):
        for c in range(KC):
            self.act(sqb[c][:, :w], xt_tiles[c][:, :w], AF.Square, reads=[xt_tiles[c]], writes=[sqb[c]])
        for c in range(KC):
            self.mm(pt, pt[:, :w], self.onesb[:, :], sqb[c][:, :w], c == 0, c == KC - 1, reads=[self.onesb, sqb[c]])
        self.act(lnv[:, :w], pt[:, :w], AF.Ln, reads=[pt], writes=[lnv], scale=1.0 / D, bias=self.epsc[:, 0:1])
        self.act(rstd[:, :w], lnv[:, :w], AF.Exp, reads=[lnv], writes=[rstd], scale=-0.5)

    def p1_inproj(self, l, Xin):
        sb, dr, fw, ps = self.sb, self.dr, self.fw, self.ps
        sb.mark()
        W = 256
        win = sb.alloc(KC * IN_W, BF16, name="win")
        winv = win[:, :].rearrange("p (c n) -> p c n", c=KC)
        for nb in range(11):
            self.dma("pool", winv[:, :, nb * 512:(nb + 1) * 512],
                   dr["w_in"][l, :, nb * 512:(nb + 1) * 512].rearrange("(c p) n -> p c n", p=128), pwrites=[win])
        ropeC = sb.alloc(S, F32); ropeS = sb.alloc(S, F32); pmb = self.pmb
        self.dma("sp", ropeC[:, :], dr["c_ropeC"][:, :], writes=[ropeC])
        self.dma("sp", ropeS[:, :], dr["c_ropeS"][:, :], writes=[ropeS])
        xt = [[sb.alloc(W, F32) for c in range(KC)] for i in range(2)]
        sqb = [sb.alloc(W, BF16) for c in range(KC)]
        hT = [sb.alloc(W, BF16) for c in range(KC)]
        tmpf = [sb.alloc(W, F32) for i in range(2)]
        rstd = sb.alloc(W, F32); lnv = sb.alloc(W, F32)
        qraw = [sb.alloc(W, BF16) for i in range(2)]
        t1 = [sb.alloc(W, F32) for i in range(2)]
        t2 = [sb.alloc(W, F32) for i in range(2)]
        tmpe = [sb.alloc(W, F32) for i in range(2)]
        tmpe2 = [sb.alloc(W, F32) for i in range(2)]
        st_q = sb.alloc(KC * W, BF16); st_k = sb.alloc(2 * W, BF16)
        st_u = sb.alloc(KC * W, F32)
        st_g = [sb.alloc(KC * W, BF16) for i in range(3)]
        st_v = [sb.alloc(256, BF16) for i in range(2)]
        ntile = T // W
        tl = [(s, ti) for s in range(NS) for ti in range(ntile)]
        v3 = lambda t, n: t[:, :].rearrange("p (c w) -> p c w", c=n)

        def load_x(idx):
            s, ti = tl[idx]
            xb = xt[idx % 2]
            for c in range(KC):
                self.dma("sp", xb[c][:, :], Xin[s, c * 128:(c + 1) * 128, ti * W:(ti + 1) * W], writes=[xb[c]])

        load_x(0)
        for it in range(len(tl)):
            s, ti = tl[it]
            t0 = ti * W
            is_ctx = ti == 0
            e3 = 2 if is_ctx else s
            xb = xt[it % 2]
            if it + 1 < len(tl):
                load_x(it + 1)
            self.rstd_from(xb, W, sqb, ps[0], rstd, lnv)
            for c in range(KC):
                tf = tmpf[c % 2]
                self.stt(tf[:, :], xb[c][:, :], self.A1[l][:, c * 3 + e3:c * 3 + e3 + 1], rstd[:, :], ALU.mult, ALU.mult,
                         reads=[xb[c], self.A1[l], rstd], writes=[tf])
                self.act(hT[c][:, :], tf[:, :], AF.Identity, reads=[tf, self.modc[l]], writes=[hT[c]],
                         bias=self.mcol(l, 0, c, e3), scale=1.0)
            for oc in range(44):
                if 10 <= oc < 12:
                    continue
                pt = ps[1 + (oc % 4)]
                for k in range(KC):
                    self.mm(pt, pt[:, :W], winv[:, k, oc * 128:(oc + 1) * 128], hT[k][:, :], k == 0, k == KC - 1, reads=[win, hT[k]])
                if oc < 10:
                    stt_, dst = (st_q, st_q[:, oc * W:(oc + 1) * W]) if oc < 8 else (st_k, st_k[:, (oc - 8) * W:(oc - 7) * W])
                    if is_ctx:
                        self.cp("act", dst, pt[:, :W], reads=[pt], pwrites=[stt_])
                    else:
                        qr_ = qraw[oc % 2]; a1 = t1[oc % 2]; a2 = t2[oc % 2]
                        p2 = ps[5 + (oc % 2)]
                        lt0 = t0 - LC
                        self.cp("act", qr_[:, :], pt[:, :W], reads=[pt], writes=[qr_])
                        self.mm(p2, p2[:, :W], pmb[:, :], qr_[:, :], True, True, reads=[pmb, qr_])
                        e1_ = tmpe[oc % 2]; e2_ = tmpe2[oc % 2]
                        self.cp("act", e1_[:, :], pt[:, :W], reads=[pt], writes=[e1_])
                        self.cp("act", e2_[:, :], p2[:, :W], reads=[p2], writes=[e2_])
                        self.tt("dve", a1[:, :], e1_[:, :], ropeC[:, lt0:lt0 + W], ALU.mult, reads=[e1_, ropeC], writes=[a1])
                        self.tt("dve", a2[:, :], e2_[:, :], ropeS[:, lt0:lt0 + W], ALU.mult, reads=[e2_, ropeS], writes=[a2])
                        self.tt("dve", dst, a1[:, :], a2[:, :], ALU.add, reads=[a1, a2], pwrites=[stt_])
                elif oc < 20:
                    self.cp("act", st_u[:, (oc - 12) * W:(oc - 11) * W], pt[:, :W], reads=[pt], pwrites=[st_u])
                else:
                    g = (oc - 20) // 8
                    c_ = (oc - 20) % 8
                    self.act(st_g[g][:, c_ * W:(c_ + 1) * W], pt[:, :W], AF.Gelu if g == 0 else AF.Sigmoid, reads=[pt], pwrites=[st_g[g]])
            for j in range(W // 128):
                pt = ps[7]
                for k in range(KC):
                    self.mm(pt, pt[:, :256], hT[k][:, j * 128:(j + 1) * 128], winv[:, k, 1280:1536], k == 0, k == KC - 1, reads=[win, hT[k]])
                sv = st_v[j % 2]
                self.cp("dve", sv[:, :], pt[:, :256], reads=[pt], writes=[sv])
                self.dma("pool", dr["vtm"][s, t0 + j * 128:t0 + (j + 1) * 128, :], sv[:, :], reads=[sv])
            fm = lambda nm: dr[nm][s, :, t0:t0 + W].rearrange("(c p) w -> p c w", p=128)
            self.dma("pool", fm("qT"), v3(st_q, KC), reads=[st_q])
            self.dma("pool", dr["kT"][s, :, t0:t0 + W].rearrange("(c p) w -> p c w", p=128), v3(st_k, 2), reads=[st_k])
            self.dma("sp", fm("uT"), v3(st_u, KC), reads=[st_u])
            self.dma("sp", fm("gzT"), v3(st_g[0], KC), reads=[st_g[0]])
            self.dma("pool", fm("smaT"), v3(st_g[1], KC), reads=[st_g[1]])
            self.dma("sp", fm("smrT"), v3(st_g[2], KC), reads=[st_g[2]])
        self.phase_end()
        sb.release()

    def p2_attn(self, l, last):
        sb, dr, fw, ps = self.sb, self.dr, self.fw, self.ps
        sb.mark()
        mprev = sb.alloc(512, BF16); mnext = sb.alloc(512, BF16)
        self.dma("pool", mprev[:, :], dr["c_mprev"][:, :], writes=[mprev])
        self.dma("pool", mnext[:, :], dr["c_mnext"][:, :], writes=[mnext])
        sk = sb.alloc(8, F32); ske = sb.alloc(8, F32); onef = sb.alloc(128, F32)
        esf = sb.alloc(1024, F32); eshi = sb.alloc(1024, BF16); eslo = sb.alloc(1024, BF16)
        self.dma("sp", sk[0:1, :], dr["sink"][l], writes=[sk])
        self.act(ske[0:1, :], sk[0:1, :], AF.Exp, reads=[sk], writes=[ske])
        fw.op("dve", lambda e: e.memset(onef[:, :], 1.0), writes=[onef])
        for h in range(8):
            self.ts("dve", esf[0:1, h * 128:(h + 1) * 128], onef[0:1, :], ske[0:1, h:h + 1], ALU.mult, reads=[onef, ske], pwrites=[esf])
        self.cp("dve", eshi[0:1, :], esf[0:1, :], reads=[esf], writes=[eshi])
        self.tt("dve", eslo[0:1, :], esf[0:1, :], eshi[0:1, :], ALU.subtract, reads=[esf, eshi], writes=[eslo])
        qall = sb.alloc(8 * T, BF16, name="qall")
        qv = qall[:, :].rearrange("p (h t) -> p h t", h=8)
        kt = [sb.alloc(T, BF16) for i in range(2)]
        vt = sb.alloc(18 * 256, BF16)
        Er = [sb.alloc(512, BF16) for i in range(10)]
        lnD2 = [sb.alloc(512, F32) for i in range(2)]; rD2 = [sb.alloc(512, F32) for i in range(2)]
        ost = [sb.alloc(4 * 512, BF16) for i in range(2)]
        scale = 1.0 / np.sqrt(128.0)
        for s in range(NS):
            for h in range(8):
                self.dma("sp", qv[:, h, :], dr["qT"][s, h * 128:(h + 1) * 128, :], pwrites=[qall])
            for c in range(2):
                self.dma("sp", kt[c][:, :], dr["kT"][s, c * 128:(c + 1) * 128, :], writes=[kt[c]])
            self.dma("sp", vt[:, :].rearrange("p (b f) -> p b f", f=256), dr["vtm"][s].rearrange("(b p) f -> p b f", p=128), writes=[vt])
            qblocks = [("lat", i) for i in range(16)]
            if not last:
                qblocks = [("ctx", 0), ("ctx", 1)] + qblocks
            for kind, i in qblocks:
                if kind == "lat":
                    tq = LC + i * 128
                    keys = [(0, 0, None), (128, 1, None)]
                    if i > 0:
                        keys.append((LC + (i - 1) * 128, 2 + i - 1, mprev))
                    keys.append((LC + i * 128, 2 + i, None))
                    if i < 15:
                        keys.append((LC + (i + 1) * 128, 2 + i + 1, mnext))
                    sw, si = 512, i % 4
                else:
                    tq = i * 128
                    keys = [(0, 0, None), (128, 1, None)]
                    sw, si = 256, i
                for kv in range(2):
                    Ek = Er[kv * 5:(kv + 1) * 5]; lnD = lnD2[kv]; rD = rD2[kv]
                    for idx, (kc0, vb, mk) in enumerate(keys):
                        pS = ps[idx % 3]
                        for g in range(4):
                            self.mm(pS, pS[:, g * 128:(g + 1) * 128], kt[kv][:, kc0:kc0 + 128], qv[:, 4 * kv + g, tq:tq + 128], True, True, reads=[kt[kv], qall])
                        E = Ek[idx]
                        self.act(E[:, :], pS[:, :], AF.Exp, reads=[pS], writes=[E], scale=float(scale))
                        if mk is not None:
                            self.tt("dve", E[:, :], E[:, :], mk[:, :], ALU.mult, reads=[E, mk], writes=[E])
                    pO, pD = ps[3 + (kv % 2) * 2], ps[4 + (kv % 2) * 2]
                    n = len(keys)
                    for idx, (kc0, vb, mk) in enumerate(keys):
                        self.mm(pO, pO[:, :], vt[:, vb * 256 + kv * 128:vb * 256 + (kv + 1) * 128], Ek[idx][:, :], idx == 0, idx == n - 1, reads=[vt, Ek[idx]])
                    for idx, (kc0, vb, mk) in enumerate(keys):
                        self.mm(pD, pD[:, :], self.onesb[:, :], Ek[idx][:, :], idx == 0, False, reads=[self.onesb, Ek[idx]])
                    self.mm(pD, pD[:, :], self.onesb[0:1, :], eshi[0:1, kv * 512:(kv + 1) * 512], False, False, reads=[self.onesb, eshi])
                    self.mm(pD, pD[:, :], self.onesb[0:1, :], eslo[0:1, kv * 512:(kv + 1) * 512], False, True, reads=[self.onesb, eslo])
                    self.act(lnD[:, :], pD[:, :], AF.Ln, reads=[pD], writes=[lnD])
                    self.act(rD[:, :], lnD[:, :], AF.Exp, reads=[lnD], writes=[rD], scale=-1.0)
                    ov = ost[kv][:, :].rearrange("p (g t) -> p g t", g=4)
                    self.tt("dve", ov[:, :, si * 128:(si + 1) * 128], pO[:, :].rearrange("p (g t) -> p g t", g=4),
                            rD[:, :].rearrange("p (g t) -> p g t", g=4), ALU.mult, reads=[pO, rD], pwrites=[ost[kv]])
                if (kind == "lat" and i % 4 == 3) or (kind == "ctx" and i == 1):
                    tq0 = tq + 128 - sw
                    for kv in range(2):
                        ov = ost[kv][:, :].rearrange("p (g t) -> p g t", g=4)
                        self.dma("pool", dr["attT"][s, kv * 512:(kv + 1) * 512, tq0:tq0 + sw].rearrange("(g d) t -> d g t", d=128),
                               ov[:, :, 0:sw], reads=[ost[kv]])
        self.phase_end()
        sb.release()

    def p3_rec(self, l):
        sb, dr, fw, ps = self.sb, self.dr, self.fw, self.ps
        sb.mark()
        lw = sb.alloc(2 * 2 * 8 * 128, BF16, name="lw")
        lwv = lw[:, :].rearrange("p (w d b j) -> p w d b j", w=2, d=2, b=8)
        for w_ in range(2):
            for d_ in range(2):
                self.dma("pool", lwv[:, w_, d_, :, :], dr["lru_w"][l, w_, d_].rearrange("b i j -> i b j"), pwrites=[lw])
        lv = sb.alloc(48, F32); cv = sb.alloc(40, F32)
        self.dma("sp", lv[:, :], dr["lru_vT"][l].rearrange("p a d c -> p (a d c)"), writes=[lv])
        self.dma("sp", cv[:, :], dr["convT"][l].rearrange("p a c -> p (a c)"), writes=[cv])
        onec = sb.alloc(1, F32)
        fw.op("dve", lambda e: e.memset(onec[:, :], 1.0), writes=[onec])
        e1 = sb.alloc(16, F32); l1 = sb.alloc(16, F32); cl = sb.alloc(16, F32)
        self.act(e1[:, :], lv[:, 32:48], AF.Exp, reads=[lv], writes=[e1], scale=-1.0)
        self.act(l1[:, :], e1[:, :], AF.Ln, reads=[e1, onec], writes=[l1], bias=onec[:, 0:1], scale=1.0)
        self.ts("dve", cl[:, :], l1[:, :], -8.0, ALU.mult, reads=[l1], writes=[cl])
        bu = [dict(u=sb.alloc(T, F32), uc=sb.alloc(T, F32), ucb=sb.alloc(T, BF16), gz=sb.alloc(T, BF16)) for i in range(2)]
        bd = [dict(r=sb.alloc(T, F32), gi=sb.alloc(T, F32), a=sb.alloc(T, F32)) for d_ in range(2)]
        hf = sb.alloc(T, F32); hb = sb.alloc(T, F32); og = sb.alloc(T, BF16)
        tiles = [(0, 256)] + [(256 + 512 * i, 512) for i in range(4)]
        items = [(s, c) for s in range(NS) for c in range(KC)]

        def conv_act(i):
            s, c = items[i]
            B = bu[i % 2]
            u, uc, gz = B["u"], B["uc"], B["gz"]
            self.dma("sp", u[:, :], dr["uT"][s, c * 128:(c + 1) * 128, :], writes=[u])
            self.dma("sp", gz[:, :], dr["gzT"][s, c * 128:(c + 1) * 128, :], writes=[gz])
            self.act(uc[:, :], u[:, :], AF.Identity, reads=[u, cv], writes=[uc], scale=cv[:, 2 * KC + c:2 * KC + c + 1], bias=cv[:, 4 * KC + c:4 * KC + c + 1])

        def conv_dve(i):
            s, c = items[i]
            B = bu[i % 2]
            u, uc, ucb = B["u"], B["uc"], B["ucb"]
            for k, d in ((0, -2), (1, -1), (3, 1)):
                for (sa, sb_) in ((0, LC), (LC, T)):
                    lo = max(sa, sa - d); hi = min(sb_, sb_ - d)
                    self.stt(uc[:, lo:hi], u[:, lo + d:hi + d], cv[:, k * KC + c:k * KC + c + 1], uc[:, lo:hi], ALU.mult, ALU.add,
                             reads=[u, cv, uc], pwrites=[uc])
            self.cp("dve", ucb[:, :], uc[:, :], reads=[uc], writes=[ucb])

        def gates(i, d_):
            s, c = items[i]
            ucb = bu[i % 2]["ucb"]
            r, gi, a = bd[d_]["r"], bd[d_]["gi"], bd[d_]["a"]
            for ti, (t0, w) in enumerate(tiles):
                pA = ps[(2 * ti) % 8]; pX = ps[(2 * ti + 1) % 8]
                self.mm(pA, pA[:, :w], lwv[:, 0, d_, c, :], ucb[:, t0:t0 + w], True, True, reads=[lw, ucb])
                self.mm(pX, pX[:, :w], lwv[:, 1, d_, c, :], ucb[:, t0:t0 + w], True, True, reads=[lw, ucb])
                self.act(r[:, t0:t0 + w], pA[:, :w], AF.Sigmoid, reads=[pA, lv], pwrites=[r], bias=lv[:, (0 * 2 + d_) * KC + c:(0 * 2 + d_) * KC + c + 1], scale=1.0)
                self.act(gi[:, t0:t0 + w], pX[:, :w], AF.Sigmoid, reads=[pX, lv], pwrites=[gi], bias=lv[:, (1 * 2 + d_) * KC + c:(1 * 2 + d_) * KC + c + 1], scale=1.0)
            self.act(a[:, :], r[:, :], AF.Exp, reads=[r, cl], writes=[a], scale=cl[:, d_ * KC + c:d_ * KC + c + 1])
            self.act(r[:, :], a[:, :], AF.Square, reads=[a], writes=[r])
            self.act(r[:, :], r[:, :], AF.Sqrt, reads=[r, onec], writes=[r], scale=-1.0, bias=onec[:, 0:1])

        def scan_dve(i, d_):
            uc = bu[i % 2]["uc"]
            q_, gi, a = bd[d_]["r"], bd[d_]["gi"], bd[d_]["a"]
            self.tt("dve", gi[:, :], gi[:, :], uc[:, :], ALU.mult, reads=[gi, uc], writes=[gi])
            self.tt("dve", gi[:, :], gi[:, :], q_[:, :], ALU.mult, reads=[gi, q_], writes=[gi])
            if d_ == 0:
                fw.op("dve", lambda e, a=a, gi=gi: e.tensor_tensor_scan(out=hf[:, :], data0=a[:, :], data1=gi[:, :], initial=0.0, op0=ALU.mult, op1=ALU.add),
                      reads=[a, gi], writes=[hf])
            else:
                fw.op("dve", lambda e, a=a, gi=gi: e.tensor_tensor_scan(out=hb[:, LC - 1::-1], data0=a[:, LC - 1::-1], data1=gi[:, LC - 1::-1], initial=0.0, op0=ALU.mult, op1=ALU.add),
                      reads=[a, gi], writes=[hb])
                fw.op("dve", lambda e, a=a, gi=gi: e.tensor_tensor_scan(out=hb[:, T - 1:LC - 1:-1], data0=a[:, T - 1:LC - 1:-1], data1=gi[:, T - 1:LC - 1:-1], initial=hb[:, 0:1], op0=ALU.mult, op1=ALU.add),
                      reads=[a, gi, hb], pwrites=[hb])

        def tail(i):
            s, c = items[i]
            gz = bu[i % 2]["gz"]
            self.tt("dve", hf[:, :], hf[:, :], hb[:, :], ALU.add, reads=[hf, hb], writes=[hf])
            self.tt("dve", og[:, :], hf[:, :], gz[:, :], ALU.mult, reads=[hf, gz], writes=[og])
            self.dma("pool", dr["rgT"][s, c * 128:(c + 1) * 128, :], og[:, :], reads=[og])

        n_it = len(items)
        conv_act(0)
        conv_dve(0)
        for i in range(n_it):
            if i + 1 < n_it:
                conv_act(i + 1)
            gates(i, 0)
            if i + 1 < n_it:
                conv_dve(i + 1)
            gates(i, 1)
            scan_dve(i, 0)
            scan_dve(i, 1)
            tail(i)
        self.phase_end()
        sb.release()

    def pF_copy(self):
        sb, dr, fw = self.sb, self.dr, self.fw
        sb.mark()
        buf = [sb.alloc(S, F32) for i in range(2)]
        i = 0
        for s in range(NS):
            for c in range(KC):
                b = buf[i % 2]
                self.dma("sp", b[:, :], dr["xT"][s, c * 128:(c + 1) * 128, LC:T], writes=[b])
                self.dma("sp", dr["outT"][s, c * 128:(c + 1) * 128, :], b[:, :], reads=[b])
                i += 1
        self.phase_end()
        sb.release()

    def p4_merge(self, l, Xin, Xout, last):
        sb, dr, fw, ps = self.sb, self.dr, self.fw, self.ps
        sb.mark()
        W = 512
        wts = []
        for name in ("w_attn_br", "w_rec_br", "w_out"):
            wt = sb.alloc(KC * D, BF16, name=name)
            wv = wt[:, :].rearrange("p (c n) -> p c n", c=KC)
            for hh in range(2):
                self.dma("pool", wv[:, :, hh * 512:(hh + 1) * 512], dr[name][l, :, hh * 512:(hh + 1) * 512].rearrange("(c p) n -> p c n", p=128), pwrites=[wt])
            wts.append((wt, wv))
        (wa, wav), (wr, wrv), (wo, wov) = wts
        wrt = sb.alloc(KC * NE, F32)
        wrtv = wrt[:, :].rearrange("p (c e) -> p c e", c=KC)
        self.dma("sp", wrtv, dr["w_router"][l].rearrange("(c p) e -> p c e", p=128), writes=[wrt])
        att = sb.alloc(KC * W, BF16); rg = sb.alloc(KC * W, BF16); sma = sb.alloc(KC * W, BF16); smr = sb.alloc(KC * W, BF16)
        xin = sb.alloc(KC * W, F32)
        mg = [sb.alloc(W, BF16) for c in range(KC)]
        xn = [sb.alloc(W, F32) for c in range(KC)]
        sqb = [sb.alloc(W, BF16) for c in range(KC)]
        h2f = [sb.alloc(W, F32) for c in range(KC)]
        h2b = [sb.alloc(W, BF16) for c in range(KC)]
        tm1 = [sb.alloc(W, F32) for i in range(2)]; tm2 = [sb.alloc(W, F32) for i in range(2)]
        rstd = sb.alloc(W, F32); lnv = sb.alloc(W, F32)
        lgs = sb.alloc(W, F32)
        stg = [sb.alloc(D, BF16) for i in range(2)]
        v3 = lambda t: t[:, :].rearrange("p (c w) -> p c w", c=KC)
        tiles4 = [(0, 256)] + [(LC + 512 * i, 512) for i in range(4)]
        for s in range(NS):
            for ti, (t0, w) in enumerate(tiles4):
                if last and ti == 0:
                    continue
                e3 = 2 if ti == 0 else s
                for (tl, nm) in ((att, "attT"), (rg, "rgT"), (sma, "smaT"), (smr, "smrT")):
                    self.dma("sp", v3(tl)[:, :, :w], dr[nm][s, :, t0:t0 + w].rearrange("(c p) w -> p c w", p=128), writes=[tl])
                self.dma("sp", v3(xin)[:, :, :w], Xin[s, :, t0:t0 + w].rearrange("(c p) w -> p c w", p=128), writes=[xin])
                for m in range(KC):
                    pA = ps[(2 * m) % 4]; pR = ps[(2 * m + 1) % 4]
                    for k in range(KC):
                        self.mm(pA, pA[:, :w], wav[:, k, m * 128:(m + 1) * 128], v3(att)[:, k, :w], k == 0, k == KC - 1, reads=[wa, att])
                    for k in range(KC):
                        self.mm(pR, pR[:, :w], wrv[:, k, m * 128:(m + 1) * 128], v3(rg)[:, k, :w], k == 0, k == KC - 1, reads=[wr, rg])
                    a1 = tm1[m % 2]; a2 = tm2[m % 2]
                    self.tt("dve", a1[:, :w], pA[:, :w], v3(sma)[:, m, :w], ALU.mult, reads=[pA, sma], writes=[a1])
                    self.tt("dve", a2[:, :w], pR[:, :w], v3(smr)[:, m, :w], ALU.mult, reads=[pR, smr], writes=[a2])
                    self.tt("dve", mg[m][:, :w], a1[:, :w], a2[:, :w], ALU.add, reads=[a1, a2], writes=[mg[m]])
                for m in range(KC):
                    pD = ps[4 + (m % 2)]
                    for k in range(KC):
                        self.mm(pD, pD[:, :w], wov[:, k, m * 128:(m + 1) * 128], mg[k][:, :w], k == 0, k == KC - 1, reads=[wo, mg[k]])
                    self.stt(xn[m][:, :w], pD[:, :w], self.mcol(l, 2, m, e3), v3(xin)[:, m, :w], ALU.mult, ALU.add, reads=[pD, self.modc[l], xin], writes=[xn[m]])
                    self.dma("pool", Xout[s, m * 128:(m + 1) * 128, t0:t0 + w], xn[m][:, :w], reads=[xn[m]])
                self.rstd_from(xn, w, sqb, ps[6], rstd, lnv)
                for m in range(KC):
                    tf = tm1[m % 2]
                    self.stt(tf[:, :w], xn[m][:, :w], self.A2[l][:, m * 3 + e3:m * 3 + e3 + 1], rstd[:, :w], ALU.mult, ALU.mult, reads=[xn[m], self.A2[l], rstd], writes=[tf])
                    self.act(h2f[m][:, :w], tf[:, :w], AF.Identity, reads=[tf, self.modc[l]], writes=[h2f[m]], bias=self.mcol(l, 3, m, e3), scale=1.0)
                    self.cp("act", h2b[m][:, :w], h2f[m][:, :w], reads=[h2f[m]], writes=[h2b[m]])
                pL = ps[7]
                for k in range(KC):
                    self.mm(pL, pL[0:NE, :w], wrtv[:, k, :], h2f[k][:, :w], k == 0, k == KC - 1, reads=[wrt, h2f[k]])
                self.cp("act", lgs[0:NE, :w], pL[0:NE, :w], reads=[pL], writes=[lgs])
                self.dma("pool", dr["lgT"][s, :, t0:t0 + w], lgs[0:NE, :w], reads=[lgs])
                for j in range(w // 128):
                    pT = ps[2 + j % 2]
                    pTb = pT[:, :].bitcast(BF16)
                    for m in range(KC):
                        self.tr(pT, pTb[:, m * 128:(m + 1) * 128], h2b[m][:, j * 128:(j + 1) * 128], self.identb[:, :], reads=[h2b[m], self.identb])
                    sg = stg[j % 2]
                    self.cp("act", sg[:, :], pTb[:, 0:D], reads=[pT], writes=[sg])
                    self.dma("pool", dr["h2tm"][s, t0 + j * 128:t0 + (j + 1) * 128, :], sg[:, :], reads=[sg])
        self.phase_end()
        sb.release()

    def p5_route(self, l, last):
        sb, dr, fw, ps = self.sb, self.dr, self.fw, self.ps
        sb.mark()
        bd = sb.alloc(32, F32)
        self.dma("sp", bd[0:32, :], dr["c_bdones"][:, :], writes=[bd])
        lg = sb.alloc(S, F32); E = sb.alloc(S, F32); rs = sb.alloc(S, F32); aff = sb.alloc(S, F32); work = sb.alloc(S, F32)
        m8 = sb.alloc(8, F32); mask = sb.alloc(S, F32); pin = sb.alloc(S, F32); posm = sb.alloc(S, F32); onesf = sb.alloc(S, F32)
        fw.op("dve", lambda e: e.memset(onesf[:, :], 1.0), writes=[onesf])
        groups = [(0, LC, S, CAPL, self.posT_lat)]
        if not last:
            groups.append((1, 0, LC, CAPC, self.posT_ctx))
        P = 32
        for gi, a0, n, cap, posT in groups:
            self.dma("sp", lg[0:P, :n], dr["lgT"][:, :, a0:a0 + n].rearrange("s e t -> (s e) t"), writes=[lg])
            self.act(E[0:P, :n], lg[0:P, :n], AF.Exp, reads=[lg], writes=[E])
            for c0 in range(0, n, 512):
                w = min(512, n - c0)
                pS = ps[(c0 // 512) % 2]
                self.mm(pS, pS[0:P, :w], bd[0:P, 0:P], E[0:P, c0:c0 + w], True, True, reads=[bd, E])
                fw.op("dve", lambda e, c0=c0, w=w, pS=pS: e.reciprocal(out=rs[0:P, c0:c0 + w], in_=pS[0:P, :w]), reads=[pS], pwrites=[rs])
            self.tt("dve", aff[0:P, :n], E[0:P, :n], rs[0:P, :n], ALU.mult, reads=[E, rs], writes=[aff])
            self.cp("dve", work[0:P, :n], aff[0:P, :n], reads=[aff], writes=[work])
            rounds = cap // 8
            for r_ in range(rounds):
                fw.op("dve", lambda e, n=n: e.max(out=m8[0:P, :], in_=work[0:P, :n]), reads=[work], writes=[m8])
                if r_ < rounds - 1:
                    fw.op("dve", lambda e, n=n: e.match_replace(out=work[0:P, :n], in_to_replace=m8[0:P, :], in_values=work[0:P, :n], imm_value=-1.0),
                          reads=[m8, work], writes=[work])
            self.ts("dve", mask[0:P, :n], aff[0:P, :n], m8[0:P, 7:8], ALU.is_ge, reads=[aff, m8], writes=[mask])
            fw.op("dve", lambda e, n=n: e.tensor_tensor_scan(out=pin[0:P, :n], data0=onesf[0:P, :n], data1=mask[0:P, :n], initial=0.0, op0=ALU.mult, op1=ALU.add),
                  reads=[onesf, mask], writes=[pin])
            self.tt("dve", pin[0:P, :n], pin[0:P, :n], mask[0:P, :n], ALU.mult, reads=[pin, mask], writes=[pin])
            self.ts("dve", posm[0:P, :n], pin[0:P, :n], -1.0, ALU.add, reads=[pin], writes=[posm])
            self.dma("sp", dr["rt"][gi, :, 0, 0:n], aff[0:P, :n], reads=[aff])
            self.dma("sp", dr["rt"][gi, :, 1, 0:n], posm[0:P, :n], reads=[posm])
            self.dma("sp", dr["rt"][gi, :, 2, 0:n], mask[0:P, :n], reads=[mask])
            pT = ps[2]
            ntc = n // 128
            for tc in range(ntc):
                self.tr(pT, pT[:, tc * 32:(tc + 1) * 32], posm[0:P, tc * 128:(tc + 1) * 128], self.ident[0:P, 0:P], reads=[posm, self.ident])
            self.cp("act", posT[:, 0:ntc * 32], pT[:, 0:ntc * 32], reads=[pT], writes=[posT])
        self.phase_end()
        sb.release()

    def p6_experts(self, l, last):
        self.p6a_gather(l, last)
        self.p6b_ffn(l, last)

    def p6a_gather(self, l, last):
        sb, dr, fw, ps = self.sb, self.dr, self.fw, self.ps
        sb.mark()
        NCX = 0 if last else 2 * CAPC
        NX = 512 + NCX
        iota_f = sb.alloc(256, F32)
        self.dma("sp", iota_f[:, :], dr["c_iota_f"][:, :], writes=[iota_f])
        h2l = []
        h2c = []
        for s in range(NS):
            t_ = sb.alloc(16 * D, BF16, name="h2l")
            tv = t_[:, :].rearrange("p (c d) -> p c d", c=16)
            for q4 in range(4):
                self.dma("sp", tv[:, q4 * 4:(q4 + 1) * 4, :], dr["h2tm"][s, LC + q4 * 512:LC + (q4 + 1) * 512, :].rearrange("(c p) d -> p c d", p=128), pwrites=[t_])
            h2l.append((t_, tv))
            if not last:
                c_ = sb.alloc(2 * D, BF16, name="h2c")
                cv_ = c_[:, :].rearrange("p (c d) -> p c d", c=2)
                self.dma("sp", cv_, dr["h2tm"][s, 0:LC, :].rearrange("(c p) d -> p c d", p=128), writes=[c_])
                h2c.append((c_, cv_))
        xeT = [[sb.alloc(NX, BF16) for k in range(KC)] for i in range(2)]
        sel = [[sb.alloc(256, BF16) for i in range(16)] for j in range(2)]
        selc = [sb.alloc(32, BF16) for i in range(2)]
        it = 0
        for e in range(NE):
            xb = xeT[e % 2]
            for s in range(NS):
                col = s * NE + e
                sl = sel[it % 2]
                it += 1
                for tc in range(16):
                    self.ts("dve", sl[tc][:, :], iota_f[:, 0:256], self.posT_lat[:, tc * 32 + col:tc * 32 + col + 1], ALU.is_equal,
                            reads=[iota_f, self.posT_lat], writes=[sl[tc]])
                for m in range(KC):
                    pX = ps[m % 4]
                    for tc in range(16):
                        self.mm(pX, pX[:, :256], h2l[s][1][:, tc, m * 128:(m + 1) * 128], sl[tc][:, :], tc == 0, tc == 15, reads=[h2l[s][0], sl[tc]])
                    self.cp("act", xb[m][:, s * 256:(s + 1) * 256], pX[:, :256], reads=[pX], pwrites=[xb[m]])
                if NCX:
                    for tc in range(2):
                        self.ts("dve", selc[tc][:, :], iota_f[:, 0:32], self.posT_ctx[:, tc * 32 + col:tc * 32 + col + 1], ALU.is_equal,
                                reads=[iota_f, self.posT_ctx], writes=[selc[tc]])
                    for m in range(KC):
                        pX = ps[4 + m % 4]
                        for tc in range(2):
                            self.mm(pX, pX[:, :32], h2c[s][1][:, tc, m * 128:(m + 1) * 128], selc[tc][:, :], tc == 0, tc == 1, reads=[h2c[s][0], selc[tc]])
                        self.cp("act", xb[m][:, 512 + s * 32:512 + (s + 1) * 32], pX[:, :32], reads=[pX], pwrites=[xb[m]])
            for m in range(KC):
                self.dma("sp", dr["xe"][e, m * 128:(m + 1) * 128, 0:NX], xb[m][:, :], reads=[xb[m]])
        self.phase_end()
        sb.release()

    def p6b_ffn(self, l, last):
        sb, dr, fw, ps = self.sb, self.dr, self.fw, self.ps
        sb.mark()
        NCX = 0 if last else 2 * CAPC
        NX = 512 + NCX
        NR = 4
        wring = [sb.alloc(8192, BF16, name="wring%d" % i) for i in range(NR)]
        xeb = [sb.alloc(KC * NX, BF16) for i in range(2)]
        hid = [sb.alloc(NX, BF16) for f in range(16)]
        sgt = [sb.alloc(512, F32) for i in range(2)]
        sgc = [sb.alloc(64, F32) for i in range(2)]
        yst = [sb.alloc(512, BF16) for i in range(2)]
        units = []
        for e in range(NE):
            for fh in range(2):
                units.append(("w_gate", e, fh)); units.append(("w_up", e, fh))
            for dh in range(2):
                units.append(("w_down", e, dh))
        loaded = {}

        def issue(ui):
            if ui >= len(units) or ui in loaded:
                return
            nm, e, hh = units[ui]
            wt = wring[ui % NR]
            if nm == "w_down":
                self.dma("pool", wt[:, :].rearrange("p (c n) -> p c n", c=16), dr[nm][l, e, :, hh * 512:(hh + 1) * 512].rearrange("(c p) n -> p c n", p=128), writes=[wt])
            else:
                self.dma("pool", wt[:, :].rearrange("p (c n) -> p c n", c=KC), dr[nm][l, e, :, hh * 1024:(hh + 1) * 1024].rearrange("(c p) n -> p c n", p=128), writes=[wt])
            loaded[ui] = wt

        for ui in range(NR - 1):
            issue(ui)
        ui = 0
        yi = 0
        for e in range(NE):
            xt_ = xeb[e % 2]
            xv = xt_[:, :].rearrange("p (k n) -> p k n", k=KC)
            if e == 0:
                self.dma("sp", xv, dr["xe"][e, :, 0:NX].rearrange("(k p) n -> p k n", p=128), writes=[xt_])
            if e + 1 < NE:
                xn_ = xeb[(e + 1) % 2]
                self.dma("sp", xn_[:, :].rearrange("p (k n) -> p k n", k=KC), dr["xe"][e + 1, :, 0:NX].rearrange("(k p) n -> p k n", p=128), writes=[xn_])
            for fh in range(2):
                for uj in range(ui, ui + NR):
                    issue(uj)
                wg = loaded[ui]; wu = loaded[ui + 1]; ui += 2
                wgv = wg[:, :].rearrange("p (c n) -> p c n", c=KC)
                wuv = wu[:, :].rearrange("p (c n) -> p c n", c=KC)
                for f in range(8):
                    fi = fh * 8 + f
                    pG, pU, pGc, pUc = ps[fi % 2], ps[2 + fi % 2], ps[4], ps[5]
                    for k in range(KC):
                        self.mm(pG, pG[:, :512], wgv[:, k, f * 128:(f + 1) * 128], xv[:, k, 0:512], k == 0, k == KC - 1, reads=[wg, xt_])
                    for k in range(KC):
                        self.mm(pU, pU[:, :512], wuv[:, k, f * 128:(f + 1) * 128], xv[:, k, 0:512], k == 0, k == KC - 1, reads=[wu, xt_])
                    if NCX:
                        for k in range(KC):
                            self.mm(pGc, pGc[:, :NCX], wgv[:, k, f * 128:(f + 1) * 128], xv[:, k, 512:NX], k == 0, k == KC - 1, reads=[wg, xt_])
                        for k in range(KC):
                            self.mm(pUc, pUc[:, :NCX], wuv[:, k, f * 128:(f + 1) * 128], xv[:, k, 512:NX], k == 0, k == KC - 1, reads=[wu, xt_])
                    sg = sgt[fi % 2]
                    self.act(sg[:, :], pG[:, :512], AF.Silu, reads=[pG], writes=[sg])
                    self.tt("dve", hid[fi][:, 0:512], sg[:, :], pU[:, :512], ALU.mult, reads=[sg, pU], pwrites=[hid[fi]])
                    if NCX:
                        sc_ = sgc[fi % 2]
                        self.act(sc_[:, :], pGc[:, :NCX], AF.Silu, reads=[pGc], writes=[sc_])
                        self.tt("dve", hid[fi][:, 512:NX], sc_[:, :], pUc[:, :NCX], ALU.mult, reads=[sc_, pUc], pwrites=[hid[fi]])
            for dh in range(2):
                for uj in range(ui, ui + NR):
                    issue(uj)
                wd = loaded[ui]; ui += 1
                wdv = wd[:, :].rearrange("p (c n) -> p c n", c=16)
                rgs = [(s * 256 + c * 128, 128, ("ye", s, c)) for s in range(NS) for c in range(2)]
                if NCX:
                    rgs.append((512, NCX, ("yec",)))
                for (c0, M, dst) in rgs:
                    pY = ps[6 + yi % 2]
                    ys = yst[yi % 2]
                    yi += 1
                    for f in range(16):
                        self.mm(pY, pY[0:M, :512], hid[f][:, c0:c0 + M], wdv[:, f, :], f == 0, f == 15, reads=[hid[f], wd])
                    self.cp("act", ys[0:M, :], pY[0:M, :512], reads=[pY], writes=[ys])
                    if dst[0] == "ye":
                        self.dma("sp", dr["ye"][dst[1], e, dst[2] * 128:(dst[2] + 1) * 128, dh * 512:(dh + 1) * 512], ys[0:128, :], reads=[ys])
                    else:
                        for s in range(NS):
                            self.dma("sp", dr["yec"][s, e, :, dh * 512:(dh + 1) * 512], ys[s * CAPC:(s + 1) * CAPC, :], reads=[ys])
        self.phase_end()
        sb.release()

    def p7_scatter(self, l, Xm, Xout, last):
        sb, dr, fw, ps = self.sb, self.dr, self.fw, self.ps
        sb.mark()
        W = 512
        rowsel = sb.alloc(32 * 128, BF16)
        self.dma("pool", rowsel[0:32, :], dr["c_rowsel"][:, :], writes=[rowsel])
        iota_p = sb.alloc(2, F32)
        self.dma("sp", iota_p[:, :], dr["c_iota_p"][:, :], writes=[iota_p])
        gfin = sb.alloc(KC, F32)
        self.dma("sp", gfin[:, :], dr["gfinT"][:, :], writes=[gfin])
        affr = sb.alloc(S, BF16); posr = sb.alloc(S, BF16)
        yeall = sb.alloc(NE * 2 * D, BF16, name="yeall")
        selT = [sb.alloc(W, BF16) for i in range(32)]
        affb = [sb.alloc(W, F32) for i in range(2)]
        xin = sb.alloc(KC * W, F32)
        xn = [sb.alloc(W, F32) for c in range(KC)]
        sqb = [sb.alloc(W, BF16) for c in range(KC)]
        ot = [sb.alloc(W, F32) for i in range(2)]
        rstd = sb.alloc(W, F32); lnv = sb.alloc(W, F32)
        groups = [(0, LC, S, 128, 2)]
        if not last:
            groups.append((1, 0, LC, CAPC, 1))
        v3 = lambda t: t[:, :].rearrange("p (c w) -> p c w", c=KC)
        for gi, a0, n, NP, ncc in groups:
            self.dma("pool", affr[0:32, :n], dr["rt"][gi, :, 0, 0:n], writes=[affr])
            self.dma("pool", posr[0:32, :n], dr["rt"][gi, :, 1, 0:n], writes=[posr])
            for s in range(NS):
                e3 = s if gi == 0 else 2
                if gi == 0:
                    yv = yeall[:, :].rearrange("p (e c d) -> p e c d", e=NE, c=2)
                    for q8 in range(8):
                        self.dma("sp", yv[:, q8 * 2:(q8 + 1) * 2, :, :], dr["ye"][s, q8 * 2:(q8 + 1) * 2, :, :].rearrange("e (c p) d -> p e c d", p=128), pwrites=[yeall])
                else:
                    yv = yeall[:, 0:NE * D].rearrange("p (e c d) -> p e c d", e=NE, c=1)
                    self.dma("sp", yv[0:CAPC, :, 0, :], dr["yec"][s].rearrange("e j d -> j e d"), writes=[yeall])
                for c0 in range(0, n, W):
                    w = min(W, n - c0)
                    for e in range(NE):
                        r_ = s * NE + e
                        pP = ps[e % 2]; pA = ps[2 + e % 2]
                        self.mm(pP, pP[:, :w], rowsel[0:32, r_ * 128:(r_ + 1) * 128], posr[0:32, c0:c0 + w], True, True, reads=[rowsel, posr])
                        self.mm(pA, pA[:, :w], rowsel[0:32, r_ * 128:(r_ + 1) * 128], affr[0:32, c0:c0 + w], True, True, reads=[rowsel, affr])
                        ab = affb[e % 2]
                        self.cp("act", ab[:, :w], pA[:, :w], reads=[pA], writes=[ab])
                        for cc in range(ncc):
                            st_ = selT[e * 2 + cc]
                            self.stt(st_[0:NP, :w], pP[0:NP, :w], iota_p[0:NP, cc:cc + 1], ab[0:NP, :w], ALU.is_equal, ALU.mult, reads=[pP, iota_p, ab], writes=[st_])
                    self.dma("sp", v3(xin)[:, :, :w], Xm[s, :, a0 + c0:a0 + c0 + w].rearrange("(c p) w -> p c w", p=128), writes=[xin])
                    for m in range(KC):
                        pY = ps[4 + m % 2]
                        nmm = NE * ncc
                        idx = 0
                        for e in range(NE):
                            for cc in range(ncc):
                                self.mm(pY, pY[:, :w], yv[0:NP, e, cc, m * 128:(m + 1) * 128], selT[e * 2 + cc][0:NP, :w], idx == 0, idx == nmm - 1, reads=[yeall, selT[e * 2 + cc]])
                                idx += 1
                        self.stt(xn[m][:, :w], pY[:, :w], self.mcol(l, 5, m, e3), v3(xin)[:, m, :w], ALU.mult, ALU.add, reads=[pY, self.modc[l], xin], writes=[xn[m]])
                        if not last:
                            self.dma("pool", Xout[s, m * 128:(m + 1) * 128, a0 + c0:a0 + c0 + w], xn[m][:, :w], reads=[xn[m]])
                    if last:
                        self.rstd_from(xn, w, sqb, ps[6], rstd, lnv)
                        for m in range(KC):
                            o_ = ot[m % 2]
                            self.stt(o_[:, :w], xn[m][:, :w], gfin[:, m:m + 1], rstd[:, :w], ALU.mult, ALU.mult, reads=[xn[m], gfin, rstd], writes=[o_])
                            self.dma("pool", dr["outT"][s, m * 128:(m + 1) * 128, c0:c0 + w], o_[:, :w], reads=[o_])
        self.phase_end()
        sb.release()

def prep_shared(inp):
    f = lambda a: np.ascontiguousarray(a, dtype=np.float32)
    sh = {}
    sh["ada_w"] = f(inp["ada_w"])
    sh["ada_bT"] = f(inp["ada_b"].reshape(DEPTH, 48, 128).transpose(0, 2, 1))
    sh["gmixT"] = f(inp["norm_mix_g"].reshape(DEPTH, KC, 128).transpose(0, 2, 1))
    sh["gffnT"] = f(inp["norm_ffn_g"].reshape(DEPTH, KC, 128).transpose(0, 2, 1))
    sh["gfinT"] = f(inp["final_norm_g"].reshape(KC, 128).T)
    sh["w_in"] = f(inp["w_in"])
    sh["sink"] = f(inp["attn_sink"].reshape(DEPTH, 1, 8))
    cw = np.concatenate([inp["conv_w"], inp["conv_b"][:, None, :]], axis=1)
    sh["convT"] = f(cw.reshape(DEPTH, 5, KC, 128).transpose(0, 3, 1, 2))
    sh["lru_w"] = f(np.stack([inp["lru_wa"], inp["lru_wx"]], axis=1))
    lv = np.stack([inp["lru_ba"], inp["lru_bx"], inp["lru_lambda"]], axis=1)
    sh["lru_vT"] = f(lv.reshape(DEPTH, 3, 2, KC, 128).transpose(0, 4, 1, 2, 3))
    for k in ("w_attn_br", "w_rec_br", "w_out", "w_router", "w_gate", "w_up", "w_down"):
        sh[k] = f(inp[k])
    for k, v in host_consts().items():
        sh["c_" + k] = f(v)
    return sh


def prep_core(inp, core):
    b0 = core * NS
    xs = inp["x"][b0:b0 + NS]
    cs = inp["ctx"][b0:b0 + NS]
    xT = np.concatenate([cs, xs], axis=1).transpose(0, 2, 1)
    cc = np.stack([inp["c"][b0], inp["c"][b0 + 1], inp["c_ctx"]], axis=1)
    return {"xT": np.ascontiguousarray(xT, dtype=np.float32),
            "cT": np.ascontiguousarray(cc.reshape(KC, 128, 3).transpose(1, 0, 2), dtype=np.float32)}


_NC_CACHE = {}


def kernel(**inputs):
    inp = {k: np.asarray(v) for k, v in inputs.items()}
    n = 8
    if "nc" not in _NC_CACHE:
        kb = KB()
        _NC_CACHE["nc"] = kb.build()
        _NC_CACHE["names"] = set(kb.dr.keys())
    nc = _NC_CACHE["nc"]
    sh = prep_shared(inp)
    in_maps = []
    for core in range(n):
        m = dict(sh)
        m.update(prep_core(inp, core))
        in_maps.append({k: v for k, v in m.items() if k in _NC_CACHE["names"]})
    res = run_bass_kernel_spmd(nc, in_maps, core_ids=list(range(n)))
    outs = [np.asarray(r["outT"]).transpose(0, 2, 1) for r in res.results]
    return np.ascontiguousarray(np.concatenate(outs, axis=0), dtype=np.float32)
```

```python
import numpy as np
import concourse.bass as bass
import concourse.mybir as mybir

F32 = mybir.dt.float32
BF16 = mybir.dt.bfloat16
I32 = mybir.dt.int32
AF = mybir.ActivationFunctionType
ALU = mybir.AluOpType
AX = mybir.AxisListType
from concourse.bass_utils import run_bass_kernel_spmd
ENGS = ("pe", "act", "dve", "pool", "sp")


class Op:
    __slots__ = ("eng", "emit", "deps", "dma_deps", "signal", "count", "is_dma", "dsem", "dval", "prev_dval", "idx")
    _ctr = 0

    def __init__(self, eng, emit, is_dma=False):
        self.eng = eng
        self.emit = emit
        self.deps = {}
        self.dma_deps = []
        self.signal = False
        self.count = 0
        self.is_dma = is_dma
        self.dsem = None
        self.dval = 0
        self.prev_dval = 0
        Op._ctr += 1
        self.idx = Op._ctr


class Tile:
    def __init__(self, ap, name=""):
        self.ap = ap
        self.name = name
        self.w_c = {}
        self.w_d = []
        self.r_c = {}
        self.r_d = []
        self.g_c = {}
        self.g_d = []

    def __getitem__(self, k):
        return self.ap[k]


def _merge(dst_c, dst_d, src_c, src_d):
    for e, o in src_c.items():
        if e not in dst_c or dst_c[e].idx < o.idx:
            dst_c[e] = o
    for o in src_d:
        if o not in dst_d:
            dst_d.append(o)


class FW:
    def __init__(self, nc, n_dma_sems=4):
        self.nc = nc
        self.ops = {e: [] for e in ENGS}
        self.sem = {}
        self.dma_pool = {}
        self.dma_rr = {}
        self.n_dma_sems = n_dma_sems
        self.out_dmas = []
        self.last = {e: None for e in ENGS}
        self._cms = []

    def setup_sems(self, es):
        nc = self.nc
        for e in ENGS:
            self.sem[e] = es.enter_context(nc.semaphore("s_" + e))
        for q in ("sp", "act", "pool"):
            self.dma_pool[q] = [[es.enter_context(nc.semaphore("d_%s%d" % (q, i))), 0] for i in range(self.n_dma_sems)]
            self.dma_rr[q] = 0

    def _add_read(self, op, t):
        _merge(op.deps, op.dma_deps, t.w_c, t.w_d)
        if op.is_dma:
            t.r_d.append(op)
        else:
            t.r_c[op.eng] = op

    def _add_write(self, op, t, partial):
        if (not partial) or t.r_c or t.r_d:
            t.g_c = {}
            t.g_d = []
            _merge(t.g_c, t.g_d, t.r_c, t.r_d)
            _merge(t.g_c, t.g_d, t.w_c, t.w_d)
            t.r_c, t.r_d, t.w_c, t.w_d = {}, [], {}, []
        _merge(op.deps, op.dma_deps, t.g_c, t.g_d)
        if op.is_dma:
            t.w_d.append(op)
        else:
            t.w_c[op.eng] = op

    def _record(self, o, reads, writes, pwrites):
        wset = set(id(t) for t in writes) | set(id(t) for t in pwrites)
        for t in reads:
            _merge(o.deps, o.dma_deps, t.w_c, t.w_d)
        for t in writes:
            self._add_write(o, t, False)
        for t in pwrites:
            self._add_write(o, t, True)
        for t in reads:
            if id(t) not in wset:
                if o.is_dma:
                    t.r_d.append(o)
                else:
                    t.r_c[o.eng] = o

    def op(self, eng, emit, reads=(), writes=(), pwrites=()):
        o = Op(eng, emit)
        self._record(o, reads, writes, pwrites)
        self.ops[eng].append(o)
        self.last[eng] = o
        return o

    def dma(self, q, out, in_, reads=(), writes=(), pwrites=(), **kw):
        o = Op(q, (lambda eng: eng.dma_start(out=out, in_=in_, **kw)), is_dma=True)
        pool = self.dma_pool[q]
        i = self.dma_rr[q]
        self.dma_rr[q] = (i + 1) % len(pool)
        o.dsem = pool[i][0]
        o.prev_dval = pool[i][1]
        pool[i][1] += 16
        o.dval = pool[i][1]
        self._record(o, reads, writes, pwrites)
        self.ops[q].append(o)
        self.out_dmas.append(o)
        return o

    def barrier(self):
        lasts = {e: o for e, o in self.last.items() if o is not None}
        for e in ENGS:
            o = Op(e, None)
            for e2, o2 in lasts.items():
                if e2 != e:
                    o.deps[e2] = o2
            o.dma_deps = list(self.out_dmas)
            self.ops[e].append(o)
        self.out_dmas = []

    def finalize(self):
        for e in ENGS:
            for o in self.ops[e]:
                for e2, p in o.deps.items():
                    if p.is_dma:
                        continue
                    if e2 == e and e == "pe":
                        continue
                    p.signal = True
        for e in ENGS:
            c = 0
            for o in self.ops[e]:
                if o.is_dma or o.emit is None:
                    continue
                if o.signal:
                    c += 1
                    o.count = c
        nc = self.nc
        engmap = {"pe": "tensor", "act": "scalar", "dve": "vector", "pool": "gpsimd", "sp": "sync"}
        stats = {}
        with nc.Block() as block:
            for e in ENGS:
                def body(engine, e=e):
                    waited = {}
                    nw = 0

                    def wait(sem, val):
                        nonlocal nw
                        k = id(sem)
                        if waited.get(k, 0) >= val:
                            return
                        waited[k] = val
                        engine.wait_ge(sem, val)
                        nw += 1

                    for o in self.ops[e]:
                        for e2, p in o.deps.items():
                            if p.is_dma:
                                wait(p.dsem, p.dval)
                                continue
                            if e2 == e and e == "pe":
                                continue
                            if p is o:
                                continue
                            wait(self.sem[e2], p.count)
                        for p in o.dma_deps:
                            wait(p.dsem, p.dval)
                        if o.emit is None:
                            continue
                        if o.is_dma:
                            if o.prev_dval > 0:
                                wait(o.dsem, o.prev_dval)
                            ins = o.emit(engine)
                            ins.then_inc(o.dsem, 16)
                        else:
                            ins = o.emit(engine)
                            if o.signal:
                                ins.then_inc(self.sem[e], 1)
                    stats[e] = (len(self.ops[e]), nw)
                getattr(block, engmap[e])(body)
        return stats


class Sbuf:
    def __init__(self, big, nwords):
        self.big = big
        self.n = nwords
        self.off = 0
        self.marks = []

    def mark(self):
        self.marks.append(self.off)

    def release(self):
        self.off = self.marks.pop()

    def alloc(self, nelem, dtype=F32, parts=128, name=""):
        if dtype == BF16:
            nw = (nelem + 1) // 2
        else:
            nw = nelem
        nw = (nw + 7) // 8 * 8
        assert self.off + nw <= self.n, "SBUF overflow %s: %d + %d > %d" % (name, self.off, nw, self.n)
        ap = self.big[0:parts, self.off:self.off + nw]
        self.off += nw
        if dtype == BF16:
            ap = ap.bitcast(BF16)[:, 0:nelem]
        elif dtype != F32:
            ap = ap.bitcast(dtype)[:, 0:nelem]
        else:
            ap = ap[:, 0:nelem]
        return Tile(ap, name)

from contextlib import ExitStack
import ml_dtypes

D = 1024
NS = 2
LC = 256
S = 2048
T = LC + S
DEPTH = 2
NE = 16
DEXP = 2048
IN_W = 5632
CAPL = 256
CAPC = 32
EPS = 1e-6
KC = 8

SB_WORDS = 46080


def host_consts():
    c = {}
    half = 32
    freqs = (10000.0 ** (-np.arange(half, dtype=np.float32) / half)).astype(np.float32)
    t = np.arange(S)
    rows = (t // 64).astype(np.float32)
    cols = (t % 64).astype(np.float32)
    C = np.zeros((128, S), np.float32)
    Sn = np.zeros((128, S), np.float32)
    for d in range(128):
        pos = rows if d < 64 else cols
        j = (d % 64) % 32
        ang = (pos * freqs[j]).astype(np.float32)
        C[d] = np.cos(ang)
        Sn[d] = np.sin(ang)
    c["ropeC"] = C
    c["ropeS"] = Sn
    Pm = np.zeros((128, 128), np.float32)
    for d in range(128):
        i = d % 64
        if i < 32:
            Pm[d + 32, d] = -1.0
        else:
            Pm[d - 32, d] = 1.0
    c["pm"] = Pm
    c["ident"] = np.eye(128, dtype=np.float32)
    kk = np.arange(128)[:, None]
    qq = np.arange(128)[None, :]
    mprev = (qq <= kk).astype(np.float32)
    mnext = (kk <= qq).astype(np.float32)
    c["mprev"] = np.tile(mprev, (1, 4))
    c["mnext"] = np.tile(mnext, (1, 4))
    c["iota_f"] = np.tile(np.arange(256, dtype=np.float32)[None, :], (128, 1))
    c["iota_p"] = np.stack([np.arange(128, dtype=np.float32), np.arange(128, dtype=np.float32) + 128], axis=1)
    sel = np.zeros((32, 32, 128), np.float32)
    for r in range(32):
        sel[r, r, :] = 1.0
    c["rowsel"] = sel.transpose(1, 0, 2).reshape(32, 32 * 128)
    bd = np.zeros((32, 32), np.float32)
    bd[:16, :16] = 1.0
    bd[16:, 16:] = 1.0
    c["bdones"] = bd
    return c


CONST_SHAPES = {"ropeC": (128, S), "ropeS": (128, S), "pm": (128, 128), "ident": (128, 128),
                "mprev": (128, 512), "mnext": (128, 512), "iota_f": (128, 256), "iota_p": (128, 2),
                "rowsel": (32, 32 * 128), "bdones": (32, 32)}


class KB:
    def __init__(self, debug=None, stop_after=None):
        self.debug = debug or []
        self.stop_after = stop_after
        self.nc = bass.Bass("TRN2", target_bir_lowering=False)
        self.dr = {}

    def din(self, name, shape, dt=F32):
        self.dr[name] = self.nc.dram_tensor(name, list(shape), dt, kind="ExternalInput").ap()
        return self.dr[name]

    def dscr(self, name, shape, dt=F32, out=False):
        kind = "ExternalOutput" if (out or name in self.debug) else "Internal"
        self.dr[name] = self.nc.dram_tensor(name, list(shape), dt, kind=kind).ap()
        return self.dr[name]

    def mm(self, pst, out, lhsT, rhs, start, stop, reads):
        self.fw.op("pe", lambda e: e.matmul(out, lhsT=lhsT, rhs=rhs, start=start, stop=stop), reads=reads, pwrites=[pst])

    def tr(self, pst, out, in_, ident, reads):
        self.fw.op("pe", lambda e: e.transpose(out, in_, ident), reads=reads, pwrites=[pst])

    def act(self, out, in_, func, reads, writes=(), pwrites=(), **kw):
        self.fw.op("act", lambda e: e.activation(out=out, in_=in_, func=func, **kw), reads=reads, writes=writes, pwrites=pwrites)

    def tt(self, eng, out, in0, in1, op, reads, writes=(), pwrites=()):
        self.fw.op(eng, lambda e: e.tensor_tensor(out=out, in0=in0, in1=in1, op=op), reads=reads, writes=writes, pwrites=pwrites)

    def ts(self, eng, out, in0, s1, op0, reads, s2=None, op1=None, writes=(), pwrites=()):
        if op1 is None:
            self.fw.op(eng, lambda e: e.tensor_scalar(out=out, in0=in0, scalar1=s1, scalar2=None, op0=op0), reads=reads, writes=writes, pwrites=pwrites)
        else:
            self.fw.op(eng, lambda e: e.tensor_scalar(out=out, in0=in0, scalar1=s1, scalar2=s2, op0=op0, op1=op1), reads=reads, writes=writes, pwrites=pwrites)

    def stt(self, out, in0, scalar, in1, op0, op1, reads, writes=(), pwrites=()):
        self.fw.op("dve", lambda e: e.scalar_tensor_tensor(out=out, in0=in0, scalar=scalar, in1=in1, op0=op0, op1=op1), reads=reads, writes=writes, pwrites=pwrites)

    def cp(self, eng, out, in_, reads, writes=(), pwrites=()):
        if eng == "act":
            self.fw.op("act", lambda e: e.copy(out=out, in_=in_), reads=reads, writes=writes, pwrites=pwrites)
        else:
            self.fw.op(eng, lambda e: e.tensor_copy(out=out, in_=in_), reads=reads, writes=writes, pwrites=pwrites)

    def dma(self, q, out, in_, reads=(), writes=(), pwrites=(), maxdesc=512):
        shp = tuple(out.shape)
        if len(shp) == 3 and shp[0] * shp[1] > maxdesc and tuple(in_.shape) == shp:
            step = max(1, maxdesc // shp[0])
            first = True
            for i0 in range(0, shp[1], step):
                i1 = min(shp[1], i0 + step)
                if first:
                    self.fw.dma(q, out[:, i0:i1, :], in_[:, i0:i1, :], reads=reads, writes=writes, pwrites=pwrites)
                    first = False
                else:
                    self.fw.dma(q, out[:, i0:i1, :], in_[:, i0:i1, :], reads=reads, pwrites=tuple(writes) + tuple(pwrites))
            return
        self.fw.dma(q, out, in_, reads=reads, writes=writes, pwrites=pwrites)

    def phase_end(self):
        self.fw.barrier()

    def build(self):
        nc = self.nc
        din, dscr = self.din, self.dscr
        din("xT", (NS, D, T))
        din("cT", (128, KC, 3))
        din("ada_w", (DEPTH, D, 6 * D))
        din("ada_bT", (DEPTH, 128, 48))
        din("gmixT", (DEPTH, 128, KC))
        din("gffnT", (DEPTH, 128, KC))
        din("gfinT", (128, KC))
        din("w_in", (DEPTH, D, IN_W))
        din("sink", (DEPTH, 1, 8))
        din("convT", (DEPTH, 128, 5, KC))
        din("lru_w", (DEPTH, 2, 2, 8, 128, 128))
        din("lru_vT", (DEPTH, 128, 3, 2, KC))
        din("w_attn_br", (DEPTH, D, D))
        din("w_rec_br", (DEPTH, D, D))
        din("w_out", (DEPTH, D, D))
        din("w_router", (DEPTH, D, NE))
        if self.stop_after not in ("p0", "p1", "p2", "p3", "p4", "p5", "wip"):
            din("w_gate", (DEPTH, NE, D, DEXP))
            din("w_up", (DEPTH, NE, D, DEXP))
            din("w_down", (DEPTH, NE, DEXP, D))
        for k, shp in CONST_SHAPES.items():
            din("c_" + k, shp)
        dscr("outT", (NS, D, S), out=True)
        dscr("X1", (NS, D, T)); dscr("X2", (NS, D, T)); dscr("X3", (NS, D, T))
        dscr("qT", (NS, D, T), BF16); dscr("kT", (NS, 256, T), BF16); dscr("vtm", (NS, T, 256), BF16)
        dscr("uT", (NS, D, T)); dscr("gzT", (NS, D, T), BF16); dscr("smaT", (NS, D, T), BF16); dscr("smrT", (NS, D, T), BF16)
        dscr("attT", (NS, D, T), BF16); dscr("rgT", (NS, D, T), BF16)
        dscr("h2tm", (NS, T, D), BF16); dscr("lgT", (NS, NE, T))
        dscr("ye", (NS, NE, CAPL, D), BF16); dscr("yec", (NS, NE, CAPC, D), BF16); dscr("xe", (NE, D, 512 + 2 * CAPC), BF16)
        dscr("modc", (DEPTH, 128, 6, KC, 3))
        dscr("rt", (2, 32, 3, S))

        with ExitStack() as es:
            big = es.enter_context(nc.sbuf_tensor("big", [128, SB_WORDS], F32))
            pst = [es.enter_context(nc.psum_tensor("ps%d" % i, [128, 512], F32)) for i in range(8)]
            self.fw = FW(nc)
            self.fw.setup_sems(es)
            self.sb = Sbuf(big, SB_WORDS)
            self.ps = [Tile(p, "ps%d" % i) for i, p in enumerate(pst)]
            self._body()
            self.fw.barrier()
            self.stats = self.fw.finalize()
        return nc

    def _body(self):
        sb = self.sb
        dr = self.dr
        fw = self.fw
        self.ident = sb.alloc(128, F32, name="ident")
        self.dma("sp", self.ident[:, :], dr["c_ident"][:, :], writes=[self.ident])
        self.identb = sb.alloc(128, BF16, name="identb")
        self.dma("pool", self.identb[:, :], dr["c_ident"][:, :], writes=[self.identb])
        self.pmb = sb.alloc(128, BF16, name="pmb")
        self.dma("pool", self.pmb[:, :], dr["c_pm"][:, :], writes=[self.pmb])
        self.onesb = sb.alloc(128, BF16, name="onesb")
        fw.op("dve", lambda e: e.memset(self.onesb[:, :], 1.0), writes=[self.onesb])
        self.epsc = sb.alloc(1, F32, name="epsc")
        fw.op("dve", lambda e: e.memset(self.epsc[:, :], EPS), writes=[self.epsc])
        self.silu_c = sb.alloc(KC * 3, F32, name="silu_c")
        ctmp = sb.alloc(KC * 3, F32, name="ctmp")
        self.dma("sp", ctmp[:, :], dr["cT"].rearrange("p c e -> p (c e)"), writes=[ctmp])
        self.act(self.silu_c[:, :], ctmp[:, :], AF.Silu, reads=[ctmp], writes=[self.silu_c])
        self.modc = [sb.alloc(6 * KC * 3, F32, name="modc%d" % l) for l in range(DEPTH)]
        self.A1 = [sb.alloc(KC * 3, F32) for l in range(DEPTH)]
        self.A2 = [sb.alloc(KC * 3, F32) for l in range(DEPTH)]
        self.posT_lat = sb.alloc(16 * 32, F32, name="posT_lat")
        self.posT_ctx = sb.alloc(2 * 32, F32, name="posT_ctx")
        self.base_off = sb.off
        Xs = ["xT", "X1", "X2", "X3"]
        for l in range(DEPTH):
            last = l == DEPTH - 1
            self.cur_last = last
            self.p0_mod(l)
            if self.stop_after == "p0":
                return
            self.p1_inproj(l, dr["xT"] if l == 0 else dr["X2"])
            if self.stop_after == "p1":
                return
            self.p2_attn(l, last)
            if self.stop_after == "p2":
                return
            self.p3_rec(l)
            if self.stop_after == "p3":
                return
            if self.stop_after == "wip_DISABLED":
                self.pF_copy()
                return
            self.p4_merge(l, dr["xT"] if l == 0 else dr["X2"], dr["X1"], last)
            if self.stop_after == "p4":
                return
            self.p5_route(l, last)
            if self.stop_after == "p5":
                return
            self.p6_experts(l, last)
            if self.stop_after == "p6":
                return
            self.p7_scatter(l, dr["X1"], dr["X2"] if not last else None, last)
            if self.stop_after == "p7":
                return

    def p0_mod(self, l):
        sb, dr, fw, ps = self.sb, self.dr, self.fw, self.ps
        sb.mark()
        NB = 12
        wbuf = [sb.alloc(KC * 512, F32, name="adaw%d" % i) for i in range(2)]
        adab = sb.alloc(48, F32)
        gm = sb.alloc(KC, F32)
        gf = sb.alloc(KC, F32)
        self.dma("sp", adab[:, :], dr["ada_bT"][l], writes=[adab])
        self.dma("sp", gm[:, :], dr["gmixT"][l], writes=[gm])
        self.dma("sp", gf[:, :], dr["gffnT"][l], writes=[gf])
        pt = ps[0]
        modc = self.modc[l]
        for nb in range(NB):
            wb = wbuf[nb % 2]
            self.dma("sp" if nb % 2 == 0 else "pool", wb[:, :].rearrange("p (c n) -> p c n", c=KC),
                   dr["ada_w"][l, :, nb * 512:(nb + 1) * 512].rearrange("(c p) n -> p c n", p=128), writes=[wb])
            for jj in range(4):
                j = nb * 4 + jj
                for k in range(KC):
                    self.mm(pt, pt[:, j * 3:(j + 1) * 3], wb[:, k * 512 + jj * 128:k * 512 + (jj + 1) * 128],
                            self.silu_c[:, k * 3:(k + 1) * 3], k == 0, k == KC - 1, reads=[wb, self.silu_c])
        for e3 in range(3):
            self.tt("dve", modc[:, :].rearrange("p (j e) -> p j e", e=3)[:, :, e3],
                    pt[:, 0:144].rearrange("p (j e) -> p j e", e=3)[:, :, e3], adab[:, :], ALU.add,
                    reads=[pt, adab], pwrites=[modc])
        m4 = modc[:, :].rearrange("p (w c e) -> p w c e", w=6, c=KC)
        a1 = self.A1[l][:, :].rearrange("p (c e) -> p c e", e=3)
        a2 = self.A2[l][:, :].rearrange("p (c e) -> p c e", e=3)
        for e3 in range(3):
            self.stt(a1[:, :, e3], m4[:, 1, :, e3], 1.0, gm[:, :], ALU.add, ALU.mult, reads=[modc, gm], pwrites=[self.A1[l]])
            self.stt(a2[:, :, e3], m4[:, 4, :, e3], 1.0, gf[:, :], ALU.add, ALU.mult, reads=[modc, gf], pwrites=[self.A2[l]])
        if "modc" in self.debug:
            self.dma("sp", dr["modc"][l].rearrange("p w c e -> p (w c e)"), modc[:, :], reads=[modc])
        self.phase_end()
        sb.release()

    def mcol(self, l, which, c, e):
        i = (which * KC + c) * 3 + e
        return self.modc[l][:, i:i + 1]

    def rstd_from(self, xt_tiles, w, sqb, pt, rstd, lnv):
        for c in range(KC):
            self.act(sqb[c][:, :w], xt_tiles[c][:, :w], AF.Square, reads=[xt_tiles[c]], writes=[sqb[c]])
        for c in range(KC):
            self.mm(pt, pt[:, :w], self.onesb[:, :], sqb[c][:, :w], c == 0, c == KC - 1, reads=[self.onesb, sqb[c]])
        self.act(lnv[:, :w], pt[:, :w], AF.Ln, reads=[pt], writes=[lnv], scale=1.0 / D, bias=self.epsc[:, 0:1])
        self.act(rstd[:, :w], lnv[:, :w], AF.Exp, reads=[lnv], writes=[rstd], scale=-0.5)

    def p1_inproj(self, l, Xin):
        sb, dr, fw, ps = self.sb, self.dr, self.fw, self.ps
        sb.mark()
        W = 256
        win = sb.alloc(KC * IN_W, BF16, name="win")
        winv = win[:, :].rearrange("p (c n) -> p c n", c=KC)
        winb = [Tile(winv[:, :, nb * 512:(nb + 1) * 512], "winb%d" % nb) for nb in range(11)]
        for nb in range(11):
            self.dma("pool", winv[:, :, nb * 512:(nb + 1) * 512],
                   dr["w_in"][l, :, nb * 512:(nb + 1) * 512].rearrange("(c p) n -> p c n", p=128), writes=[winb[nb]])
        ropeC = sb.alloc(S, F32); ropeS = sb.alloc(S, F32); pmb = self.pmb
        self.dma("sp", ropeC[:, :], dr["c_ropeC"][:, :], writes=[ropeC])
        self.dma("sp", ropeS[:, :], dr["c_ropeS"][:, :], writes=[ropeS])
        xt = [[sb.alloc(W, F32) for c in range(KC)] for i in range(2)]
        sqb = [sb.alloc(W, BF16) for c in range(KC)]
        hT = [sb.alloc(W, BF16) for c in range(KC)]
        tmpf = [sb.alloc(W, F32) for i in range(2)]
        rstd = sb.alloc(W, F32); lnv = sb.alloc(W, F32)
        qraw = [sb.alloc(W, BF16) for i in range(2)]
        t1 = [sb.alloc(W, F32) for i in range(2)]
        t2 = [sb.alloc(W, F32) for i in range(2)]
        tmpe = [sb.alloc(W, F32) for i in range(2)]
        tmpe2 = [sb.alloc(W, F32) for i in range(2)]
        st_q = sb.alloc(KC * W, BF16); st_k = sb.alloc(2 * W, BF16)
        st_u = sb.alloc(KC * W, F32)
        st_g = [sb.alloc(KC * W, BF16) for i in range(3)]
        st_v = [sb.alloc(256, BF16) for i in range(2)]
        ntile = T // W
        tl = [(s, ti) for s in range(NS) for ti in range(ntile)]
        v3 = lambda t, n: t[:, :].rearrange("p (c w) -> p c w", c=n)

        def load_x(idx):
            s, ti = tl[idx]
            xb = xt[idx % 2]
            for c in range(KC):
                self.dma("sp", xb[c][:, :], Xin[s, c * 128:(c + 1) * 128, ti * W:(ti + 1) * W], writes=[xb[c]])

        load_x(0)
        for it in range(len(tl)):
            s, ti = tl[it]
            t0 = ti * W
            is_ctx = ti == 0
            e3 = 2 if is_ctx else s
            xb = xt[it % 2]
            if it + 1 < len(tl):
                load_x(it + 1)
            self.rstd_from(xb, W, sqb, ps[0], rstd, lnv)
            for c in range(KC):
                tf = tmpf[c % 2]
                self.stt(tf[:, :], xb[c][:, :], self.A1[l][:, c * 3 + e3:c * 3 + e3 + 1], rstd[:, :], ALU.mult, ALU.mult,
                         reads=[xb[c], self.A1[l], rstd], writes=[tf])
                self.act(hT[c][:, :], tf[:, :], AF.Identity, reads=[tf, self.modc[l]], writes=[hT[c]],
                         bias=self.mcol(l, 0, c, e3), scale=1.0)
            for oc in range(44):
                if 10 <= oc < 12:
                    continue
                if self.cur_last and is_ctx and (oc < 8 or oc >= 20):
                    continue
                pt = ps[1 + (oc % 4)]
                for k in range(KC):
                    self.mm(pt, pt[:, :W], winv[:, k, oc * 128:(oc + 1) * 128], hT[k][:, :], k == 0, k == KC - 1, reads=[winb[oc // 4], hT[k]])
                if oc < 10:
                    stt_, dst = (st_q, st_q[:, oc * W:(oc + 1) * W]) if oc < 8 else (st_k, st_k[:, (oc - 8) * W:(oc - 7) * W])
                    if is_ctx:
                        self.cp("act", dst, pt[:, :W], reads=[pt], pwrites=[stt_])
                    else:
                        qr_ = qraw[oc % 2]; a1 = t1[oc % 2]; a2 = t2[oc % 2]
                        p2 = ps[5 + (oc % 2)]
                        lt0 = t0 - LC
                        self.cp("act", qr_[:, :], pt[:, :W], reads=[pt], writes=[qr_])
                        self.mm(p2, p2[:, :W], pmb[:, :], qr_[:, :], True, True, reads=[pmb, qr_])
                        e1_ = tmpe[oc % 2]; e2_ = tmpe2[oc % 2]
                        self.cp("act", e1_[:, :], pt[:, :W], reads=[pt], writes=[e1_])
                        self.cp("act", e2_[:, :], p2[:, :W], reads=[p2], writes=[e2_])
                        self.tt("dve", a1[:, :], e1_[:, :], ropeC[:, lt0:lt0 + W], ALU.mult, reads=[e1_, ropeC], writes=[a1])
                        self.tt("dve", a2[:, :], e2_[:, :], ropeS[:, lt0:lt0 + W], ALU.mult, reads=[e2_, ropeS], writes=[a2])
                        self.tt("dve", dst, a1[:, :], a2[:, :], ALU.add, reads=[a1, a2], pwrites=[stt_])
                elif oc < 20:
                    self.cp("act", st_u[:, (oc - 12) * W:(oc - 11) * W], pt[:, :W], reads=[pt], pwrites=[st_u])
                else:
                    g = (oc - 20) // 8
                    c_ = (oc - 20) % 8
                    self.act(st_g[g][:, c_ * W:(c_ + 1) * W], pt[:, :W], AF.Gelu if g == 0 else AF.Sigmoid, reads=[pt], pwrites=[st_g[g]])
            for j in range(W // 128):
                pt = ps[7]
                for k in range(KC):
                    self.mm(pt, pt[:, :256], hT[k][:, j * 128:(j + 1) * 128], winv[:, k, 1280:1536], k == 0, k == KC - 1, reads=[winb[2], hT[k]])
                sv = st_v[j % 2]
                self.cp("dve", sv[:, :], pt[:, :256], reads=[pt], writes=[sv])
                self.dma("pool", dr["vtm"][s, t0 + j * 128:t0 + (j + 1) * 128, :], sv[:, :], reads=[sv])
            fm = lambda nm: dr[nm][s, :, t0:t0 + W].rearrange("(c p) w -> p c w", p=128)
            self.dma("pool", fm("qT"), v3(st_q, KC), reads=[st_q])
            self.dma("pool", dr["kT"][s, :, t0:t0 + W].rearrange("(c p) w -> p c w", p=128), v3(st_k, 2), reads=[st_k])
            self.dma("sp", fm("uT"), v3(st_u, KC), reads=[st_u])
            self.dma("sp", fm("gzT"), v3(st_g[0], KC), reads=[st_g[0]])
            self.dma("pool", fm("smaT"), v3(st_g[1], KC), reads=[st_g[1]])
            self.dma("sp", fm("smrT"), v3(st_g[2], KC), reads=[st_g[2]])
        self.phase_end()
        sb.release()

    def p2_attn(self, l, last):
        sb, dr, fw, ps = self.sb, self.dr, self.fw, self.ps
        sb.mark()
        mprev = sb.alloc(512, BF16); mnext = sb.alloc(512, BF16)
        self.dma("pool", mprev[:, :], dr["c_mprev"][:, :], writes=[mprev])
        self.dma("pool", mnext[:, :], dr["c_mnext"][:, :], writes=[mnext])
        sk = sb.alloc(8, F32); ske = sb.alloc(8, F32); onef = sb.alloc(128, F32)
        esf = sb.alloc(1024, F32); eshi = sb.alloc(1024, BF16); eslo = sb.alloc(1024, BF16)
        self.dma("sp", sk[0:1, :], dr["sink"][l], writes=[sk])
        self.act(ske[0:1, :], sk[0:1, :], AF.Exp, reads=[sk], writes=[ske])
        fw.op("dve", lambda e: e.memset(onef[:, :], 1.0), writes=[onef])
        for h in range(8):
            self.ts("dve", esf[0:1, h * 128:(h + 1) * 128], onef[0:1, :], ske[0:1, h:h + 1], ALU.mult, reads=[onef, ske], pwrites=[esf])
        self.cp("dve", eshi[0:1, :], esf[0:1, :], reads=[esf], writes=[eshi])
        self.tt("dve", eslo[0:1, :], esf[0:1, :], eshi[0:1, :], ALU.subtract, reads=[esf, eshi], writes=[eslo])
        qall = sb.alloc(8 * T, BF16, name="qall")
        qv = qall[:, :].rearrange("p (h t) -> p h t", h=8)
        kt = [sb.alloc(T, BF16) for i in range(2)]
        vt = sb.alloc(18 * 256, BF16)
        Er = [sb.alloc(512, BF16) for i in range(10)]
        lnD2 = [sb.alloc(512, F32) for i in range(2)]; rD2 = [sb.alloc(512, F32) for i in range(2)]
        ost = [sb.alloc(4 * 512, BF16) for i in range(2)]
        scale = 1.0 / np.sqrt(128.0)
        for s in range(NS):
            for h in range(8):
                self.dma("sp", qv[:, h, :], dr["qT"][s, h * 128:(h + 1) * 128, :], pwrites=[qall])
            for c in range(2):
                self.dma("sp", kt[c][:, :], dr["kT"][s, c * 128:(c + 1) * 128, :], writes=[kt[c]])
            self.dma("sp", vt[:, :].rearrange("p (b f) -> p b f", f=256), dr["vtm"][s].rearrange("(b p) f -> p b f", p=128), writes=[vt])
            qblocks = [("lat", i) for i in range(16)]
            if not last:
                qblocks = [("ctx", 0), ("ctx", 1)] + qblocks
            for kind, i in qblocks:
                if kind == "lat":
                    tq = LC + i * 128
                    keys = [(0, 0, None), (128, 1, None)]
                    if i > 0:
                        keys.append((LC + (i - 1) * 128, 2 + i - 1, mprev))
                    keys.append((LC + i * 128, 2 + i, None))
                    if i < 15:
                        keys.append((LC + (i + 1) * 128, 2 + i + 1, mnext))
                    sw, si = 512, i % 4
                else:
                    tq = i * 128
                    keys = [(0, 0, None), (128, 1, None)]
                    sw, si = 256, i
                for kv in range(2):
                    Ek = Er[kv * 5:(kv + 1) * 5]; lnD = lnD2[kv]; rD = rD2[kv]
                    for idx, (kc0, vb, mk) in enumerate(keys):
                        pS = ps[idx % 3]
                        for g in range(4):
                            self.mm(pS, pS[:, g * 128:(g + 1) * 128], kt[kv][:, kc0:kc0 + 128], qv[:, 4 * kv + g, tq:tq + 128], True, True, reads=[kt[kv], qall])
                        E = Ek[idx]
                        self.act(E[:, :], pS[:, :], AF.Exp, reads=[pS], writes=[E], scale=float(scale))
                        if mk is not None:
                            self.tt("dve", E[:, :], E[:, :], mk[:, :], ALU.mult, reads=[E, mk], writes=[E])
                    pO, pD = ps[3 + (kv % 2) * 2], ps[4 + (kv % 2) * 2]
                    n = len(keys)
                    for idx, (kc0, vb, mk) in enumerate(keys):
                        self.mm(pO, pO[:, :], vt[:, vb * 256 + kv * 128:vb * 256 + (kv + 1) * 128], Ek[idx][:, :], idx == 0, idx == n - 1, reads=[vt, Ek[idx]])
                    for idx, (kc0, vb, mk) in enumerate(keys):
                        self.mm(pD, pD[:, :], self.onesb[:, :], Ek[idx][:, :], idx == 0, False, reads=[self.onesb, Ek[idx]])
                    self.mm(pD, pD[:, :], self.onesb[0:1, :], eshi[0:1, kv * 512:(kv + 1) * 512], False, False, reads=[self.onesb, eshi])
                    self.mm(pD, pD[:, :], self.onesb[0:1, :], eslo[0:1, kv * 512:(kv + 1) * 512], False, True, reads=[self.onesb, eslo])
                    self.act(lnD[:, :], pD[:, :], AF.Ln, reads=[pD], writes=[lnD])
                    self.act(rD[:, :], lnD[:, :], AF.Exp, reads=[lnD], writes=[rD], scale=-1.0)
                    ov = ost[kv][:, :].rearrange("p (g t) -> p g t", g=4)
                    self.tt("dve", ov[:, :, si * 128:(si + 1) * 128], pO[:, :].rearrange("p (g t) -> p g t", g=4),
                            rD[:, :].rearrange("p (g t) -> p g t", g=4), ALU.mult, reads=[pO, rD], pwrites=[ost[kv]])
                if (kind == "lat" and i % 4 == 3) or (kind == "ctx" and i == 1):
                    tq0 = tq + 128 - sw
                    for kv in range(2):
                        ov = ost[kv][:, :].rearrange("p (g t) -> p g t", g=4)
                        self.dma("pool", dr["attT"][s, kv * 512:(kv + 1) * 512, tq0:tq0 + sw].rearrange("(g d) t -> d g t", d=128),
                               ov[:, :, 0:sw], reads=[ost[kv]])
        self.phase_end()
        sb.release()

    def p3_rec(self, l):
        sb, dr, fw, ps = self.sb, self.dr, self.fw, self.ps
        sb.mark()
        lw = sb.alloc(2 * 2 * 8 * 128, BF16, name="lw")
        lwv = lw[:, :].rearrange("p (w d b j) -> p w d b j", w=2, d=2, b=8)
        for w_ in range(2):
            for d_ in range(2):
                self.dma("pool", lwv[:, w_, d_, :, :], dr["lru_w"][l, w_, d_].rearrange("b i j -> i b j"), pwrites=[lw])
        lv = sb.alloc(48, F32); cv = sb.alloc(40, F32)
        self.dma("sp", lv[:, :], dr["lru_vT"][l].rearrange("p a d c -> p (a d c)"), writes=[lv])
        self.dma("sp", cv[:, :], dr["convT"][l].rearrange("p a c -> p (a c)"), writes=[cv])
        onec = sb.alloc(1, F32)
        fw.op("dve", lambda e: e.memset(onec[:, :], 1.0), writes=[onec])
        e1 = sb.alloc(16, F32); l1 = sb.alloc(16, F32); cl = sb.alloc(16, F32)
        self.act(e1[:, :], lv[:, 32:48], AF.Exp, reads=[lv], writes=[e1], scale=-1.0)
        self.act(l1[:, :], e1[:, :], AF.Ln, reads=[e1, onec], writes=[l1], bias=onec[:, 0:1], scale=1.0)
        self.ts("dve", cl[:, :], l1[:, :], -8.0, ALU.mult, reads=[l1], writes=[cl])
        bu = [dict(u=sb.alloc(T, F32), uc=sb.alloc(T, F32), ucb=sb.alloc(T, BF16), gz=sb.alloc(T, BF16)) for i in range(2)]
        bd = [dict(r=sb.alloc(T, F32), gi=sb.alloc(T, F32), a=sb.alloc(T, F32)) for d_ in range(2)]
        hf = sb.alloc(T, F32); hb = sb.alloc(T, F32); og = sb.alloc(T, BF16)
        tiles = [(0, 256)] + [(256 + 512 * i, 512) for i in range(4)]
        items = [(s, c) for s in range(NS) for c in range(KC)]

        def conv_act(i):
            s, c = items[i]
            B = bu[i % 2]
            u, uc, gz = B["u"], B["uc"], B["gz"]
            self.dma("sp", u[:, :], dr["uT"][s, c * 128:(c + 1) * 128, :], writes=[u])
            self.dma("sp", gz[:, :], dr["gzT"][s, c * 128:(c + 1) * 128, :], writes=[gz])
            self.act(uc[:, :], u[:, :], AF.Identity, reads=[u, cv], writes=[uc], scale=cv[:, 2 * KC + c:2 * KC + c + 1], bias=cv[:, 4 * KC + c:4 * KC + c + 1])

        def conv_dve(i):
            s, c = items[i]
            B = bu[i % 2]
            u, uc, ucb = B["u"], B["uc"], B["ucb"]
            for k, d in ((0, -2), (1, -1), (3, 1)):
                for (sa, sb_) in ((0, LC), (LC, T)):
                    lo = max(sa, sa - d); hi = min(sb_, sb_ - d)
                    self.stt(uc[:, lo:hi], u[:, lo + d:hi + d], cv[:, k * KC + c:k * KC + c + 1], uc[:, lo:hi], ALU.mult, ALU.add,
                             reads=[u, cv, uc], pwrites=[uc])
            self.cp("dve", ucb[:, :], uc[:, :], reads=[uc], writes=[ucb])

        def gates(i, d_):
            s, c = items[i]
            ucb = bu[i % 2]["ucb"]
            r, gi, a = bd[d_]["r"], bd[d_]["gi"], bd[d_]["a"]
            for ti, (t0, w) in enumerate(tiles):
                pA = ps[(2 * ti) % 8]; pX = ps[(2 * ti + 1) % 8]
                self.mm(pA, pA[:, :w], lwv[:, 0, d_, c, :], ucb[:, t0:t0 + w], True, True, reads=[lw, ucb])
                self.mm(pX, pX[:, :w], lwv[:, 1, d_, c, :], ucb[:, t0:t0 + w], True, True, reads=[lw, ucb])
                self.act(r[:, t0:t0 + w], pA[:, :w], AF.Sigmoid, reads=[pA, lv], pwrites=[r], bias=lv[:, (0 * 2 + d_) * KC + c:(0 * 2 + d_) * KC + c + 1], scale=1.0)
                self.act(gi[:, t0:t0 + w], pX[:, :w], AF.Sigmoid, reads=[pX, lv], pwrites=[gi], bias=lv[:, (1 * 2 + d_) * KC + c:(1 * 2 + d_) * KC + c + 1], scale=1.0)
            self.act(a[:, :], r[:, :], AF.Exp, reads=[r, cl], writes=[a], scale=cl[:, d_ * KC + c:d_ * KC + c + 1])
            self.act(r[:, :], a[:, :], AF.Square, reads=[a], writes=[r])
            self.act(r[:, :], r[:, :], AF.Sqrt, reads=[r, onec], writes=[r], scale=-1.0, bias=onec[:, 0:1])

        def scan_dve(i, d_):
            uc = bu[i % 2]["uc"]
            q_, gi, a = bd[d_]["r"], bd[d_]["gi"], bd[d_]["a"]
            self.tt("dve", gi[:, :], gi[:, :], uc[:, :], ALU.mult, reads=[gi, uc], writes=[gi])
            self.tt("dve", gi[:, :], gi[:, :], q_[:, :], ALU.mult, reads=[gi, q_], writes=[gi])
            if d_ == 0:
                fw.op("dve", lambda e, a=a, gi=gi: e.tensor_tensor_scan(out=hf[:, :], data0=a[:, :], data1=gi[:, :], initial=0.0, op0=ALU.mult, op1=ALU.add),
                      reads=[a, gi], writes=[hf])
            else:
                fw.op("dve", lambda e, a=a, gi=gi: e.tensor_tensor_scan(out=hb[:, LC - 1::-1], data0=a[:, LC - 1::-1], data1=gi[:, LC - 1::-1], initial=0.0, op0=ALU.mult, op1=ALU.add),
                      reads=[a, gi], writes=[hb])
                fw.op("dve", lambda e, a=a, gi=gi: e.tensor_tensor_scan(out=hb[:, T - 1:LC - 1:-1], data0=a[:, T - 1:LC - 1:-1], data1=gi[:, T - 1:LC - 1:-1], initial=hb[:, 0:1], op0=ALU.mult, op1=ALU.add),
                      reads=[a, gi, hb], pwrites=[hb])

        def tail(i):
            s, c = items[i]
            gz = bu[i % 2]["gz"]
            self.tt("dve", hf[:, :], hf[:, :], hb[:, :], ALU.add, reads=[hf, hb], writes=[hf])
            self.tt("dve", og[:, :], hf[:, :], gz[:, :], ALU.mult, reads=[hf, gz], writes=[og])
            self.dma("pool", dr["rgT"][s, c * 128:(c + 1) * 128, :], og[:, :], reads=[og])

        n_it = len(items)
        conv_act(0)
        conv_dve(0)
        for i in range(n_it):
            if i + 1 < n_it:
                conv_act(i + 1)
            gates(i, 0)
            if i + 1 < n_it:
                conv_dve(i + 1)
            gates(i, 1)
            scan_dve(i, 0)
            scan_dve(i, 1)
            tail(i)
        self.phase_end()
        sb.release()

    def pF_copy(self):
        sb, dr, fw = self.sb, self.dr, self.fw
        sb.mark()
        buf = [sb.alloc(S, F32) for i in range(2)]
        i = 0
        for s in range(NS):
            for c in range(KC):
                b = buf[i % 2]
                self.dma("sp", b[:, :], dr["xT"][s, c * 128:(c + 1) * 128, LC:T], writes=[b])
                self.dma("sp", dr["outT"][s, c * 128:(c + 1) * 128, :], b[:, :], reads=[b])
                i += 1
        self.phase_end()
        sb.release()

    def p4_merge(self, l, Xin, Xout, last):
        sb, dr, fw, ps = self.sb, self.dr, self.fw, self.ps
        sb.mark()
        W = 512
        wts = []
        for name in ("w_attn_br", "w_rec_br", "w_out"):
            wt = sb.alloc(KC * D, BF16, name=name)
            wv = wt[:, :].rearrange("p (c n) -> p c n", c=KC)
            halves = [Tile(wv[:, :, hh * 512:(hh + 1) * 512], name + "_h%d" % hh) for hh in range(2)]
            for hh in range(2):
                self.dma("pool", wv[:, :, hh * 512:(hh + 1) * 512], dr[name][l, :, hh * 512:(hh + 1) * 512].rearrange("(c p) n -> p c n", p=128), writes=[halves[hh]])
            wts.append((halves, wv))
        (wa, wav), (wr, wrv), (wo, wov) = wts
        wrt = sb.alloc(KC * NE, F32)
        wrtv = wrt[:, :].rearrange("p (c e) -> p c e", c=KC)
        self.dma("sp", wrtv, dr["w_router"][l].rearrange("(c p) e -> p c e", p=128), writes=[wrt])
        att = sb.alloc(KC * W, BF16); rg = sb.alloc(KC * W, BF16); sma = sb.alloc(KC * W, BF16); smr = sb.alloc(KC * W, BF16)
        xin = sb.alloc(KC * W, F32)
        mg = [sb.alloc(W, BF16) for c in range(KC)]
        xn = [sb.alloc(W, F32) for c in range(KC)]
        sqb = [sb.alloc(W, BF16) for c in range(KC)]
        h2f = [sb.alloc(W, F32) for c in range(KC)]
        h2b = [sb.alloc(W, BF16) for c in range(KC)]
        tm1 = [sb.alloc(W, F32) for i in range(2)]; tm2 = [sb.alloc(W, F32) for i in range(2)]
        rstd = sb.alloc(W, F32); lnv = sb.alloc(W, F32)
        lgs = sb.alloc(W, F32)
        stg = [sb.alloc(D, BF16) for i in range(2)]
        v3 = lambda t: t[:, :].rearrange("p (c w) -> p c w", c=KC)
        tiles4 = [(0, 256)] + [(LC + 512 * i, 512) for i in range(4)]
        for s in range(NS):
            for ti, (t0, w) in enumerate(tiles4):
                if last and ti == 0:
                    continue
                e3 = 2 if ti == 0 else s
                for (tl, nm) in ((att, "attT"), (rg, "rgT"), (sma, "smaT"), (smr, "smrT")):
                    self.dma("sp", v3(tl)[:, :, :w], dr[nm][s, :, t0:t0 + w].rearrange("(c p) w -> p c w", p=128), writes=[tl])
                self.dma("sp", v3(xin)[:, :, :w], Xin[s, :, t0:t0 + w].rearrange("(c p) w -> p c w", p=128), writes=[xin])
                for m in range(KC):
                    pA = ps[(2 * m) % 4]; pR = ps[(2 * m + 1) % 4]
                    for k in range(KC):
                        self.mm(pA, pA[:, :w], wav[:, k, m * 128:(m + 1) * 128], v3(att)[:, k, :w], k == 0, k == KC - 1, reads=[wa[m // 4], att])
                    for k in range(KC):
                        self.mm(pR, pR[:, :w], wrv[:, k, m * 128:(m + 1) * 128], v3(rg)[:, k, :w], k == 0, k == KC - 1, reads=[wr[m // 4], rg])
                    a1 = tm1[m % 2]; a2 = tm2[m % 2]
                    self.tt("dve", a1[:, :w], pA[:, :w], v3(sma)[:, m, :w], ALU.mult, reads=[pA, sma], writes=[a1])
                    self.tt("dve", a2[:, :w], pR[:, :w], v3(smr)[:, m, :w], ALU.mult, reads=[pR, smr], writes=[a2])
                    self.tt("dve", mg[m][:, :w], a1[:, :w], a2[:, :w], ALU.add, reads=[a1, a2], writes=[mg[m]])
                for m in range(KC):
                    pD = ps[4 + (m % 2)]
                    for k in range(KC):
                        self.mm(pD, pD[:, :w], wov[:, k, m * 128:(m + 1) * 128], mg[k][:, :w], k == 0, k == KC - 1, reads=[wo[m // 4], mg[k]])
                    self.stt(xn[m][:, :w], pD[:, :w], self.mcol(l, 2, m, e3), v3(xin)[:, m, :w], ALU.mult, ALU.add, reads=[pD, self.modc[l], xin], writes=[xn[m]])
                    self.dma("pool", Xout[s, m * 128:(m + 1) * 128, t0:t0 + w], xn[m][:, :w], reads=[xn[m]])
                self.rstd_from(xn, w, sqb, ps[6], rstd, lnv)
                for m in range(KC):
                    tf = tm1[m % 2]
                    self.stt(tf[:, :w], xn[m][:, :w], self.A2[l][:, m * 3 + e3:m * 3 + e3 + 1], rstd[:, :w], ALU.mult, ALU.mult, reads=[xn[m], self.A2[l], rstd], writes=[tf])
                    self.act(h2f[m][:, :w], tf[:, :w], AF.Identity, reads=[tf, self.modc[l]], writes=[h2f[m]], bias=self.mcol(l, 3, m, e3), scale=1.0)
                    self.cp("act", h2b[m][:, :w], h2f[m][:, :w], reads=[h2f[m]], writes=[h2b[m]])
                pL = ps[7]
                for k in range(KC):
                    self.mm(pL, pL[0:NE, :w], wrtv[:, k, :], h2f[k][:, :w], k == 0, k == KC - 1, reads=[wrt, h2f[k]])
                self.cp("act", lgs[0:NE, :w], pL[0:NE, :w], reads=[pL], writes=[lgs])
                self.dma("pool", dr["lgT"][s, :, t0:t0 + w], lgs[0:NE, :w], reads=[lgs])
                for j in range(w // 128):
                    pT = ps[2 + j % 2]
                    pTb = pT[:, :].bitcast(BF16)
                    for m in range(KC):
                        self.tr(pT, pTb[:, m * 128:(m + 1) * 128], h2b[m][:, j * 128:(j + 1) * 128], self.identb[:, :], reads=[h2b[m], self.identb])
                    sg = stg[j % 2]
                    self.cp("act", sg[:, :], pTb[:, 0:D], reads=[pT], writes=[sg])
                    self.dma("pool", dr["h2tm"][s, t0 + j * 128:t0 + (j + 1) * 128, :], sg[:, :], reads=[sg])
        self.phase_end()
        sb.release()

    def p5_route(self, l, last):
        sb, dr, fw, ps = self.sb, self.dr, self.fw, self.ps
        sb.mark()
        bd = sb.alloc(32, F32)
        self.dma("sp", bd[0:32, :], dr["c_bdones"][:, :], writes=[bd])
        lg = sb.alloc(S, F32); E = sb.alloc(S, F32); rs = sb.alloc(S, F32); aff = sb.alloc(S, F32); work = sb.alloc(S, F32)
        m8 = sb.alloc(8, F32); mask = sb.alloc(S, F32); pin = sb.alloc(S, F32); posm = sb.alloc(S, F32); onesf = sb.alloc(S, F32)
        fw.op("dve", lambda e: e.memset(onesf[:, :], 1.0), writes=[onesf])
        groups = [(0, LC, S, CAPL, self.posT_lat)]
        if not last:
            groups.append((1, 0, LC, CAPC, self.posT_ctx))
        P = 32
        for gi, a0, n, cap, posT in groups:
            self.dma("sp", lg[0:P, :n], dr["lgT"][:, :, a0:a0 + n].rearrange("s e t -> (s e) t"), writes=[lg])
            self.act(E[0:P, :n], lg[0:P, :n], AF.Exp, reads=[lg], writes=[E])
            for c0 in range(0, n, 512):
                w = min(512, n - c0)
                pS = ps[(c0 // 512) % 2]
                self.mm(pS, pS[0:P, :w], bd[0:P, 0:P], E[0:P, c0:c0 + w], True, True, reads=[bd, E])
                fw.op("dve", lambda e, c0=c0, w=w, pS=pS: e.reciprocal(out=rs[0:P, c0:c0 + w], in_=pS[0:P, :w]), reads=[pS], pwrites=[rs])
            self.tt("dve", aff[0:P, :n], E[0:P, :n], rs[0:P, :n], ALU.mult, reads=[E, rs], writes=[aff])
            self.cp("dve", work[0:P, :n], aff[0:P, :n], reads=[aff], writes=[work])
            rounds = cap // 8
            for r_ in range(rounds):
                fw.op("dve", lambda e, n=n: e.max(out=m8[0:P, :], in_=work[0:P, :n]), reads=[work], writes=[m8])
                if r_ < rounds - 1:
                    fw.op("dve", lambda e, n=n: e.match_replace(out=work[0:P, :n], in_to_replace=m8[0:P, :], in_values=work[0:P, :n], imm_value=-1.0),
                          reads=[m8, work], writes=[work])
            self.ts("dve", mask[0:P, :n], aff[0:P, :n], m8[0:P, 7:8], ALU.is_ge, reads=[aff, m8], writes=[mask])
            fw.op("dve", lambda e, n=n: e.tensor_tensor_scan(out=pin[0:P, :n], data0=onesf[0:P, :n], data1=mask[0:P, :n], initial=0.0, op0=ALU.mult, op1=ALU.add),
                  reads=[onesf, mask], writes=[pin])
            self.tt("dve", pin[0:P, :n], pin[0:P, :n], mask[0:P, :n], ALU.mult, reads=[pin, mask], writes=[pin])
            self.ts("dve", posm[0:P, :n], pin[0:P, :n], -1.0, ALU.add, reads=[pin], writes=[posm])
            self.dma("sp", dr["rt"][gi, :, 0, 0:n], aff[0:P, :n], reads=[aff])
            self.dma("sp", dr["rt"][gi, :, 1, 0:n], posm[0:P, :n], reads=[posm])
            self.dma("sp", dr["rt"][gi, :, 2, 0:n], mask[0:P, :n], reads=[mask])
            pT = ps[2]
            ntc = n // 128
            for tc in range(ntc):
                self.tr(pT, pT[:, tc * 32:(tc + 1) * 32], posm[0:P, tc * 128:(tc + 1) * 128], self.ident[0:P, 0:P], reads=[posm, self.ident])
            self.cp("act", posT[:, 0:ntc * 32], pT[:, 0:ntc * 32], reads=[pT], writes=[posT])
        self.phase_end()
        sb.release()

    def p6_experts(self, l, last):
        self.p6a_gather(l, last)
        self.p6b_ffn(l, last)

    def p6a_gather(self, l, last):
        sb, dr, fw, ps = self.sb, self.dr, self.fw, self.ps
        sb.mark()
        NCX = 0 if last else 2 * CAPC
        NX = 512 + NCX
        iota_f = sb.alloc(256, F32)
        self.dma("sp", iota_f[:, :], dr["c_iota_f"][:, :], writes=[iota_f])
        h2l = []
        h2c = []
        for s in range(NS):
            t_ = sb.alloc(16 * D, BF16, name="h2l")
            tv = t_[:, :].rearrange("p (c d) -> p c d", c=16)
            for q4 in range(4):
                self.dma("sp", tv[:, q4 * 4:(q4 + 1) * 4, :], dr["h2tm"][s, LC + q4 * 512:LC + (q4 + 1) * 512, :].rearrange("(c p) d -> p c d", p=128), pwrites=[t_])
            h2l.append((t_, tv))
            if not last:
                c_ = sb.alloc(2 * D, BF16, name="h2c")
                cv_ = c_[:, :].rearrange("p (c d) -> p c d", c=2)
                self.dma("sp", cv_, dr["h2tm"][s, 0:LC, :].rearrange("(c p) d -> p c d", p=128), writes=[c_])
                h2c.append((c_, cv_))
        xeT = [[sb.alloc(NX, BF16) for k in range(KC)] for i in range(2)]
        sel = [[sb.alloc(256, BF16) for i in range(16)] for j in range(2)]
        selc = [sb.alloc(32, BF16) for i in range(2)]
        it = 0
        for e in range(NE):
            xb = xeT[e % 2]
            for s in range(NS):
                col = s * NE + e
                sl = sel[it % 2]
                it += 1
                for tc in range(16):
                    self.ts("dve", sl[tc][:, :], iota_f[:, 0:256], self.posT_lat[:, tc * 32 + col:tc * 32 + col + 1], ALU.is_equal,
                            reads=[iota_f, self.posT_lat], writes=[sl[tc]])
                for m in range(KC):
                    pX = ps[m % 4]
                    for tc in range(16):
                        self.mm(pX, pX[:, :256], h2l[s][1][:, tc, m * 128:(m + 1) * 128], sl[tc][:, :], tc == 0, tc == 15, reads=[h2l[s][0], sl[tc]])
                    self.cp("act", xb[m][:, s * 256:(s + 1) * 256], pX[:, :256], reads=[pX], pwrites=[xb[m]])
                if NCX:
                    for tc in range(2):
                        self.ts("dve", selc[tc][:, :], iota_f[:, 0:32], self.posT_ctx[:, tc * 32 + col:tc * 32 + col + 1], ALU.is_equal,
                                reads=[iota_f, self.posT_ctx], writes=[selc[tc]])
                    for m in range(KC):
                        pX = ps[4 + m % 4]
                        for tc in range(2):
                            self.mm(pX, pX[:, :32], h2c[s][1][:, tc, m * 128:(m + 1) * 128], selc[tc][:, :], tc == 0, tc == 1, reads=[h2c[s][0], selc[tc]])
                        self.cp("act", xb[m][:, 512 + s * 32:512 + (s + 1) * 32], pX[:, :32], reads=[pX], pwrites=[xb[m]])
            for m in range(KC):
                self.dma("sp", dr["xe"][e, m * 128:(m + 1) * 128, 0:NX], xb[m][:, :], reads=[xb[m]])
        self.phase_end()
        sb.release()

    def p6b_ffn(self, l, last):
        sb, dr, fw, ps = self.sb, self.dr, self.fw, self.ps
        sb.mark()
        NCX = 0 if last else 2 * CAPC
        NX = 512 + NCX
        NR = 4
        wring = [sb.alloc(8192, BF16, name="wring%d" % i) for i in range(NR)]
        xeb = [sb.alloc(KC * NX, BF16) for i in range(2)]
        hid = [sb.alloc(NX, BF16) for f in range(16)]
        sgt = [sb.alloc(512, F32) for i in range(2)]
        sgc = [sb.alloc(64, F32) for i in range(2)]
        yst = [sb.alloc(512, BF16) for i in range(2)]
        units = []
        for e in range(NE):
            for fh in range(2):
                units.append(("w_gate", e, fh)); units.append(("w_up", e, fh))
            for dh in range(2):
                units.append(("w_down", e, dh))
        loaded = {}

        def issue(ui):
            if ui >= len(units) or ui in loaded:
                return
            nm, e, hh = units[ui]
            wt = wring[ui % NR]
            if nm == "w_down":
                self.dma("pool", wt[:, :].rearrange("p (c n) -> p c n", c=16), dr[nm][l, e, :, hh * 512:(hh + 1) * 512].rearrange("(c p) n -> p c n", p=128), writes=[wt])
            else:
                self.dma("pool", wt[:, :].rearrange("p (c n) -> p c n", c=KC), dr[nm][l, e, :, hh * 1024:(hh + 1) * 1024].rearrange("(c p) n -> p c n", p=128), writes=[wt])
            loaded[ui] = wt

        for ui in range(NR - 1):
            issue(ui)
        ui = 0
        yi = 0
        for e in range(NE):
            xt_ = xeb[e % 2]
            xv = xt_[:, :].rearrange("p (k n) -> p k n", k=KC)
            if e == 0:
                self.dma("sp", xv, dr["xe"][e, :, 0:NX].rearrange("(k p) n -> p k n", p=128), writes=[xt_])
            if e + 1 < NE:
                xn_ = xeb[(e + 1) % 2]
                self.dma("sp", xn_[:, :].rearrange("p (k n) -> p k n", k=KC), dr["xe"][e + 1, :, 0:NX].rearrange("(k p) n -> p k n", p=128), writes=[xn_])
            for fh in range(2):
                for uj in range(ui, ui + NR):
                    issue(uj)
                wg = loaded[ui]; wu = loaded[ui + 1]; ui += 2
                wgv = wg[:, :].rearrange("p (c n) -> p c n", c=KC)
                wuv = wu[:, :].rearrange("p (c n) -> p c n", c=KC)
                for f in range(8):
                    fi = fh * 8 + f
                    pG, pU, pGc, pUc = ps[fi % 2], ps[2 + fi % 2], ps[4], ps[5]
                    for k in range(KC):
                        self.mm(pG, pG[:, :512], wgv[:, k, f * 128:(f + 1) * 128], xv[:, k, 0:512], k == 0, k == KC - 1, reads=[wg, xt_])
                    for k in range(KC):
                        self.mm(pU, pU[:, :512], wuv[:, k, f * 128:(f + 1) * 128], xv[:, k, 0:512], k == 0, k == KC - 1, reads=[wu, xt_])
                    if NCX:
                        for k in range(KC):
                            self.mm(pGc, pGc[:, :NCX], wgv[:, k, f * 128:(f + 1) * 128], xv[:, k, 512:NX], k == 0, k == KC - 1, reads=[wg, xt_])
                        for k in range(KC):
                            self.mm(pUc, pUc[:, :NCX], wuv[:, k, f * 128:(f + 1) * 128], xv[:, k, 512:NX], k == 0, k == KC - 1, reads=[wu, xt_])
                    sg = sgt[fi % 2]
                    self.act(sg[:, :], pG[:, :512], AF.Silu, reads=[pG], writes=[sg])
                    self.tt("dve", hid[fi][:, 0:512], sg[:, :], pU[:, :512], ALU.mult, reads=[sg, pU], pwrites=[hid[fi]])
                    if NCX:
                        sc_ = sgc[fi % 2]
                        self.act(sc_[:, :], pGc[:, :NCX], AF.Silu, reads=[pGc], writes=[sc_])
                        self.tt("dve", hid[fi][:, 512:NX], sc_[:, :], pUc[:, :NCX], ALU.mult, reads=[sc_, pUc], pwrites=[hid[fi]])
            for dh in range(2):
                for uj in range(ui, ui + NR):
                    issue(uj)
                wd = loaded[ui]; ui += 1
                wdv = wd[:, :].rearrange("p (c n) -> p c n", c=16)
                rgs = [(s * 256 + c * 128, 128, ("ye", s, c)) for s in range(NS) for c in range(2)]
                if NCX:
                    rgs.append((512, NCX, ("yec",)))
                for (c0, M, dst) in rgs:
                    pY = ps[6 + yi % 2]
                    ys = yst[yi % 2]
                    yi += 1
                    for f in range(16):
                        self.mm(pY, pY[0:M, :512], hid[f][:, c0:c0 + M], wdv[:, f, :], f == 0, f == 15, reads=[hid[f], wd])
                    self.cp("act", ys[0:M, :], pY[0:M, :512], reads=[pY], writes=[ys])
                    if dst[0] == "ye":
                        self.dma("sp", dr["ye"][dst[1], e, dst[2] * 128:(dst[2] + 1) * 128, dh * 512:(dh + 1) * 512], ys[0:128, :], reads=[ys])
                    else:
                        for s in range(NS):
                            self.dma("sp", dr["yec"][s, e, :, dh * 512:(dh + 1) * 512], ys[s * CAPC:(s + 1) * CAPC, :], reads=[ys])
        self.phase_end()
        sb.release()

    def p7_scatter(self, l, Xm, Xout, last):
        sb, dr, fw, ps = self.sb, self.dr, self.fw, self.ps
        sb.mark()
        W = 512
        rowsel = sb.alloc(32 * 128, BF16)
        self.dma("pool", rowsel[0:32, :], dr["c_rowsel"][:, :], writes=[rowsel])
        iota_p = sb.alloc(2, F32)
        self.dma("sp", iota_p[:, :], dr["c_iota_p"][:, :], writes=[iota_p])
        gfin = sb.alloc(KC, F32)
        self.dma("sp", gfin[:, :], dr["gfinT"][:, :], writes=[gfin])
        affr = sb.alloc(S, BF16); posr = sb.alloc(S, BF16)
        yeall = sb.alloc(NE * 2 * D, BF16, name="yeall")
        selT = [sb.alloc(W, BF16) for i in range(32)]
        affb = [sb.alloc(W, F32) for i in range(2)]
        xin = sb.alloc(KC * W, F32)
        xn = [sb.alloc(W, F32) for c in range(KC)]
        sqb = [sb.alloc(W, BF16) for c in range(KC)]
        ot = [sb.alloc(W, F32) for i in range(2)]
        rstd = sb.alloc(W, F32); lnv = sb.alloc(W, F32)
        groups = [(0, LC, S, 128, 2)]
        if not last:
            groups.append((1, 0, LC, CAPC, 1))
        v3 = lambda t: t[:, :].rearrange("p (c w) -> p c w", c=KC)
        for gi, a0, n, NP, ncc in groups:
            self.dma("pool", affr[0:32, :n], dr["rt"][gi, :, 0, 0:n], writes=[affr])
            self.dma("pool", posr[0:32, :n], dr["rt"][gi, :, 1, 0:n], writes=[posr])
            for s in range(NS):
                e3 = s if gi == 0 else 2
                if gi == 0:
                    yv = yeall[:, :].rearrange("p (e c d) -> p e c d", e=NE, c=2)
                    for q8 in range(8):
                        self.dma("sp", yv[:, q8 * 2:(q8 + 1) * 2, :, :], dr["ye"][s, q8 * 2:(q8 + 1) * 2, :, :].rearrange("e (c p) d -> p e c d", p=128), pwrites=[yeall])
                else:
                    yv = yeall[:, 0:NE * D].rearrange("p (e c d) -> p e c d", e=NE, c=1)
                    self.dma("sp", yv[0:CAPC, :, 0, :], dr["yec"][s].rearrange("e j d -> j e d"), writes=[yeall])
                for c0 in range(0, n, W):
                    w = min(W, n - c0)
                    for e in range(NE):
                        r_ = s * NE + e
                        pP = ps[e % 2]; pA = ps[2 + e % 2]
                        self.mm(pP, pP[:, :w], rowsel[0:32, r_ * 128:(r_ + 1) * 128], posr[0:32, c0:c0 + w], True, True, reads=[rowsel, posr])
                        self.mm(pA, pA[:, :w], rowsel[0:32, r_ * 128:(r_ + 1) * 128], affr[0:32, c0:c0 + w], True, True, reads=[rowsel, affr])
                        ab = affb[e % 2]
                        self.cp("act", ab[:, :w], pA[:, :w], reads=[pA], writes=[ab])
                        for cc in range(ncc):
                            st_ = selT[e * 2 + cc]
                            self.stt(st_[0:NP, :w], pP[0:NP, :w], iota_p[0:NP, cc:cc + 1], ab[0:NP, :w], ALU.is_equal, ALU.mult, reads=[pP, iota_p, ab], writes=[st_])
                    self.dma("sp", v3(xin)[:, :, :w], Xm[s, :, a0 + c0:a0 + c0 + w].rearrange("(c p) w -> p c w", p=128), writes=[xin])
                    for m in range(KC):
                        pY = ps[4 + m % 2]
                        nmm = NE * ncc
                        idx = 0
                        for e in range(NE):
                            for cc in range(ncc):
                                self.mm(pY, pY[:, :w], yv[0:NP, e, cc, m * 128:(m + 1) * 128], selT[e * 2 + cc][0:NP, :w], idx == 0, idx == nmm - 1, reads=[yeall, selT[e * 2 + cc]])
                                idx += 1
                        self.stt(xn[m][:, :w], pY[:, :w], self.mcol(l, 5, m, e3), v3(xin)[:, m, :w], ALU.mult, ALU.add, reads=[pY, self.modc[l], xin], writes=[xn[m]])
                        if not last:
                            self.dma("pool", Xout[s, m * 128:(m + 1) * 128, a0 + c0:a0 + c0 + w], xn[m][:, :w], reads=[xn[m]])
                    if last:
                        self.rstd_from(xn, w, sqb, ps[6], rstd, lnv)
                        for m in range(KC):
                            o_ = ot[m % 2]
                            self.stt(o_[:, :w], xn[m][:, :w], gfin[:, m:m + 1], rstd[:, :w], ALU.mult, ALU.mult, reads=[xn[m], gfin, rstd], writes=[o_])
                            self.dma("pool", dr["outT"][s, m * 128:(m + 1) * 128, c0:c0 + w], o_[:, :w], reads=[o_])
        self.phase_end()
        sb.release()

def prep_shared(inp):
    f = lambda a: np.ascontiguousarray(a, dtype=np.float32)
    sh = {}
    sh["ada_w"] = f(inp["ada_w"])
    sh["ada_bT"] = f(inp["ada_b"].reshape(DEPTH, 48, 128).transpose(0, 2, 1))
    sh["gmixT"] = f(inp["norm_mix_g"].reshape(DEPTH, KC, 128).transpose(0, 2, 1))
    sh["gffnT"] = f(inp["norm_ffn_g"].reshape(DEPTH, KC, 128).transpose(0, 2, 1))
    sh["gfinT"] = f(inp["final_norm_g"].reshape(KC, 128).T)
    sh["w_in"] = f(inp["w_in"])
    sh["sink"] = f(inp["attn_sink"].reshape(DEPTH, 1, 8))
    cw = np.concatenate([inp["conv_w"], inp["conv_b"][:, None, :]], axis=1)
    sh["convT"] = f(cw.reshape(DEPTH, 5, KC, 128).transpose(0, 3, 1, 2))
    sh["lru_w"] = f(np.stack([inp["lru_wa"], inp["lru_wx"]], axis=1))
    lv = np.stack([inp["lru_ba"], inp["lru_bx"], inp["lru_lambda"]], axis=1)
    sh["lru_vT"] = f(lv.reshape(DEPTH, 3, 2, KC, 128).transpose(0, 4, 1, 2, 3))
    for k in ("w_attn_br", "w_rec_br", "w_out", "w_router", "w_gate", "w_up", "w_down"):
        sh[k] = f(inp[k])
    for k, v in host_consts().items():
        sh["c_" + k] = f(v)
    return sh


def prep_core(inp, core):
    b0 = core * NS
    xs = inp["x"][b0:b0 + NS]
    cs = inp["ctx"][b0:b0 + NS]
    xT = np.concatenate([cs, xs], axis=1).transpose(0, 2, 1)
    cc = np.stack([inp["c"][b0], inp["c"][b0 + 1], inp["c_ctx"]], axis=1)
    return {"xT": np.ascontiguousarray(xT, dtype=np.float32),
            "cT": np.ascontiguousarray(cc.reshape(KC, 128, 3).transpose(1, 0, 2), dtype=np.float32)}


_NC_CACHE = {}


def kernel(**inputs):
    inp = {k: np.asarray(v) for k, v in inputs.items()}
    n = 8
    if "nc" not in _NC_CACHE:
        kb = KB()
        _NC_CACHE["nc"] = kb.build()
        _NC_CACHE["names"] = set(kb.dr.keys())
    nc = _NC_CACHE["nc"]
    sh = prep_shared(inp)
    in_maps = []
    for core in range(n):
        m = dict(sh)
        m.update(prep_core(inp, core))
        in_maps.append({k: v for k, v in m.items() if k in _NC_CACHE["names"]})
    res = run_bass_kernel_spmd(nc, in_maps, core_ids=list(range(n)))
    outs = [np.asarray(r["outT"]).transpose(0, 2, 1) for r in res.results]
    return np.ascontiguousarray(np.concatenate(outs, axis=0), dtype=np.float32)
```

```python
import numpy as np
import concourse.bass as bass
import concourse.mybir as mybir

F32 = mybir.dt.float32
BF16 = mybir.dt.bfloat16
I32 = mybir.dt.int32
AF = mybir.ActivationFunctionType
ALU = mybir.AluOpType
AX = mybir.AxisListType
from concourse.bass_utils import run_bass_kernel_spmd
ENGS = ("pe", "act", "dve", "pool", "sp")


class Op:
    __slots__ = ("eng", "emit", "deps", "dma_deps", "signal", "count", "is_dma", "dsem", "dval", "prev_dval", "idx")
    _ctr = 0

    def __init__(self, eng, emit, is_dma=False):
        self.eng = eng
        self.emit = emit
        self.deps = {}
        self.dma_deps = []
        self.signal = False
        self.count = 0
        self.is_dma = is_dma
        self.dsem = None
        self.dval = 0
        self.prev_dval = 0
        Op._ctr += 1
        self.idx = Op._ctr


class Tile:
    def __init__(self, ap, name=""):
        self.ap = ap
        self.name = name
        self.w_c = {}
        self.w_d = []
        self.r_c = {}
        self.r_d = []
        self.g_c = {}
        self.g_d = []

    def __getitem__(self, k):
        return self.ap[k]


def _merge(dst_c, dst_d, src_c, src_d):
    for e, o in src_c.items():
        if e not in dst_c or dst_c[e].idx < o.idx:
            dst_c[e] = o
    for o in src_d:
        if o not in dst_d:
            dst_d.append(o)


class FW:
    def __init__(self, nc, n_dma_sems=4):
        self.nc = nc
        self.ops = {e: [] for e in ENGS}
        self.sem = {}
        self.dma_pool = {}
        self.dma_rr = {}
        self.n_dma_sems = n_dma_sems
        self.out_dmas = []
        self.last = {e: None for e in ENGS}
        self._cms = []

    def setup_sems(self, es):
        nc = self.nc
        for e in ENGS:
            self.sem[e] = es.enter_context(nc.semaphore("s_" + e))
        for q in ("sp", "act", "pool"):
            nq = 6 if q == "pool" else self.n_dma_sems
            self.dma_pool[q] = [[es.enter_context(nc.semaphore("d_%s%d" % (q, i))), 0] for i in range(nq)]
            self.dma_rr[q] = 0

    def _add_read(self, op, t):
        _merge(op.deps, op.dma_deps, t.w_c, t.w_d)
        if op.is_dma:
            t.r_d.append(op)
        else:
            t.r_c[op.eng] = op

    def _add_write(self, op, t, partial):
        if (not partial) or t.r_c or t.r_d:
            t.g_c = {}
            t.g_d = []
            _merge(t.g_c, t.g_d, t.r_c, t.r_d)
            _merge(t.g_c, t.g_d, t.w_c, t.w_d)
            t.r_c, t.r_d, t.w_c, t.w_d = {}, [], {}, []
        _merge(op.deps, op.dma_deps, t.g_c, t.g_d)
        if op.is_dma:
            t.w_d.append(op)
        else:
            t.w_c[op.eng] = op

    def _record(self, o, reads, writes, pwrites):
        wset = set(id(t) for t in writes) | set(id(t) for t in pwrites)
        for t in reads:
            _merge(o.deps, o.dma_deps, t.w_c, t.w_d)
        for t in writes:
            self._add_write(o, t, False)
        for t in pwrites:
            self._add_write(o, t, True)
        for t in reads:
            if id(t) not in wset:
                if o.is_dma:
                    t.r_d.append(o)
                else:
                    t.r_c[o.eng] = o

    def op(self, eng, emit, reads=(), writes=(), pwrites=()):
        o = Op(eng, emit)
        self._record(o, reads, writes, pwrites)
        self.ops[eng].append(o)
        self.last[eng] = o
        return o

    def dma(self, q, out, in_, reads=(), writes=(), pwrites=(), **kw):
        o = Op(q, (lambda eng: eng.dma_start(out=out, in_=in_, **kw)), is_dma=True)
        pool = self.dma_pool[q]
        i = self.dma_rr[q]
        self.dma_rr[q] = (i + 1) % len(pool)
        o.dsem = pool[i][0]
        o.prev_dval = pool[i][1]
        pool[i][1] += 16
        o.dval = pool[i][1]
        self._record(o, reads, writes, pwrites)
        self.ops[q].append(o)
        self.out_dmas.append(o)
        return o

    def barrier(self):
        lasts = {e: o for e, o in self.last.items() if o is not None}
        for e in ENGS:
            o = Op(e, None)
            for e2, o2 in lasts.items():
                if e2 != e:
                    o.deps[e2] = o2
            o.dma_deps = list(self.out_dmas)
            self.ops[e].append(o)
        self.out_dmas = []

    def finalize(self):
        for e in ENGS:
            for o in self.ops[e]:
                for e2, p in o.deps.items():
                    if p.is_dma:
                        continue
                    if e2 == e and e == "pe":
                        continue
                    p.signal = True
        for e in ENGS:
            c = 0
            for o in self.ops[e]:
                if o.is_dma or o.emit is None:
                    continue
                if o.signal:
                    c += 1
                    o.count = c
        nc = self.nc
        engmap = {"pe": "tensor", "act": "scalar", "dve": "vector", "pool": "gpsimd", "sp": "sync"}
        stats = {}
        with nc.Block() as block:
            for e in ENGS:
                def body(engine, e=e):
                    waited = {}
                    nw = 0

                    def wait(sem, val):
                        nonlocal nw
                        k = id(sem)
                        if waited.get(k, 0) >= val:
                            return
                        waited[k] = val
                        engine.wait_ge(sem, val)
                        nw += 1

                    for o in self.ops[e]:
                        for e2, p in o.deps.items():
                            if p.is_dma:
                                wait(p.dsem, p.dval)
                                continue
                            if e2 == e and e == "pe":
                                continue
                            if p is o:
                                continue
                            wait(self.sem[e2], p.count)
                        for p in o.dma_deps:
                            wait(p.dsem, p.dval)
                        if o.emit is None:
                            continue
                        if o.is_dma:
                            if o.prev_dval > 0:
                                wait(o.dsem, o.prev_dval)
                            ins = o.emit(engine)
                            ins.then_inc(o.dsem, 16)
                        else:
                            ins = o.emit(engine)
                            if o.signal:
                                ins.then_inc(self.sem[e], 1)
                    stats[e] = (len(self.ops[e]), nw)
                getattr(block, engmap[e])(body)
        return stats


class Sbuf:
    def __init__(self, big, nwords):
        self.big = big
        self.n = nwords
        self.off = 0
        self.marks = []

    def mark(self):
        self.marks.append(self.off)

    def release(self):
        self.off = self.marks.pop()

    def alloc(self, nelem, dtype=F32, parts=128, name=""):
        if dtype == BF16:
            nw = (nelem + 1) // 2
        else:
            nw = nelem
        nw = (nw + 7) // 8 * 8
        assert self.off + nw <= self.n, "SBUF overflow %s: %d + %d > %d" % (name, self.off, nw, self.n)
        ap = self.big[0:parts, self.off:self.off + nw]
        self.off += nw
        if dtype == BF16:
            ap = ap.bitcast(BF16)[:, 0:nelem]
        elif dtype != F32:
            ap = ap.bitcast(dtype)[:, 0:nelem]
        else:
            ap = ap[:, 0:nelem]
        return Tile(ap, name)

from contextlib import ExitStack
import ml_dtypes

D = 1024
NS = 2
LC = 256
S = 2048
T = LC + S
DEPTH = 2
NE = 16
DEXP = 2048
IN_W = 5632
CAPL = 256
CAPC = 32
EPS = 1e-6
KC = 8

SB_WORDS = 46080


def host_consts():
    c = {}
    half = 32
    freqs = (10000.0 ** (-np.arange(half, dtype=np.float32) / half)).astype(np.float32)
    t = np.arange(S)
    rows = (t // 64).astype(np.float32)
    cols = (t % 64).astype(np.float32)
    C = np.zeros((128, S), np.float32)
    Sn = np.zeros((128, S), np.float32)
    for d in range(128):
        pos = rows if d < 64 else cols
        j = (d % 64) % 32
        ang = (pos * freqs[j]).astype(np.float32)
        C[d] = np.cos(ang)
        Sn[d] = np.sin(ang)
    c["ropeC"] = C
    c["ropeS"] = Sn
    Pm = np.zeros((128, 128), np.float32)
    for d in range(128):
        i = d % 64
        if i < 32:
            Pm[d + 32, d] = -1.0
        else:
            Pm[d - 32, d] = 1.0
    c["pm"] = Pm
    c["ident"] = np.eye(128, dtype=np.float32)
    kk = np.arange(128)[:, None]
    qq = np.arange(128)[None, :]
    mprev = (qq <= kk).astype(np.float32)
    mnext = (kk <= qq).astype(np.float32)
    c["mprev"] = np.tile(mprev, (1, 4))
    c["mnext"] = np.tile(mnext, (1, 4))
    c["iota_f"] = np.tile(np.arange(256, dtype=np.float32)[None, :], (128, 1))
    c["iota_p"] = np.stack([np.arange(128, dtype=np.float32), np.arange(128, dtype=np.float32) + 128], axis=1)
    sel = np.zeros((32, 32, 128), np.float32)
    for r in range(32):
        sel[r, r, :] = 1.0
    c["rowsel"] = sel.transpose(1, 0, 2).reshape(32, 32 * 128)
    bd = np.zeros((32, 32), np.float32)
    bd[:16, :16] = 1.0
    bd[16:, 16:] = 1.0
    c["bdones"] = bd
    return c


CONST_SHAPES = {"ropeC": (128, S), "ropeS": (128, S), "pm": (128, 128), "ident": (128, 128),
                "mprev": (128, 512), "mnext": (128, 512), "iota_f": (128, 256), "iota_p": (128, 2),
                "rowsel": (32, 32 * 128), "bdones": (32, 32)}


class KB:
    def __init__(self, debug=None, stop_after=None):
        self.debug = debug or []
        self.stop_after = stop_after
        self.nc = bass.Bass("TRN2", target_bir_lowering=False)
        self.dr = {}

    def din(self, name, shape, dt=F32):
        self.dr[name] = self.nc.dram_tensor(name, list(shape), dt, kind="ExternalInput").ap()
        return self.dr[name]

    def dscr(self, name, shape, dt=F32, out=False):
        kind = "ExternalOutput" if (out or name in self.debug) else "Internal"
        self.dr[name] = self.nc.dram_tensor(name, list(shape), dt, kind=kind).ap()
        return self.dr[name]

    def mm(self, pst, out, lhsT, rhs, start, stop, reads):
        self.fw.op("pe", lambda e: e.matmul(out, lhsT=lhsT, rhs=rhs, start=start, stop=stop), reads=reads, pwrites=[pst])

    def tr(self, pst, out, in_, ident, reads):
        self.fw.op("pe", lambda e: e.transpose(out, in_, ident), reads=reads, pwrites=[pst])

    def act(self, out, in_, func, reads, writes=(), pwrites=(), **kw):
        self.fw.op("act", lambda e: e.activation(out=out, in_=in_, func=func, **kw), reads=reads, writes=writes, pwrites=pwrites)

    def tt(self, eng, out, in0, in1, op, reads, writes=(), pwrites=()):
        self.fw.op(eng, lambda e: e.tensor_tensor(out=out, in0=in0, in1=in1, op=op), reads=reads, writes=writes, pwrites=pwrites)

    def ts(self, eng, out, in0, s1, op0, reads, s2=None, op1=None, writes=(), pwrites=()):
        if op1 is None:
            self.fw.op(eng, lambda e: e.tensor_scalar(out=out, in0=in0, scalar1=s1, scalar2=None, op0=op0), reads=reads, writes=writes, pwrites=pwrites)
        else:
            self.fw.op(eng, lambda e: e.tensor_scalar(out=out, in0=in0, scalar1=s1, scalar2=s2, op0=op0, op1=op1), reads=reads, writes=writes, pwrites=pwrites)

    def stt(self, out, in0, scalar, in1, op0, op1, reads, writes=(), pwrites=()):
        self.fw.op("dve", lambda e: e.scalar_tensor_tensor(out=out, in0=in0, scalar=scalar, in1=in1, op0=op0, op1=op1), reads=reads, writes=writes, pwrites=pwrites)

    def cp(self, eng, out, in_, reads, writes=(), pwrites=()):
        if eng == "act":
            self.fw.op("act", lambda e: e.copy(out=out, in_=in_), reads=reads, writes=writes, pwrites=pwrites)
        else:
            self.fw.op(eng, lambda e: e.tensor_copy(out=out, in_=in_), reads=reads, writes=writes, pwrites=pwrites)

    def dma(self, q, out, in_, reads=(), writes=(), pwrites=(), maxdesc=512):
        shp = tuple(out.shape)
        if len(shp) == 3 and shp[0] * shp[1] > maxdesc and tuple(in_.shape) == shp:
            step = max(1, maxdesc // shp[0])
            first = True
            for i0 in range(0, shp[1], step):
                i1 = min(shp[1], i0 + step)
                if first:
                    self.fw.dma(q, out[:, i0:i1, :], in_[:, i0:i1, :], reads=reads, writes=writes, pwrites=pwrites)
                    first = False
                else:
                    self.fw.dma(q, out[:, i0:i1, :], in_[:, i0:i1, :], reads=reads, pwrites=tuple(writes) + tuple(pwrites))
            return
        self.fw.dma(q, out, in_, reads=reads, writes=writes, pwrites=pwrites)

    def phase_end(self):
        self.fw.barrier()

    def build(self):
        nc = self.nc
        din, dscr = self.din, self.dscr
        din("xT", (NS, D, T))
        din("cT", (128, KC, 3))
        din("ada_w", (DEPTH, D, 6 * D))
        din("ada_bT", (DEPTH, 128, 48))
        din("gmixT", (DEPTH, 128, KC))
        din("gffnT", (DEPTH, 128, KC))
        din("gfinT", (128, KC))
        din("w_in", (DEPTH, D, IN_W))
        din("sink", (DEPTH, 1, 8))
        din("convT", (DEPTH, 128, 5, KC))
        din("lru_w", (DEPTH, 2, 2, 8, 128, 128))
        din("lru_vT", (DEPTH, 128, 3, 2, KC))
        din("w_attn_br", (DEPTH, D, D))
        din("w_rec_br", (DEPTH, D, D))
        din("w_out", (DEPTH, D, D))
        din("w_router", (DEPTH, D, NE))
        if self.stop_after not in ("p0", "p1", "p2", "p3", "p4", "p5", "wip"):
            din("w_gate", (DEPTH, NE, D, DEXP))
            din("w_up", (DEPTH, NE, D, DEXP))
            din("w_down", (DEPTH, NE, DEXP, D))
        for k, shp in CONST_SHAPES.items():
            din("c_" + k, shp)
        dscr("outT", (NS, D, S), out=True)
        dscr("X1", (NS, D, T)); dscr("X2", (NS, D, T)); dscr("X3", (NS, D, T))
        dscr("qT", (NS, D, T), BF16); dscr("kT", (NS, 256, T), BF16); dscr("vtm", (NS, T, 256), BF16)
        dscr("uT", (NS, D, T)); dscr("gzT", (NS, D, T), BF16); dscr("smaT", (NS, D, T), BF16); dscr("smrT", (NS, D, T), BF16)
        dscr("attT", (NS, D, T), BF16); dscr("rgT", (NS, D, T), BF16)
        dscr("h2tm", (NS, T, D), BF16); dscr("lgT", (NS, NE, T))
        dscr("ye", (NS, NE, CAPL, D), BF16); dscr("yec", (NS, NE, CAPC, D), BF16); dscr("xe", (NE, D, 512 + 2 * CAPC), BF16)
        dscr("modc", (DEPTH, 128, 6, KC, 3))
        dscr("rt", (2, 32, 3, S))

        with ExitStack() as es:
            big = es.enter_context(nc.sbuf_tensor("big", [128, SB_WORDS], F32))
            pst = [es.enter_context(nc.psum_tensor("ps%d" % i, [128, 512], F32)) for i in range(8)]
            self.fw = FW(nc)
            self.fw.setup_sems(es)
            self.sb = Sbuf(big, SB_WORDS)
            self.ps = [Tile(p, "ps%d" % i) for i, p in enumerate(pst)]
            self._body()
            self.fw.barrier()
            self.stats = self.fw.finalize()
        return nc

    def _body(self):
        sb = self.sb
        dr = self.dr
        fw = self.fw
        self.ident = sb.alloc(128, F32, name="ident")
        self.dma("sp", self.ident[:, :], dr["c_ident"][:, :], writes=[self.ident])
        self.identb = sb.alloc(128, BF16, name="identb")
        self.dma("pool", self.identb[:, :], dr["c_ident"][:, :], writes=[self.identb])
        self.pmb = sb.alloc(128, BF16, name="pmb")
        self.dma("pool", self.pmb[:, :], dr["c_pm"][:, :], writes=[self.pmb])
        self.onesb = sb.alloc(128, BF16, name="onesb")
        fw.op("dve", lambda e: e.memset(self.onesb[:, :], 1.0), writes=[self.onesb])
        self.epsc = sb.alloc(1, F32, name="epsc")
        fw.op("dve", lambda e: e.memset(self.epsc[:, :], EPS), writes=[self.epsc])
        self.silu_c = sb.alloc(KC * 3, F32, name="silu_c")
        ctmp = sb.alloc(KC * 3, F32, name="ctmp")
        self.dma("sp", ctmp[:, :], dr["cT"].rearrange("p c e -> p (c e)"), writes=[ctmp])
        self.act(self.silu_c[:, :], ctmp[:, :], AF.Silu, reads=[ctmp], writes=[self.silu_c])
        self.modc = [sb.alloc(6 * KC * 3, F32, name="modc%d" % l) for l in range(DEPTH)]
        self.A1 = [sb.alloc(KC * 3, F32) for l in range(DEPTH)]
        self.A2 = [sb.alloc(KC * 3, F32) for l in range(DEPTH)]
        self.posT_lat = sb.alloc(16 * 32, F32, name="posT_lat")
        self.posT_ctx = sb.alloc(2 * 32, F32, name="posT_ctx")
        self.base_off = sb.off
        Xs = ["xT", "X1", "X2", "X3"]
        for l in range(DEPTH):
            last = l == DEPTH - 1
            self.cur_last = last
            self.p0_mod(l)
            if self.stop_after == "p0":
                return
            self.p1_inproj(l, dr["xT"] if l == 0 else dr["X2"])
            if self.stop_after == "p1":
                return
            self.p2_attn(l, last)
            if self.stop_after == "p2":
                return
            self.p3_rec(l)
            if self.stop_after == "p3":
                return
            if self.stop_after == "wip_DISABLED":
                self.pF_copy()
                return
            self.p4_merge(l, dr["xT"] if l == 0 else dr["X2"], dr["X1"], last)
            if self.stop_after == "p4":
                return
            self.p5_route(l, last)
            if self.stop_after == "p5":
                return
            self.p6_experts(l, last)
            if self.stop_after == "p6":
                return
            self.p7_scatter(l, dr["X1"], dr["X2"] if not last else None, last)
            if self.stop_after == "p7":
                return

    def p0_mod(self, l):
        sb, dr, fw, ps = self.sb, self.dr, self.fw, self.ps
        sb.mark()
        NB = 12
        wbuf = [sb.alloc(KC * 512, F32, name="adaw%d" % i) for i in range(2)]
        adab = sb.alloc(48, F32)
        gm = sb.alloc(KC, F32)
        gf = sb.alloc(KC, F32)
        self.dma("sp", adab[:, :], dr["ada_bT"][l], writes=[adab])
        self.dma("sp", gm[:, :], dr["gmixT"][l], writes=[gm])
        self.dma("sp", gf[:, :], dr["gffnT"][l], writes=[gf])
        pt = ps[0]
        modc = self.modc[l]
        for nb in range(NB):
            wb = wbuf[nb % 2]
            self.dma("sp" if nb % 2 == 0 else "pool", wb[:, :].rearrange("p (c n) -> p c n", c=KC),
                   dr["ada_w"][l, :, nb * 512:(nb + 1) * 512].rearrange("(c p) n -> p c n", p=128), writes=[wb])
            for jj in range(4):
                j = nb * 4 + jj
                for k in range(KC):
                    self.mm(pt, pt[:, j * 3:(j + 1) * 3], wb[:, k * 512 + jj * 128:k * 512 + (jj + 1) * 128],
                            self.silu_c[:, k * 3:(k + 1) * 3], k == 0, k == KC - 1, reads=[wb, self.silu_c])
        for e3 in range(3):
            self.tt("dve", modc[:, :].rearrange("p (j e) -> p j e", e=3)[:, :, e3],
                    pt[:, 0:144].rearrange("p (j e) -> p j e", e=3)[:, :, e3], adab[:, :], ALU.add,
                    reads=[pt, adab], pwrites=[modc])
        m4 = modc[:, :].rearrange("p (w c e) -> p w c e", w=6, c=KC)
        a1 = self.A1[l][:, :].rearrange("p (c e) -> p c e", e=3)
        a2 = self.A2[l][:, :].rearrange("p (c e) -> p c e", e=3)
        for e3 in range(3):
            self.stt(a1[:, :, e3], m4[:, 1, :, e3], 1.0, gm[:, :], ALU.add, ALU.mult, reads=[modc, gm], pwrites=[self.A1[l]])
            self.stt(a2[:, :, e3], m4[:, 4, :, e3], 1.0, gf[:, :], ALU.add, ALU.mult, reads=[modc, gf], pwrites=[self.A2[l]])
        if "modc" in self.debug:
            self.dma("sp", dr["modc"][l].rearrange("p w c e -> p (w c e)"), modc[:, :], reads=[modc])
        self.phase_end()
        sb.release()

    def mcol(self, l, which, c, e):
        i = (which * KC + c) * 3 + e
        return self.modc[l][:, i:i + 1]

    def rstd_from(self, xt_tiles, w, sqb, pt, rstd, lnv):
        for c in range(KC):
            self.act(sqb[c][:, :w], xt_tiles[c][:, :w], AF.Square, reads=[xt_tiles[c]], writes=[sqb[c]])
        for c in range(KC):
            self.mm(pt, pt[:, :w], self.onesb[:, :], sqb[c][:, :w], c == 0, c == KC - 1, reads=[self.onesb, sqb[c]])
        self.act(lnv[:, :w], pt[:, :w], AF.Ln, reads=[pt], writes=[lnv], scale=1.0 / D, bias=self.epsc[:, 0:1])
        self.act(rstd[:, :w], lnv[:, :w], AF.Exp, reads=[lnv], writes=[rstd], scale=-0.5)

    def p1_inproj(self, l, Xin):
        sb, dr, fw, ps = self.sb, self.dr, self.fw, self.ps
        sb.mark()
        W = 256
        win = sb.alloc(KC * IN_W, BF16, name="win")
        winv = win[:, :].rearrange("p (c n) -> p c n", c=KC)
        winb = [Tile(winv[:, :, nb * 512:(nb + 1) * 512], "winb%d" % nb) for nb in range(11)]
        for nb in range(11):
            self.dma("pool", winv[:, :, nb * 512:(nb + 1) * 512],
                   dr["w_in"][l, :, nb * 512:(nb + 1) * 512].rearrange("(c p) n -> p c n", p=128), writes=[winb[nb]])
        ropeC = sb.alloc(S, F32); ropeS = sb.alloc(S, F32); pmb = self.pmb
        self.dma("sp", ropeC[:, :], dr["c_ropeC"][:, :], writes=[ropeC])
        self.dma("sp", ropeS[:, :], dr["c_ropeS"][:, :], writes=[ropeS])
        xt = [[sb.alloc(W, F32) for c in range(KC)] for i in range(2)]
        sqb = [sb.alloc(W, BF16) for c in range(KC)]
        hT = [sb.alloc(W, BF16) for c in range(KC)]
        tmpf = [sb.alloc(W, F32) for i in range(2)]
        rstd = sb.alloc(W, F32); lnv = sb.alloc(W, F32)
        qraw = [sb.alloc(W, BF16) for i in range(2)]
        t1 = [sb.alloc(W, F32) for i in range(2)]
        t2 = [sb.alloc(W, F32) for i in range(2)]
        tmpe = [sb.alloc(W, F32) for i in range(2)]
        tmpe2 = [sb.alloc(W, F32) for i in range(2)]
        st_q = sb.alloc(KC * W, BF16); st_k = sb.alloc(2 * W, BF16)
        st_u = sb.alloc(KC * W, F32)
        st_g = [sb.alloc(KC * W, BF16) for i in range(3)]
        st_v = [sb.alloc(256, BF16) for i in range(2)]
        ntile = T // W
        tl = [(s, ti) for s in range(NS) for ti in range(ntile)]
        v3 = lambda t, n: t[:, :].rearrange("p (c w) -> p c w", c=n)

        def load_x(idx):
            s, ti = tl[idx]
            xb = xt[idx % 2]
            for c in range(KC):
                self.dma("sp", xb[c][:, :], Xin[s, c * 128:(c + 1) * 128, ti * W:(ti + 1) * W], writes=[xb[c]])

        load_x(0)
        for it in range(len(tl)):
            s, ti = tl[it]
            t0 = ti * W
            is_ctx = ti == 0
            e3 = 2 if is_ctx else s
            xb = xt[it % 2]
            if it + 1 < len(tl):
                load_x(it + 1)
            self.rstd_from(xb, W, sqb, ps[0], rstd, lnv)
            for c in range(KC):
                tf = tmpf[c % 2]
                self.stt(tf[:, :], xb[c][:, :], self.A1[l][:, c * 3 + e3:c * 3 + e3 + 1], rstd[:, :], ALU.mult, ALU.mult,
                         reads=[xb[c], self.A1[l], rstd], writes=[tf])
                self.act(hT[c][:, :], tf[:, :], AF.Identity, reads=[tf, self.modc[l]], writes=[hT[c]],
                         bias=self.mcol(l, 0, c, e3), scale=1.0)
            for oc in range(44):
                if 10 <= oc < 12:
                    continue
                if self.cur_last and is_ctx and (oc < 8 or oc >= 20):
                    continue
                pt = ps[1 + (oc % 4)]
                for k in range(KC):
                    self.mm(pt, pt[:, :W], winv[:, k, oc * 128:(oc + 1) * 128], hT[k][:, :], k == 0, k == KC - 1, reads=[winb[oc // 4], hT[k]])
                if oc < 10:
                    stt_, dst = (st_q, st_q[:, oc * W:(oc + 1) * W]) if oc < 8 else (st_k, st_k[:, (oc - 8) * W:(oc - 7) * W])
                    if is_ctx:
                        self.cp("act", dst, pt[:, :W], reads=[pt], pwrites=[stt_])
                    else:
                        qr_ = qraw[oc % 2]; a1 = t1[oc % 2]; a2 = t2[oc % 2]
                        p2 = ps[5 + (oc % 2)]
                        lt0 = t0 - LC
                        self.cp("act", qr_[:, :], pt[:, :W], reads=[pt], writes=[qr_])
                        self.mm(p2, p2[:, :W], pmb[:, :], qr_[:, :], True, True, reads=[pmb, qr_])
                        e1_ = tmpe[oc % 2]; e2_ = tmpe2[oc % 2]
                        self.cp("act", e1_[:, :], pt[:, :W], reads=[pt], writes=[e1_])
                        self.cp("act", e2_[:, :], p2[:, :W], reads=[p2], writes=[e2_])
                        self.tt("dve", a1[:, :], e1_[:, :], ropeC[:, lt0:lt0 + W], ALU.mult, reads=[e1_, ropeC], writes=[a1])
                        self.tt("dve", a2[:, :], e2_[:, :], ropeS[:, lt0:lt0 + W], ALU.mult, reads=[e2_, ropeS], writes=[a2])
                        self.tt("dve", dst, a1[:, :], a2[:, :], ALU.add, reads=[a1, a2], pwrites=[stt_])
                elif oc < 20:
                    self.cp("act", st_u[:, (oc - 12) * W:(oc - 11) * W], pt[:, :W], reads=[pt], pwrites=[st_u])
                else:
                    g = (oc - 20) // 8
                    c_ = (oc - 20) % 8
                    self.act(st_g[g][:, c_ * W:(c_ + 1) * W], pt[:, :W], AF.Gelu if g == 0 else AF.Sigmoid, reads=[pt], pwrites=[st_g[g]])
            for j in range(W // 128):
                pt = ps[7]
                for k in range(KC):
                    self.mm(pt, pt[:, :256], hT[k][:, j * 128:(j + 1) * 128], winv[:, k, 1280:1536], k == 0, k == KC - 1, reads=[winb[2], hT[k]])
                sv = st_v[j % 2]
                self.cp("dve", sv[:, :], pt[:, :256], reads=[pt], writes=[sv])
                self.dma("pool", dr["vtm"][s, t0 + j * 128:t0 + (j + 1) * 128, :], sv[:, :], reads=[sv])
            fm = lambda nm: dr[nm][s, :, t0:t0 + W].rearrange("(c p) w -> p c w", p=128)
            self.dma("pool", fm("qT"), v3(st_q, KC), reads=[st_q])
            self.dma("pool", dr["kT"][s, :, t0:t0 + W].rearrange("(c p) w -> p c w", p=128), v3(st_k, 2), reads=[st_k])
            self.dma("sp", fm("uT"), v3(st_u, KC), reads=[st_u])
            self.dma("sp", fm("gzT"), v3(st_g[0], KC), reads=[st_g[0]])
            self.dma("pool", fm("smaT"), v3(st_g[1], KC), reads=[st_g[1]])
            self.dma("sp", fm("smrT"), v3(st_g[2], KC), reads=[st_g[2]])
        self.phase_end()
        sb.release()

    def p2_attn(self, l, last):
        sb, dr, fw, ps = self.sb, self.dr, self.fw, self.ps
        sb.mark()
        mprev = sb.alloc(512, BF16); mnext = sb.alloc(512, BF16)
        self.dma("pool", mprev[:, :], dr["c_mprev"][:, :], writes=[mprev])
        self.dma("pool", mnext[:, :], dr["c_mnext"][:, :], writes=[mnext])
        sk = sb.alloc(8, F32); ske = sb.alloc(8, F32); onef = sb.alloc(128, F32)
        esf = sb.alloc(1024, F32); eshi = sb.alloc(1024, BF16); eslo = sb.alloc(1024, BF16)
        self.dma("sp", sk[0:1, :], dr["sink"][l], writes=[sk])
        self.act(ske[0:1, :], sk[0:1, :], AF.Exp, reads=[sk], writes=[ske])
        fw.op("dve", lambda e: e.memset(onef[:, :], 1.0), writes=[onef])
        for h in range(8):
            self.ts("dve", esf[0:1, h * 128:(h + 1) * 128], onef[0:1, :], ske[0:1, h:h + 1], ALU.mult, reads=[onef, ske], pwrites=[esf])
        self.cp("dve", eshi[0:1, :], esf[0:1, :], reads=[esf], writes=[eshi])
        self.tt("dve", eslo[0:1, :], esf[0:1, :], eshi[0:1, :], ALU.subtract, reads=[esf, eshi], writes=[eslo])
        qall = sb.alloc(8 * T, BF16, name="qall")
        qv = qall[:, :].rearrange("p (h t) -> p h t", h=8)
        kt = [sb.alloc(T, BF16) for i in range(2)]
        vt = sb.alloc(18 * 256, BF16)
        Er = [sb.alloc(512, BF16) for i in range(10)]
        lnD2 = [sb.alloc(512, F32) for i in range(2)]; rD2 = [sb.alloc(512, F32) for i in range(2)]
        ost = [sb.alloc(4 * 512, BF16) for i in range(2)]
        scale = 1.0 / np.sqrt(128.0)
        for s in range(NS):
            for h in range(8):
                self.dma("sp", qv[:, h, :], dr["qT"][s, h * 128:(h + 1) * 128, :], pwrites=[qall])
            for c in range(2):
                self.dma("sp", kt[c][:, :], dr["kT"][s, c * 128:(c + 1) * 128, :], writes=[kt[c]])
            self.dma("sp", vt[:, :].rearrange("p (b f) -> p b f", f=256), dr["vtm"][s].rearrange("(b p) f -> p b f", p=128), writes=[vt])
            qblocks = [("lat", i) for i in range(16)]
            if not last:
                qblocks = [("ctx", 0), ("ctx", 1)] + qblocks
            for kind, i in qblocks:
                if kind == "lat":
                    tq = LC + i * 128
                    keys = [(0, 0, None), (128, 1, None)]
                    if i > 0:
                        keys.append((LC + (i - 1) * 128, 2 + i - 1, mprev))
                    keys.append((LC + i * 128, 2 + i, None))
                    if i < 15:
                        keys.append((LC + (i + 1) * 128, 2 + i + 1, mnext))
                    sw, si = 512, i % 4
                else:
                    tq = i * 128
                    keys = [(0, 0, None), (128, 1, None)]
                    sw, si = 256, i
                for kv in range(2):
                    Ek = Er[kv * 5:(kv + 1) * 5]; lnD = lnD2[kv]; rD = rD2[kv]
                    for idx, (kc0, vb, mk) in enumerate(keys):
                        pS = ps[idx % 3]
                        for g in range(4):
                            self.mm(pS, pS[:, g * 128:(g + 1) * 128], kt[kv][:, kc0:kc0 + 128], qv[:, 4 * kv + g, tq:tq + 128], True, True, reads=[kt[kv], qall])
                        E = Ek[idx]
                        self.act(E[:, :], pS[:, :], AF.Exp, reads=[pS], writes=[E], scale=float(scale))
                        if mk is not None:
                            self.tt("dve", E[:, :], E[:, :], mk[:, :], ALU.mult, reads=[E, mk], writes=[E])
                    pO, pD = ps[3 + (kv % 2) * 2], ps[4 + (kv % 2) * 2]
                    n = len(keys)
                    for idx, (kc0, vb, mk) in enumerate(keys):
                        self.mm(pO, pO[:, :], vt[:, vb * 256 + kv * 128:vb * 256 + (kv + 1) * 128], Ek[idx][:, :], idx == 0, idx == n - 1, reads=[vt, Ek[idx]])
                    for idx, (kc0, vb, mk) in enumerate(keys):
                        self.mm(pD, pD[:, :], self.onesb[:, :], Ek[idx][:, :], idx == 0, False, reads=[self.onesb, Ek[idx]])
                    self.mm(pD, pD[:, :], self.onesb[0:1, :], eshi[0:1, kv * 512:(kv + 1) * 512], False, False, reads=[self.onesb, eshi])
                    self.mm(pD, pD[:, :], self.onesb[0:1, :], eslo[0:1, kv * 512:(kv + 1) * 512], False, True, reads=[self.onesb, eslo])
                    self.act(lnD[:, :], pD[:, :], AF.Ln, reads=[pD], writes=[lnD])
                    self.act(rD[:, :], lnD[:, :], AF.Exp, reads=[lnD], writes=[rD], scale=-1.0)
                    ov = ost[kv][:, :].rearrange("p (g t) -> p g t", g=4)
                    self.tt("dve", ov[:, :, si * 128:(si + 1) * 128], pO[:, :].rearrange("p (g t) -> p g t", g=4),
                            rD[:, :].rearrange("p (g t) -> p g t", g=4), ALU.mult, reads=[pO, rD], pwrites=[ost[kv]])
                if (kind == "lat" and i % 4 == 3) or (kind == "ctx" and i == 1):
                    tq0 = tq + 128 - sw
                    for kv in range(2):
                        ov = ost[kv][:, :].rearrange("p (g t) -> p g t", g=4)
                        self.dma("pool", dr["attT"][s, kv * 512:(kv + 1) * 512, tq0:tq0 + sw].rearrange("(g d) t -> d g t", d=128),
                               ov[:, :, 0:sw], reads=[ost[kv]])
        self.phase_end()
        sb.release()

    def p3_rec(self, l):
        sb, dr, fw, ps = self.sb, self.dr, self.fw, self.ps
        sb.mark()
        lw = sb.alloc(2 * 2 * 8 * 128, BF16, name="lw")
        lwv = lw[:, :].rearrange("p (w d b j) -> p w d b j", w=2, d=2, b=8)
        for w_ in range(2):
            for d_ in range(2):
                self.dma("pool", lwv[:, w_, d_, :, :], dr["lru_w"][l, w_, d_].rearrange("b i j -> i b j"), pwrites=[lw])
        lv = sb.alloc(48, F32); cv = sb.alloc(40, F32)
        self.dma("sp", lv[:, :], dr["lru_vT"][l].rearrange("p a d c -> p (a d c)"), writes=[lv])
        self.dma("sp", cv[:, :], dr["convT"][l].rearrange("p a c -> p (a c)"), writes=[cv])
        onec = sb.alloc(1, F32)
        fw.op("dve", lambda e: e.memset(onec[:, :], 1.0), writes=[onec])
        e1 = sb.alloc(16, F32); l1 = sb.alloc(16, F32); cl = sb.alloc(16, F32)
        self.act(e1[:, :], lv[:, 32:48], AF.Exp, reads=[lv], writes=[e1], scale=-1.0)
        self.act(l1[:, :], e1[:, :], AF.Ln, reads=[e1, onec], writes=[l1], bias=onec[:, 0:1], scale=1.0)
        self.ts("dve", cl[:, :], l1[:, :], -8.0, ALU.mult, reads=[l1], writes=[cl])
        bu = [dict(u=sb.alloc(T, F32), uc=sb.alloc(T, F32), ucb=sb.alloc(T, BF16), gz=sb.alloc(T, BF16)) for i in range(2)]
        bd = [dict(r=sb.alloc(T, F32), gi=sb.alloc(T, F32), a=sb.alloc(T, F32)) for d_ in range(2)]
        hf = sb.alloc(T, F32); hb = sb.alloc(T, F32); og = sb.alloc(T, BF16)
        tiles = [(0, 256)] + [(256 + 512 * i, 512) for i in range(4)]
        items = [(s, c) for s in range(NS) for c in range(KC)]

        def conv_act(i):
            s, c = items[i]
            B = bu[i % 2]
            u, uc, gz = B["u"], B["uc"], B["gz"]
            self.dma("sp", u[:, :], dr["uT"][s, c * 128:(c + 1) * 128, :], writes=[u])
            self.dma("sp", gz[:, :], dr["gzT"][s, c * 128:(c + 1) * 128, :], writes=[gz])
            self.act(uc[:, :], u[:, :], AF.Identity, reads=[u, cv], writes=[uc], scale=cv[:, 2 * KC + c:2 * KC + c + 1], bias=cv[:, 4 * KC + c:4 * KC + c + 1])

        def conv_dve(i):
            s, c = items[i]
            B = bu[i % 2]
            u, uc, ucb = B["u"], B["uc"], B["ucb"]
            for k, d in ((0, -2), (1, -1), (3, 1)):
                for (sa, sb_) in ((0, LC), (LC, T)):
                    lo = max(sa, sa - d); hi = min(sb_, sb_ - d)
                    self.stt(uc[:, lo:hi], u[:, lo + d:hi + d], cv[:, k * KC + c:k * KC + c + 1], uc[:, lo:hi], ALU.mult, ALU.add,
                             reads=[u, cv, uc], pwrites=[uc])
            self.cp("dve", ucb[:, :], uc[:, :], reads=[uc], writes=[ucb])

        def gates(i, d_):
            s, c = items[i]
            ucb = bu[i % 2]["ucb"]
            r, gi, a = bd[d_]["r"], bd[d_]["gi"], bd[d_]["a"]
            for ti, (t0, w) in enumerate(tiles):
                pA = ps[(2 * ti) % 8]; pX = ps[(2 * ti + 1) % 8]
                self.mm(pA, pA[:, :w], lwv[:, 0, d_, c, :], ucb[:, t0:t0 + w], True, True, reads=[lw, ucb])
                self.mm(pX, pX[:, :w], lwv[:, 1, d_, c, :], ucb[:, t0:t0 + w], True, True, reads=[lw, ucb])
                self.act(r[:, t0:t0 + w], pA[:, :w], AF.Sigmoid, reads=[pA, lv], pwrites=[r], bias=lv[:, (0 * 2 + d_) * KC + c:(0 * 2 + d_) * KC + c + 1], scale=1.0)
                self.act(gi[:, t0:t0 + w], pX[:, :w], AF.Sigmoid, reads=[pX, lv], pwrites=[gi], bias=lv[:, (1 * 2 + d_) * KC + c:(1 * 2 + d_) * KC + c + 1], scale=1.0)
            self.act(a[:, :], r[:, :], AF.Exp, reads=[r, cl], writes=[a], scale=cl[:, d_ * KC + c:d_ * KC + c + 1])
            self.act(r[:, :], a[:, :], AF.Square, reads=[a], writes=[r])
            self.act(r[:, :], r[:, :], AF.Sqrt, reads=[r, onec], writes=[r], scale=-1.0, bias=onec[:, 0:1])

        def scan_dve(i, d_):
            uc = bu[i % 2]["uc"]
            q_, gi, a = bd[d_]["r"], bd[d_]["gi"], bd[d_]["a"]
            self.tt("dve", gi[:, :], gi[:, :], uc[:, :], ALU.mult, reads=[gi, uc], writes=[gi])
            self.tt("dve", gi[:, :], gi[:, :], q_[:, :], ALU.mult, reads=[gi, q_], writes=[gi])
            if d_ == 0:
                fw.op("dve", lambda e, a=a, gi=gi: e.tensor_tensor_scan(out=hf[:, :], data0=a[:, :], data1=gi[:, :], initial=0.0, op0=ALU.mult, op1=ALU.add),
                      reads=[a, gi], writes=[hf])
            else:
                fw.op("dve", lambda e, a=a, gi=gi: e.tensor_tensor_scan(out=hb[:, LC - 1::-1], data0=a[:, LC - 1::-1], data1=gi[:, LC - 1::-1], initial=0.0, op0=ALU.mult, op1=ALU.add),
                      reads=[a, gi], writes=[hb])
                fw.op("dve", lambda e, a=a, gi=gi: e.tensor_tensor_scan(out=hb[:, T - 1:LC - 1:-1], data0=a[:, T - 1:LC - 1:-1], data1=gi[:, T - 1:LC - 1:-1], initial=hb[:, 0:1], op0=ALU.mult, op1=ALU.add),
                      reads=[a, gi, hb], pwrites=[hb])

        def tail(i):
            s, c = items[i]
            gz = bu[i % 2]["gz"]
            self.tt("dve", hf[:, :], hf[:, :], hb[:, :], ALU.add, reads=[hf, hb], writes=[hf])
            self.tt("dve", og[:, :], hf[:, :], gz[:, :], ALU.mult, reads=[hf, gz], writes=[og])
            self.dma("pool", dr["rgT"][s, c * 128:(c + 1) * 128, :], og[:, :], reads=[og])

        n_it = len(items)
        conv_act(0)
        conv_dve(0)
        for i in range(n_it):
            if i + 1 < n_it:
                conv_act(i + 1)
            gates(i, 0)
            if i + 1 < n_it:
                conv_dve(i + 1)
            gates(i, 1)
            scan_dve(i, 0)
            scan_dve(i, 1)
            tail(i)
        self.phase_end()
        sb.release()

    def pF_copy(self):
        sb, dr, fw = self.sb, self.dr, self.fw
        sb.mark()
        buf = [sb.alloc(S, F32) for i in range(2)]
        i = 0
        for s in range(NS):
            for c in range(KC):
                b = buf[i % 2]
                self.dma("sp", b[:, :], dr["xT"][s, c * 128:(c + 1) * 128, LC:T], writes=[b])
                self.dma("sp", dr["outT"][s, c * 128:(c + 1) * 128, :], b[:, :], reads=[b])
                i += 1
        self.phase_end()
        sb.release()

    def p4_merge(self, l, Xin, Xout, last):
        sb, dr, fw, ps = self.sb, self.dr, self.fw, self.ps
        sb.mark()
        W = 512
        wts = []
        for name in ("w_attn_br", "w_rec_br", "w_out"):
            wt = sb.alloc(KC * D, BF16, name=name)
            wv = wt[:, :].rearrange("p (c n) -> p c n", c=KC)
            halves = [Tile(wv[:, :, hh * 512:(hh + 1) * 512], name + "_h%d" % hh) for hh in range(2)]
            for hh in range(2):
                self.dma("pool", wv[:, :, hh * 512:(hh + 1) * 512], dr[name][l, :, hh * 512:(hh + 1) * 512].rearrange("(c p) n -> p c n", p=128), writes=[halves[hh]])
            wts.append((halves, wv))
        (wa, wav), (wr, wrv), (wo, wov) = wts
        wrt = sb.alloc(KC * NE, F32)
        wrtv = wrt[:, :].rearrange("p (c e) -> p c e", c=KC)
        self.dma("sp", wrtv, dr["w_router"][l].rearrange("(c p) e -> p c e", p=128), writes=[wrt])
        att = sb.alloc(KC * W, BF16); rg = sb.alloc(KC * W, BF16); sma = sb.alloc(KC * W, BF16); smr = sb.alloc(KC * W, BF16)
        xin = sb.alloc(KC * W, F32)
        mg = [sb.alloc(W, BF16) for c in range(KC)]
        xn = [sb.alloc(W, F32) for c in range(KC)]
        sqb = [sb.alloc(W, BF16) for c in range(KC)]
        h2f = [sb.alloc(W, F32) for c in range(KC)]
        h2b = [sb.alloc(W, BF16) for c in range(KC)]
        tm1 = [sb.alloc(W, F32) for i in range(2)]; tm2 = [sb.alloc(W, F32) for i in range(2)]
        rstd = sb.alloc(W, F32); lnv = sb.alloc(W, F32)
        lgs = sb.alloc(W, F32)
        stg = [sb.alloc(D, BF16) for i in range(2)]
        v3 = lambda t: t[:, :].rearrange("p (c w) -> p c w", c=KC)
        tiles4 = [(0, 256)] + [(LC + 512 * i, 512) for i in range(4)]
        for s in range(NS):
            for ti, (t0, w) in enumerate(tiles4):
                if last and ti == 0:
                    continue
                e3 = 2 if ti == 0 else s
                for (tl, nm) in ((att, "attT"), (rg, "rgT"), (sma, "smaT"), (smr, "smrT")):
                    self.dma("sp", v3(tl)[:, :, :w], dr[nm][s, :, t0:t0 + w].rearrange("(c p) w -> p c w", p=128), writes=[tl])
                self.dma("sp", v3(xin)[:, :, :w], Xin[s, :, t0:t0 + w].rearrange("(c p) w -> p c w", p=128), writes=[xin])
                for m in range(KC):
                    pA = ps[(2 * m) % 4]; pR = ps[(2 * m + 1) % 4]
                    for k in range(KC):
                        self.mm(pA, pA[:, :w], wav[:, k, m * 128:(m + 1) * 128], v3(att)[:, k, :w], k == 0, k == KC - 1, reads=[wa[m // 4], att])
                    for k in range(KC):
                        self.mm(pR, pR[:, :w], wrv[:, k, m * 128:(m + 1) * 128], v3(rg)[:, k, :w], k == 0, k == KC - 1, reads=[wr[m // 4], rg])
                    a1 = tm1[m % 2]; a2 = tm2[m % 2]
                    self.tt("dve", a1[:, :w], pA[:, :w], v3(sma)[:, m, :w], ALU.mult, reads=[pA, sma], writes=[a1])
                    self.tt("dve", a2[:, :w], pR[:, :w], v3(smr)[:, m, :w], ALU.mult, reads=[pR, smr], writes=[a2])
                    self.tt("dve", mg[m][:, :w], a1[:, :w], a2[:, :w], ALU.add, reads=[a1, a2], writes=[mg[m]])
                for m in range(KC):
                    pD = ps[4 + (m % 2)]
                    for k in range(KC):
                        self.mm(pD, pD[:, :w], wov[:, k, m * 128:(m + 1) * 128], mg[k][:, :w], k == 0, k == KC - 1, reads=[wo[m // 4], mg[k]])
                    self.stt(xn[m][:, :w], pD[:, :w], self.mcol(l, 2, m, e3), v3(xin)[:, m, :w], ALU.mult, ALU.add, reads=[pD, self.modc[l], xin], writes=[xn[m]])
                    self.dma("pool", Xout[s, m * 128:(m + 1) * 128, t0:t0 + w], xn[m][:, :w], reads=[xn[m]])
                self.rstd_from(xn, w, sqb, ps[6], rstd, lnv)
                for m in range(KC):
                    tf = tm1[m % 2]
                    self.stt(tf[:, :w], xn[m][:, :w], self.A2[l][:, m * 3 + e3:m * 3 + e3 + 1], rstd[:, :w], ALU.mult, ALU.mult, reads=[xn[m], self.A2[l], rstd], writes=[tf])
                    self.act(h2f[m][:, :w], tf[:, :w], AF.Identity, reads=[tf, self.modc[l]], writes=[h2f[m]], bias=self.mcol(l, 3, m, e3), scale=1.0)
                    self.cp("act", h2b[m][:, :w], h2f[m][:, :w], reads=[h2f[m]], writes=[h2b[m]])
                pL = ps[7]
                for k in range(KC):
                    self.mm(pL, pL[0:NE, :w], wrtv[:, k, :], h2f[k][:, :w], k == 0, k == KC - 1, reads=[wrt, h2f[k]])
                self.cp("act", lgs[0:NE, :w], pL[0:NE, :w], reads=[pL], writes=[lgs])
                self.dma("pool", dr["lgT"][s, :, t0:t0 + w], lgs[0:NE, :w], reads=[lgs])
                for j in range(w // 128):
                    pT = ps[2 + j % 2]
                    pTb = pT[:, :].bitcast(BF16)
                    for m in range(KC):
                        self.tr(pT, pTb[:, m * 128:(m + 1) * 128], h2b[m][:, j * 128:(j + 1) * 128], self.identb[:, :], reads=[h2b[m], self.identb])
                    sg = stg[j % 2]
                    self.cp("act", sg[:, :], pTb[:, 0:D], reads=[pT], writes=[sg])
                    self.dma("pool", dr["h2tm"][s, t0 + j * 128:t0 + (j + 1) * 128, :], sg[:, :], reads=[sg])
        self.phase_end()
        sb.release()

    def p5_route(self, l, last):
        sb, dr, fw, ps = self.sb, self.dr, self.fw, self.ps
        sb.mark()
        bd = sb.alloc(32, F32)
        self.dma("sp", bd[0:32, :], dr["c_bdones"][:, :], writes=[bd])
        lg = sb.alloc(S, F32); E = sb.alloc(S, F32); rs = sb.alloc(S, F32); aff = sb.alloc(S, F32); work = sb.alloc(S, F32)
        m8 = sb.alloc(8, F32); mask = sb.alloc(S, F32); pin = sb.alloc(S, F32); posm = sb.alloc(S, F32); onesf = sb.alloc(S, F32)
        fw.op("dve", lambda e: e.memset(onesf[:, :], 1.0), writes=[onesf])
        groups = [(0, LC, S, CAPL, self.posT_lat)]
        if not last:
            groups.append((1, 0, LC, CAPC, self.posT_ctx))
        P = 32
        for gi, a0, n, cap, posT in groups:
            self.dma("sp", lg[0:P, :n], dr["lgT"][:, :, a0:a0 + n].rearrange("s e t -> (s e) t"), writes=[lg])
            self.act(E[0:P, :n], lg[0:P, :n], AF.Exp, reads=[lg], writes=[E])
            for c0 in range(0, n, 512):
                w = min(512, n - c0)
                pS = ps[(c0 // 512) % 2]
                self.mm(pS, pS[0:P, :w], bd[0:P, 0:P], E[0:P, c0:c0 + w], True, True, reads=[bd, E])
                fw.op("dve", lambda e, c0=c0, w=w, pS=pS: e.reciprocal(out=rs[0:P, c0:c0 + w], in_=pS[0:P, :w]), reads=[pS], pwrites=[rs])
            self.tt("dve", aff[0:P, :n], E[0:P, :n], rs[0:P, :n], ALU.mult, reads=[E, rs], writes=[aff])
            self.cp("dve", work[0:P, :n], aff[0:P, :n], reads=[aff], writes=[work])
            rounds = cap // 8
            for r_ in range(rounds):
                fw.op("dve", lambda e, n=n: e.max(out=m8[0:P, :], in_=work[0:P, :n]), reads=[work], writes=[m8])
                if r_ < rounds - 1:
                    fw.op("dve", lambda e, n=n: e.match_replace(out=work[0:P, :n], in_to_replace=m8[0:P, :], in_values=work[0:P, :n], imm_value=-1.0),
                          reads=[m8, work], writes=[work])
            self.ts("dve", mask[0:P, :n], aff[0:P, :n], m8[0:P, 7:8], ALU.is_ge, reads=[aff, m8], writes=[mask])
            fw.op("dve", lambda e, n=n: e.tensor_tensor_scan(out=pin[0:P, :n], data0=onesf[0:P, :n], data1=mask[0:P, :n], initial=0.0, op0=ALU.mult, op1=ALU.add),
                  reads=[onesf, mask], writes=[pin])
            self.tt("dve", pin[0:P, :n], pin[0:P, :n], mask[0:P, :n], ALU.mult, reads=[pin, mask], writes=[pin])
            self.ts("dve", posm[0:P, :n], pin[0:P, :n], -1.0, ALU.add, reads=[pin], writes=[posm])
            self.dma("sp", dr["rt"][gi, :, 0, 0:n], aff[0:P, :n], reads=[aff])
            self.dma("sp", dr["rt"][gi, :, 1, 0:n], posm[0:P, :n], reads=[posm])
            self.dma("sp", dr["rt"][gi, :, 2, 0:n], mask[0:P, :n], reads=[mask])
            pT = ps[2]
            ntc = n // 128
            for tc in range(ntc):
                self.tr(pT, pT[:, tc * 32:(tc + 1) * 32], posm[0:P, tc * 128:(tc + 1) * 128], self.ident[0:P, 0:P], reads=[posm, self.ident])
            self.cp("act", posT[:, 0:ntc * 32], pT[:, 0:ntc * 32], reads=[pT], writes=[posT])
        self.phase_end()
        sb.release()

    def p6_experts(self, l, last):
        self.p6a_gather(l, last)
        self.p6b_ffn(l, last)

    def p6a_gather(self, l, last):
        sb, dr, fw, ps = self.sb, self.dr, self.fw, self.ps
        sb.mark()
        NCX = 0 if last else 2 * CAPC
        NX = 512 + NCX
        iota_f = sb.alloc(256, F32)
        self.dma("sp", iota_f[:, :], dr["c_iota_f"][:, :], writes=[iota_f])
        h2l = []
        h2c = []
        for s in range(NS):
            t_ = sb.alloc(16 * D, BF16, name="h2l")
            tv = t_[:, :].rearrange("p (c d) -> p c d", c=16)
            for q4 in range(4):
                self.dma("sp", tv[:, q4 * 4:(q4 + 1) * 4, :], dr["h2tm"][s, LC + q4 * 512:LC + (q4 + 1) * 512, :].rearrange("(c p) d -> p c d", p=128), pwrites=[t_])
            h2l.append((t_, tv))
            if not last:
                c_ = sb.alloc(2 * D, BF16, name="h2c")
                cv_ = c_[:, :].rearrange("p (c d) -> p c d", c=2)
                self.dma("sp", cv_, dr["h2tm"][s, 0:LC, :].rearrange("(c p) d -> p c d", p=128), writes=[c_])
                h2c.append((c_, cv_))
        xeT = [[sb.alloc(NX, BF16) for k in range(KC)] for i in range(2)]
        sel = [[sb.alloc(256, BF16) for i in range(16)] for j in range(2)]
        selc = [sb.alloc(32, BF16) for i in range(2)]
        it = 0
        for e in range(NE):
            xb = xeT[e % 2]
            for s in range(NS):
                col = s * NE + e
                sl = sel[it % 2]
                it += 1
                for tc in range(16):
                    self.ts("dve", sl[tc][:, :], iota_f[:, 0:256], self.posT_lat[:, tc * 32 + col:tc * 32 + col + 1], ALU.is_equal,
                            reads=[iota_f, self.posT_lat], writes=[sl[tc]])
                for m in range(KC):
                    pX = ps[m % 4]
                    for tc in range(16):
                        self.mm(pX, pX[:, :256], h2l[s][1][:, tc, m * 128:(m + 1) * 128], sl[tc][:, :], tc == 0, tc == 15, reads=[h2l[s][0], sl[tc]])
                    self.cp("act", xb[m][:, s * 256:(s + 1) * 256], pX[:, :256], reads=[pX], pwrites=[xb[m]])
                if NCX:
                    for tc in range(2):
                        self.ts("dve", selc[tc][:, :], iota_f[:, 0:32], self.posT_ctx[:, tc * 32 + col:tc * 32 + col + 1], ALU.is_equal,
                                reads=[iota_f, self.posT_ctx], writes=[selc[tc]])
                    for m in range(KC):
                        pX = ps[4 + m % 4]
                        for tc in range(2):
                            self.mm(pX, pX[:, :32], h2c[s][1][:, tc, m * 128:(m + 1) * 128], selc[tc][:, :], tc == 0, tc == 1, reads=[h2c[s][0], selc[tc]])
                        self.cp("act", xb[m][:, 512 + s * 32:512 + (s + 1) * 32], pX[:, :32], reads=[pX], pwrites=[xb[m]])
            for m in range(KC):
                self.dma("sp", dr["xe"][e, m * 128:(m + 1) * 128, 0:NX], xb[m][:, :], reads=[xb[m]])
        self.phase_end()
        sb.release()

    def p6b_ffn(self, l, last):
        sb, dr, fw, ps = self.sb, self.dr, self.fw, self.ps
        sb.mark()
        NCX = 0 if last else 2 * CAPC
        NX = 512 + NCX
        NR = 6
        wring = [sb.alloc(8192, BF16, name="wring%d" % i) for i in range(NR)]
        xeb = [sb.alloc(KC * NX, BF16) for i in range(2)]
        hid = [sb.alloc(NX, BF16) for f in range(16)]
        sgt = [sb.alloc(512, F32) for i in range(2)]
        sgc = [sb.alloc(64, F32) for i in range(2)]
        yst = [sb.alloc(512, BF16) for i in range(2)]
        units = []
        for e in range(NE):
            for fh in range(2):
                units.append(("w_gate", e, fh)); units.append(("w_up", e, fh))
            for dh in range(2):
                units.append(("w_down", e, dh))
        loaded = {}

        def issue(ui):
            if ui >= len(units) or ui in loaded:
                return
            nm, e, hh = units[ui]
            wt = wring[ui % NR]
            if nm == "w_down":
                self.dma("pool", wt[:, :].rearrange("p (c n) -> p c n", c=16), dr[nm][l, e, :, hh * 512:(hh + 1) * 512].rearrange("(c p) n -> p c n", p=128), writes=[wt])
            else:
                self.dma("pool", wt[:, :].rearrange("p (c n) -> p c n", c=KC), dr[nm][l, e, :, hh * 1024:(hh + 1) * 1024].rearrange("(c p) n -> p c n", p=128), writes=[wt])
            loaded[ui] = wt

        for ui in range(NR - 1):
            issue(ui)
        ui = 0
        yi = 0
        for e in range(NE):
            xt_ = xeb[e % 2]
            xv = xt_[:, :].rearrange("p (k n) -> p k n", k=KC)
            if e == 0:
                self.dma("sp", xv, dr["xe"][e, :, 0:NX].rearrange("(k p) n -> p k n", p=128), writes=[xt_])
            if e + 1 < NE:
                xn_ = xeb[(e + 1) % 2]
                self.dma("sp", xn_[:, :].rearrange("p (k n) -> p k n", k=KC), dr["xe"][e + 1, :, 0:NX].rearrange("(k p) n -> p k n", p=128), writes=[xn_])
            for fh in range(2):
                for uj in range(ui, ui + NR):
                    issue(uj)
                wg = loaded[ui]; wu = loaded[ui + 1]; ui += 2
                wgv = wg[:, :].rearrange("p (c n) -> p c n", c=KC)
                wuv = wu[:, :].rearrange("p (c n) -> p c n", c=KC)
                for f in range(8):
                    fi = fh * 8 + f
                    pG, pU, pGc, pUc = ps[fi % 2], ps[2 + fi % 2], ps[4], ps[5]
                    for k in range(KC):
                        self.mm(pG, pG[:, :512], wgv[:, k, f * 128:(f + 1) * 128], xv[:, k, 0:512], k == 0, k == KC - 1, reads=[wg, xt_])
                    for k in range(KC):
                        self.mm(pU, pU[:, :512], wuv[:, k, f * 128:(f + 1) * 128], xv[:, k, 0:512], k == 0, k == KC - 1, reads=[wu, xt_])
                    if NCX:
                        for k in range(KC):
                            self.mm(pGc, pGc[:, :NCX], wgv[:, k, f * 128:(f + 1) * 128], xv[:, k, 512:NX], k == 0, k == KC - 1, reads=[wg, xt_])
                        for k in range(KC):
                            self.mm(pUc, pUc[:, :NCX], wuv[:, k, f * 128:(f + 1) * 128], xv[:, k, 512:NX], k == 0, k == KC - 1, reads=[wu, xt_])
                    sg = sgt[fi % 2]
                    self.act(sg[:, :], pG[:, :512], AF.Silu, reads=[pG], writes=[sg])
                    self.tt("dve", hid[fi][:, 0:512], sg[:, :], pU[:, :512], ALU.mult, reads=[sg, pU], pwrites=[hid[fi]])
                    if NCX:
                        sc_ = sgc[fi % 2]
                        self.act(sc_[:, :], pGc[:, :NCX], AF.Silu, reads=[pGc], writes=[sc_])
                        self.tt("dve", hid[fi][:, 512:NX], sc_[:, :], pUc[:, :NCX], ALU.mult, reads=[sc_, pUc], pwrites=[hid[fi]])
            for dh in range(2):
                for uj in range(ui, ui + NR):
                    issue(uj)
                wd = loaded[ui]; ui += 1
                wdv = wd[:, :].rearrange("p (c n) -> p c n", c=16)
                rgs = [(s * 256 + c * 128, 128, ("ye", s, c)) for s in range(NS) for c in range(2)]
                if NCX:
                    rgs.append((512, NCX, ("yec",)))
                for (c0, M, dst) in rgs:
                    pY = ps[6 + yi % 2]
                    ys = yst[yi % 2]
                    yi += 1
                    for f in range(16):
                        self.mm(pY, pY[0:M, :512], hid[f][:, c0:c0 + M], wdv[:, f, :], f == 0, f == 15, reads=[hid[f], wd])
                    self.cp("act", ys[0:M, :], pY[0:M, :512], reads=[pY], writes=[ys])
                    if dst[0] == "ye":
                        self.dma("sp", dr["ye"][dst[1], e, dst[2] * 128:(dst[2] + 1) * 128, dh * 512:(dh + 1) * 512], ys[0:128, :], reads=[ys])
                    else:
                        for s in range(NS):
                            self.dma("sp", dr["yec"][s, e, :, dh * 512:(dh + 1) * 512], ys[s * CAPC:(s + 1) * CAPC, :], reads=[ys])
        self.phase_end()
        sb.release()

    def p7_scatter(self, l, Xm, Xout, last):
        sb, dr, fw, ps = self.sb, self.dr, self.fw, self.ps
        sb.mark()
        W = 512
        rowsel = sb.alloc(32 * 128, BF16)
        self.dma("pool", rowsel[0:32, :], dr["c_rowsel"][:, :], writes=[rowsel])
        iota_p = sb.alloc(2, F32)
        self.dma("sp", iota_p[:, :], dr["c_iota_p"][:, :], writes=[iota_p])
        gfin = sb.alloc(KC, F32)
        self.dma("sp", gfin[:, :], dr["gfinT"][:, :], writes=[gfin])
        affr = sb.alloc(S, BF16); posr = sb.alloc(S, BF16)
        yeall = sb.alloc(NE * 2 * D, BF16, name="yeall")
        selT = [sb.alloc(W, BF16) for i in range(32)]
        affb = [sb.alloc(W, F32) for i in range(2)]
        xin = sb.alloc(KC * W, F32)
        xn = [sb.alloc(W, F32) for c in range(KC)]
        sqb = [sb.alloc(W, BF16) for c in range(KC)]
        ot = [sb.alloc(W, F32) for i in range(2)]
        rstd = sb.alloc(W, F32); lnv = sb.alloc(W, F32)
        groups = [(0, LC, S, 128, 2)]
        if not last:
            groups.append((1, 0, LC, CAPC, 1))
        v3 = lambda t: t[:, :].rearrange("p (c w) -> p c w", c=KC)
        for gi, a0, n, NP, ncc in groups:
            self.dma("pool", affr[0:32, :n], dr["rt"][gi, :, 0, 0:n], writes=[affr])
            self.dma("pool", posr[0:32, :n], dr["rt"][gi, :, 1, 0:n], writes=[posr])
            for s in range(NS):
                e3 = s if gi == 0 else 2
                if gi == 0:
                    yv = yeall[:, :].rearrange("p (e c d) -> p e c d", e=NE, c=2)
                    for q8 in range(8):
                        self.dma("sp", yv[:, q8 * 2:(q8 + 1) * 2, :, :], dr["ye"][s, q8 * 2:(q8 + 1) * 2, :, :].rearrange("e (c p) d -> p e c d", p=128), pwrites=[yeall])
                else:
                    yv = yeall[:, 0:NE * D].rearrange("p (e c d) -> p e c d", e=NE, c=1)
                    self.dma("sp", yv[0:CAPC, :, 0, :], dr["yec"][s].rearrange("e j d -> j e d"), writes=[yeall])
                for c0 in range(0, n, W):
                    w = min(W, n - c0)
                    for e in range(NE):
                        r_ = s * NE + e
                        pP = ps[e % 2]; pA = ps[2 + e % 2]
                        self.mm(pP, pP[:, :w], rowsel[0:32, r_ * 128:(r_ + 1) * 128], posr[0:32, c0:c0 + w], True, True, reads=[rowsel, posr])
                        self.mm(pA, pA[:, :w], rowsel[0:32, r_ * 128:(r_ + 1) * 128], affr[0:32, c0:c0 + w], True, True, reads=[rowsel, affr])
                        ab = affb[e % 2]
                        self.cp("act", ab[:, :w], pA[:, :w], reads=[pA], writes=[ab])
                        for cc in range(ncc):
                            st_ = selT[e * 2 + cc]
                            self.stt(st_[0:NP, :w], pP[0:NP, :w], iota_p[0:NP, cc:cc + 1], ab[0:NP, :w], ALU.is_equal, ALU.mult, reads=[pP, iota_p, ab], writes=[st_])
                    self.dma("sp", v3(xin)[:, :, :w], Xm[s, :, a0 + c0:a0 + c0 + w].rearrange("(c p) w -> p c w", p=128), writes=[xin])
                    for m in range(KC):
                        pY = ps[4 + m % 2]
                        nmm = NE * ncc
                        idx = 0
                        for e in range(NE):
                            for cc in range(ncc):
                                self.mm(pY, pY[:, :w], yv[0:NP, e, cc, m * 128:(m + 1) * 128], selT[e * 2 + cc][0:NP, :w], idx == 0, idx == nmm - 1, reads=[yeall, selT[e * 2 + cc]])
                                idx += 1
                        self.stt(xn[m][:, :w], pY[:, :w], self.mcol(l, 5, m, e3), v3(xin)[:, m, :w], ALU.mult, ALU.add, reads=[pY, self.modc[l], xin], writes=[xn[m]])
                        if not last:
                            self.dma("pool", Xout[s, m * 128:(m + 1) * 128, a0 + c0:a0 + c0 + w], xn[m][:, :w], reads=[xn[m]])
                    if last:
                        self.rstd_from(xn, w, sqb, ps[6], rstd, lnv)
                        for m in range(KC):
                            o_ = ot[m % 2]
                            self.stt(o_[:, :w], xn[m][:, :w], gfin[:, m:m + 1], rstd[:, :w], ALU.mult, ALU.mult, reads=[xn[m], gfin, rstd], writes=[o_])
                            self.dma("pool", dr["outT"][s, m * 128:(m + 1) * 128, c0:c0 + w], o_[:, :w], reads=[o_])
        self.phase_end()
        sb.release()

def prep_shared(inp):
    f = lambda a: np.ascontiguousarray(a, dtype=np.float32)
    sh = {}
    sh["ada_w"] = f(inp["ada_w"])
    sh["ada_bT"] = f(inp["ada_b"].reshape(DEPTH, 48, 128).transpose(0, 2, 1))
    sh["gmixT"] = f(inp["norm_mix_g"].reshape(DEPTH, KC, 128).transpose(0, 2, 1))
    sh["gffnT"] = f(inp["norm_ffn_g"].reshape(DEPTH, KC, 128).transpose(0, 2, 1))
    sh["gfinT"] = f(inp["final_norm_g"].reshape(KC, 128).T)
    sh["w_in"] = f(inp["w_in"])
    sh["sink"] = f(inp["attn_sink"].reshape(DEPTH, 1, 8))
    cw = np.concatenate([inp["conv_w"], inp["conv_b"][:, None, :]], axis=1)
    sh["convT"] = f(cw.reshape(DEPTH, 5, KC, 128).transpose(0, 3, 1, 2))
    sh["lru_w"] = f(np.stack([inp["lru_wa"], inp["lru_wx"]], axis=1))
    lv = np.stack([inp["lru_ba"], inp["lru_bx"], inp["lru_lambda"]], axis=1)
    sh["lru_vT"] = f(lv.reshape(DEPTH, 3, 2, KC, 128).transpose(0, 4, 1, 2, 3))
    for k in ("w_attn_br", "w_rec_br", "w_out", "w_router", "w_gate", "w_up", "w_down"):
        sh[k] = f(inp[k])
    for k, v in host_consts().items():
        sh["c_" + k] = f(v)
    return sh


def prep_core(inp, core):
    b0 = core * NS
    xs = inp["x"][b0:b0 + NS]
    cs = inp["ctx"][b0:b0 + NS]
    xT = np.concatenate([cs, xs], axis=1).transpose(0, 2, 1)
    cc = np.stack([inp["c"][b0], inp["c"][b0 + 1], inp["c_ctx"]], axis=1)
    return {"xT": np.ascontiguousarray(xT, dtype=np.float32),
            "cT": np.ascontiguousarray(cc.reshape(KC, 128, 3).transpose(1, 0, 2), dtype=np.float32)}


_NC_CACHE = {}


def kernel(**inputs):
    inp = {k: np.asarray(v) for k, v in inputs.items()}
    n = 8
    if "nc" not in _NC_CACHE:
        kb = KB()
        _NC_CACHE["nc"] = kb.build()
        _NC_CACHE["names"] = set(kb.dr.keys())
    nc = _NC_CACHE["nc"]
    sh = prep_shared(inp)
    in_maps = []
    for core in range(n):
        m = dict(sh)
        m.update(prep_core(inp, core))
        in_maps.append({k: v for k, v in m.items() if k in _NC_CACHE["names"]})
    res = run_bass_kernel_spmd(nc, in_maps, core_ids=list(range(n)))
    outs = [np.asarray(r["outT"]).transpose(0, 2, 1) for r in res.results]
    return np.ascontiguousarray(np.concatenate(outs, axis=0), dtype=np.float32)
```
